# Optimizing a Trainium2 kernel written in Bass

```python
import jax
import jax.numpy as jnp
from jax import lax
import numpy as np

D_MODEL = 1024
BATCH = 8
SEQ = 4096
DEPTH = 1

GRID_W = 64
CTX_LEN = 256
HEAD_DIM = 64
ATT_HEADS = 8
ATT_KV_HEADS = 2
ATT_GROUPS = ATT_HEADS // ATT_KV_HEADS
ATT_DIM = ATT_HEADS * HEAD_DIM
KV_DIM = ATT_KV_HEADS * HEAD_DIM
WINDOW = 128
BLOCK = 128
ROPE_BASE = 10000.0
ATT_SCALE = HEAD_DIM ** -0.5
NEG_INF = -1e30
RWKV_HEADS = 8
RWKV_DIM = RWKV_HEADS * HEAD_DIM
DECAY_LORA = 64
ICL_LORA = 64
GATE_LORA = 128
GN_EPS = 64e-5
N_EXPERTS = 32
TOP_K = 4
D_EXPERT = D_MODEL
SWIGLU_LIMIT = 7.0
SWIGLU_ALPHA = 1.702
MOE_BLOCK = 128
RMS_EPS = 1e-6
ATT_COLS = ATT_DIM + 2 * KV_DIM
RWKV_SIZES = (RWKV_DIM, RWKV_DIM, RWKV_DIM, DECAY_LORA, DECAY_LORA, ICL_LORA, ICL_LORA, GATE_LORA)
RWKV_COLS = sum(RWKV_SIZES)
RWKV_OFFSETS = tuple(int(o) for o in np.cumsum(RWKV_SIZES)[:-1])
GATE_COLS = 2 * D_MODEL
PROJ_COLS = ATT_COLS + RWKV_COLS + GATE_COLS
F32 = jnp.float32

kernel_name = 'hybrid_gqa_rwkv7_moe_diffusion_layer'


def rms_norm(x, g):
    xf = x.astype(F32)
    y = xf * lax.rsqrt(jnp.mean(xf * xf, axis=-1, keepdims=True) + RMS_EPS)
    return y.astype(x.dtype) * g


def modulate(h, shift, scale):
    return h * (1.0 + scale) + shift


def _rotate_half(t, ang):
    cos = jnp.cos(ang)[None, :, None, :].astype(t.dtype)
    sin = jnp.sin(ang)[None, :, None, :].astype(t.dtype)
    t1, t2 = jnp.split(t, 2, axis=-1)
    return jnp.concatenate([t1 * cos - t2 * sin, t2 * cos + t1 * sin], axis=-1)


def axial_rope(t):
    L = t.shape[1]
    n_rows = L // GRID_W
    row = jnp.repeat(jnp.arange(n_rows, dtype=F32), GRID_W, total_repeat_length=L)
    col = jnp.tile(jnp.arange(GRID_W, dtype=F32), n_rows)
    half = HEAD_DIM // 2
    inv_freq = ROPE_BASE ** (-jnp.arange(0, half, 2, dtype=F32) / half)
    t_row, t_col = jnp.split(t, 2, axis=-1)
    return jnp.concatenate([_rotate_half(t_row, row[:, None] * inv_freq[None, :]),
                            _rotate_half(t_col, col[:, None] * inv_freq[None, :])], axis=-1)


def token_shift(z, mu_prev, mu_next):
    prev = jnp.pad(z[:, :-1], ((0, 0), (1, 0), (0, 0)))
    nxt = jnp.pad(z[:, 1:], ((0, 0), (0, 1), (0, 0)))
    return z + mu_prev * (prev - z) + mu_next * (nxt - z)


def rwkv_features(z, p):
    B, L, _ = z.shape
    r, k, v, wl_f, wl_b, al_f, al_b, gl = jnp.split(z, RWKV_OFFSETS, axis=-1)

    def heads(t):
        return t.reshape(B, L, RWKV_HEADS, HEAD_DIM)

    def decay(w_low, w0, w2):
        w = (w0 + jnp.tanh(w_low) @ w2).astype(F32)
        w = -jax.nn.softplus(-w) - 0.5
        return heads(jnp.exp(-jnp.exp(w)))

    icl_f = jax.nn.sigmoid(p['a0_f'] + al_f @ p['a2_f'])
    icl_b = jax.nn.sigmoid(p['a0_b'] + al_b @ p['a2_b'])
    kk = heads(k * p['k_k']).astype(F32)
    kk = (kk / jnp.maximum(jnp.sqrt(jnp.sum(kk * kk, axis=-1, keepdims=True)), 1e-12)).astype(z.dtype)
    return dict(
        r=heads(r), v=heads(v),
        k_f=heads(k * (1.0 + (icl_f - 1.0) * p['k_a'])),
        k_b=heads(k * (1.0 + (icl_b - 1.0) * p['k_a'])),
        dec_f=decay(wl_f, p['w0_f'], p['w2_f']),
        dec_b=decay(wl_b, p['w0_b'], p['w2_b']),
        a=-kk, b_f=kk * heads(icl_f), b_b=kk * heads(icl_b),
        gate=jax.nn.sigmoid(gl) @ p['g2'])


def wkv_scan(r, w, k, v, a, b, state0, reverse):
    def step(S, inp):
        r_t, w_t, k_t, v_t, a_t, b_t = inp
        sa = jnp.einsum('bhij,bhj->bhi', S, a_t)
        S = S * w_t[:, :, None, :] + sa[..., None] * b_t[:, :, None, :] + v_t[..., None] * k_t[:, :, None, :]
        return S, jnp.einsum('bhij,bhj->bhi', S, r_t)

    xs = tuple(jnp.moveaxis(t.astype(F32), 1, 0) for t in (r, w, k, v, a, b))
    S, ys = lax.scan(step, state0, xs, reverse=reverse)
    return jnp.moveaxis(ys, 0, 1), S


def bidir_scan(f, s0_f, s0_b):
    y_f, s_f = wkv_scan(f['r'], f['dec_f'], f['k_f'], f['v'], f['a'], f['b_f'], s0_f, False)
    y_b, s_b = wkv_scan(f['r'], f['dec_b'], f['k_b'], f['v'], f['a'], f['b_b'], s0_b, True)
    return y_f + y_b, s_f, s_b


def rwkv_output(y, f, p):
    B, L = y.shape[:2]
    mean = jnp.mean(y, axis=-1, keepdims=True)
    var = jnp.mean(jnp.square(y - mean), axis=-1, keepdims=True)
    yn = ((y - mean) * lax.rsqrt(var + GN_EPS)).reshape(B, L, RWKV_DIM)
    yn = (yn * p['ln_x_w'] + p['ln_x_b']).astype(f['v'].dtype)
    bonus = jnp.sum(f['r'] * (f['k_f'] + f['k_b']) * p['r_k'], axis=-1, keepdims=True) * f['v']
    return (yn + bonus.reshape(B, L, RWKV_DIM)) * f['gate']


def project(h, p):
    B, L, _ = h.shape
    z = h @ p['w_in'] + p['b_in']
    z_att, z_rwkv, z_gate = jnp.split(z, [ATT_COLS, ATT_COLS + RWKV_COLS], axis=-1)
    q, k, v = jnp.split(z_att, [ATT_DIM, ATT_DIM + KV_DIM], axis=-1)
    q = q.reshape(B, L, ATT_HEADS, HEAD_DIM)
    k = k.reshape(B, L, ATT_KV_HEADS, HEAD_DIM)
    v = v.reshape(B, L, ATT_KV_HEADS, HEAD_DIM)
    feats = rwkv_features(token_shift(z_rwkv, p['mu_prev'], p['mu_next']), p)
    return q, k, v, feats, z_gate


def sink_softmax(logits, sinks):
    sink = jnp.broadcast_to(sinks.reshape(ATT_KV_HEADS, ATT_GROUPS, 1, 1).astype(F32), logits.shape[:-1] + (1,))
    return jax.nn.softmax(jnp.concatenate([logits, sink], axis=-1), axis=-1)[..., :-1]


def window_attention(q, k, v, k_ctx, v_ctx, sinks):
    B, L = q.shape[:2]
    nb = L // BLOCK
    band = BLOCK + 2 * WINDOW
    qb = jnp.moveaxis(q.reshape(B, nb, BLOCK, ATT_KV_HEADS, ATT_GROUPS, HEAD_DIM), 1, 0)
    kp = jnp.pad(k, ((0, 0), (WINDOW, WINDOW), (0, 0), (0, 0)))
    vp = jnp.pad(v, ((0, 0), (WINDOW, WINDOW), (0, 0), (0, 0)))
    k_off = jnp.arange(band) - WINDOW
    rel = k_off[None, :] - jnp.arange(BLOCK)[:, None]

    def one_block(args):
        n, q_n = args
        k_n = lax.dynamic_slice_in_dim(kp, n * BLOCK, band, axis=1)
        v_n = lax.dynamic_slice_in_dim(vp, n * BLOCK, band, axis=1)
        kpos = n * BLOCK + k_off
        valid = (jnp.abs(rel) <= WINDOW) & ((kpos >= 0) & (kpos < L))[None, :]
        s_lat = jnp.einsum('bqkgd,bskd->bkgqs', q_n, k_n).astype(F32) * ATT_SCALE
        s_lat = jnp.where(valid, s_lat, NEG_INF)
        s_ctx = jnp.einsum('bqkgd,bckd->bkgqc', q_n, k_ctx).astype(F32) * ATT_SCALE
        prob = sink_softmax(jnp.concatenate([s_lat, s_ctx], axis=-1), sinks).astype(v.dtype)
        return (jnp.einsum('bkgqs,bskd->bqkgd', prob[..., :band], v_n)
                + jnp.einsum('bkgqc,bckd->bqkgd', prob[..., band:], v_ctx))

    o = lax.map(one_block, (jnp.arange(nb), qb))
    return jnp.moveaxis(o, 0, 1).reshape(B, L, ATT_DIM)


def context_attention(q, k, v, sinks):
    B, C = q.shape[:2]
    qg = q.reshape(B, C, ATT_KV_HEADS, ATT_GROUPS, HEAD_DIM)
    s = jnp.einsum('bqkgd,bckd->bkgqc', qg, k).astype(F32) * ATT_SCALE
    prob = sink_softmax(s, sinks).astype(v.dtype)
    return jnp.einsum('bkgqc,bckd->bqkgd', prob, v).reshape(B, C, ATT_DIM)


def merge_branches(att, rwk, z_gate, p):
    g_att, g_rwkv = jnp.split(jax.nn.sigmoid(z_gate), 2, axis=-1)
    merged = g_att * (att @ p['w_up_att']) + g_rwkv * (rwk @ p['w_up_rwkv'])
    return merged @ p['w_out']


def moe_ffn(h, p):
    B, L, D = h.shape
    xt = h.reshape(-1, D)
    T = xt.shape[0]
    TK = T * TOP_K
    logits = (xt @ p['w_router'] + p['b_router']).astype(F32)
    top_val, top_idx = lax.top_k(logits, TOP_K)
    gates = jax.nn.softmax(top_val, axis=-1).astype(h.dtype)
    flat_e = top_idx.reshape(-1).astype(jnp.int32)
    flat_tok = jnp.repeat(jnp.arange(T, dtype=jnp.int32), TOP_K)
    order = jnp.argsort(flat_e)
    e_sorted = flat_e[order]
    counts = jnp.zeros((N_EXPERTS,), jnp.int32).at[flat_e].add(1)
    padded = ((counts + MOE_BLOCK - 1) // MOE_BLOCK) * MOE_BLOCK
    start = jnp.cumsum(counts) - counts
    p_end = jnp.cumsum(padded)
    p_start = p_end - padded
    dest = p_start[e_sorted] + jnp.arange(TK, dtype=jnp.int32) - start[e_sorted]
    n_blocks = -(-TK // MOE_BLOCK) + N_EXPERTS
    n_slots = n_blocks * MOE_BLOCK
    slot_tok = jnp.full((n_slots,), T, jnp.int32).at[dest].set(flat_tok[order])
    slot_g = jnp.zeros((n_slots,), h.dtype).at[dest].set(gates.reshape(-1)[order])
    block_exp = jnp.minimum(jnp.searchsorted(p_end, jnp.arange(n_blocks, dtype=jnp.int32) * MOE_BLOCK, side='right'),
                            N_EXPERTS - 1).astype(jnp.int32)
    x_pad = jnp.concatenate([xt, jnp.zeros((1, D), xt.dtype)], axis=0)
    xb = x_pad[slot_tok].reshape(n_blocks, MOE_BLOCK, D)
    w_gu, b_gu, w_dn, b_dn = p['w_gate_up'], p['b_gate_up'], p['w_down'], p['b_down']

    def expert_block(args):
        e, x_b = args
        gu = x_b @ w_gu[e] + b_gu[e]
        gate = jnp.minimum(gu[..., ::2], SWIGLU_LIMIT)
        up = jnp.clip(gu[..., 1::2], -SWIGLU_LIMIT, SWIGLU_LIMIT)
        return ((up + 1.0) * (gate * jax.nn.sigmoid(SWIGLU_ALPHA * gate))) @ w_dn[e] + b_dn[e]

    yb = lax.map(expert_block, (block_exp, xb)).reshape(n_slots, D)
    y = jnp.zeros((T + 1, D), yb.dtype).at[slot_tok].add(yb * slot_g[:, None])[:T]
    return y.reshape(B, L, D)


def setup_inputs(seed: int = 0) -> dict:
    key = jax.random.key(seed)
    ks = iter(jax.random.split(key, 48))
    D = D_MODEL

    def nrm(shape, scale):
        return scale * jax.random.normal(next(ks), shape, F32)

    def gain(shape):
        return 1.0 + nrm(shape, 0.05)

    return {
        'x': nrm((BATCH, SEQ, D), 1.0),
        'c': nrm((BATCH, D), 1.0),
        'ctx': nrm((BATCH, CTX_LEN, D), 1.0),
        'c_ctx': nrm((D,), 1.0),
        'w_ada': nrm((DEPTH, D, 6 * D), 0.5 * D ** -0.5),
        'b_ada': nrm((DEPTH, 6 * D), 0.02),
        'g_pre_mix': gain((DEPTH, D)),
        'g_post_mix': gain((DEPTH, D)),
        'g_pre_ffn': gain((DEPTH, D)),
        'g_post_ffn': gain((DEPTH, D)),
        'w_in': nrm((DEPTH, D, PROJ_COLS), D ** -0.5),
        'b_in': nrm((DEPTH, PROJ_COLS), 0.02),
        'mu_prev': jax.random.uniform(next(ks), (DEPTH, RWKV_COLS), F32, 0.0, 0.5),
        'mu_next': jax.random.uniform(next(ks), (DEPTH, RWKV_COLS), F32, 0.0, 0.5),
        'att_sinks': nrm((DEPTH, ATT_HEADS), 0.5),
        'w0_f': jax.random.uniform(next(ks), (DEPTH, RWKV_DIM), F32, -5.0, 0.0),
        'w0_b': jax.random.uniform(next(ks), (DEPTH, RWKV_DIM), F32, -5.0, 0.0),
        'w2_f': nrm((DEPTH, DECAY_LORA, RWKV_DIM), 0.5 * DECAY_LORA ** -0.5),
        'w2_b': nrm((DEPTH, DECAY_LORA, RWKV_DIM), 0.5 * DECAY_LORA ** -0.5),
        'a0_f': nrm((DEPTH, RWKV_DIM), 0.5),
        'a0_b': nrm((DEPTH, RWKV_DIM), 0.5),
        'a2_f': nrm((DEPTH, ICL_LORA, RWKV_DIM), 0.5 * ICL_LORA ** -0.5),
        'a2_b': nrm((DEPTH, ICL_LORA, RWKV_DIM), 0.5 * ICL_LORA ** -0.5),
        'g2': nrm((DEPTH, GATE_LORA, RWKV_DIM), GATE_LORA ** -0.5),
        'k_k': 0.85 + nrm((DEPTH, RWKV_DIM), 0.05),
        'k_a': gain((DEPTH, RWKV_DIM)),
        'r_k': nrm((DEPTH, RWKV_HEADS, HEAD_DIM), 0.1),
        'ln_x_w': gain((DEPTH, RWKV_DIM)),
        'ln_x_b': nrm((DEPTH, RWKV_DIM), 0.02),
        'w_up_att': nrm((DEPTH, ATT_DIM, D), ATT_DIM ** -0.5),
        'w_up_rwkv': nrm((DEPTH, RWKV_DIM, D), RWKV_DIM ** -0.5),
        'w_out': nrm((DEPTH, D, D), D ** -0.5),
        'w_router': nrm((DEPTH, D, N_EXPERTS), D ** -0.5),
        'b_router': nrm((DEPTH, N_EXPERTS), 0.01),
        'w_gate_up': nrm((DEPTH, N_EXPERTS, D, 2 * D_EXPERT), D ** -0.5),
        'b_gate_up': nrm((DEPTH, N_EXPERTS, 2 * D_EXPERT), 0.02),
        'w_down': nrm((DEPTH, N_EXPERTS, D_EXPERT, D), D_EXPERT ** -0.5),
        'b_down': nrm((DEPTH, N_EXPERTS, D), 0.02),
    }


def reference(x, c, ctx, c_ctx, w_ada, b_ada, g_pre_mix, g_post_mix, g_pre_ffn, g_post_ffn,
              w_in, b_in, mu_prev, mu_next, att_sinks, w0_f, w0_b, w2_f, w2_b, a0_f, a0_b,
              a2_f, a2_b, g2, k_k, k_a, r_k, ln_x_w, ln_x_b, w_up_att, w_up_rwkv, w_out,
              w_router, b_router, w_gate_up, b_gate_up, w_down, b_down):
    B = x.shape[0]
    silu_c = jax.nn.silu(c)
    silu_cc = jax.nn.silu(c_ctx)
    for l in range(DEPTH):
        p = dict(w_in=w_in[l], b_in=b_in[l], mu_prev=mu_prev[l], mu_next=mu_next[l], sinks=att_sinks[l],
                 w0_f=w0_f[l], w0_b=w0_b[l], w2_f=w2_f[l], w2_b=w2_b[l], a0_f=a0_f[l], a0_b=a0_b[l],
                 a2_f=a2_f[l], a2_b=a2_b[l], g2=g2[l], k_k=k_k[l], k_a=k_a[l], r_k=r_k[l],
                 ln_x_w=ln_x_w[l], ln_x_b=ln_x_b[l], w_up_att=w_up_att[l], w_up_rwkv=w_up_rwkv[l],
                 w_out=w_out[l], w_router=w_router[l], b_router=b_router[l], w_gate_up=w_gate_up[l],
                 b_gate_up=b_gate_up[l], w_down=w_down[l], b_down=b_down[l])
        mx = [m[:, None, :] for m in jnp.split(silu_c @ w_ada[l] + b_ada[l], 6, axis=-1)]
        mc = jnp.split(silu_cc @ w_ada[l] + b_ada[l], 6, axis=-1)
        hc = modulate(rms_norm(ctx, g_pre_mix[l]), mc[0], mc[1])
        qc, kc, vc, fc, zc_gate = project(hc, p)
        zero_state = jnp.zeros((B, RWKV_HEADS, HEAD_DIM, HEAD_DIM), F32)
        yc, sc_f, sc_b = bidir_scan(fc, zero_state, zero_state)
        hx = modulate(rms_norm(x, g_pre_mix[l]), mx[0], mx[1])
        q, k, v, fx, zx_gate = project(hx, p)
        att_x = window_attention(axial_rope(q), axial_rope(k), v, kc, vc, p['sinks'])
        yx, _, _ = bidir_scan(fx, sc_f, sc_b)
        mix_x = merge_branches(att_x, rwkv_output(yx, fx, p), zx_gate, p)
        x = x + mx[2] * rms_norm(mix_x, g_post_mix[l])
        hf = modulate(rms_norm(x, g_pre_ffn[l]), mx[3], mx[4])
        x = x + mx[5] * rms_norm(moe_ffn(hf, p), g_post_ffn[l])
        if l + 1 < DEPTH:
            mix_c = merge_branches(context_attention(qc, kc, vc, p['sinks']), rwkv_output(yc, fc, p), zc_gate, p)
            ctx = ctx + mc[2] * rms_norm(mix_c, g_post_mix[l])
            hcf = modulate(rms_norm(ctx, g_pre_ffn[l]), mc[3], mc[4])
            ctx = ctx + mc[5] * rms_norm(moe_ffn(hcf, p), g_post_ffn[l])
    return x
```

```python
import numpy as np
import concourse.bass as bass
import concourse.mybir as mybir
from concourse.bass_utils import run_bass_kernel_spmd

F32 = mybir.dt.float32
BF16 = mybir.dt.bfloat16
AF = mybir.ActivationFunctionType
ALU = mybir.AluOpType

SEQ = 4096
CTX = 256
TT = SEQ + CTX
D = 1024
HD = 64
NH = 8
C = 64
W = 64
NEXP = 32
NB = SEQ * 4 // 128 + NEXP


class Buf:
    __slots__ = ("name", "t", "lw", "rd", "fence")

    def __init__(self, name, t):
        self.name = name
        self.t = t
        self.lw = []
        self.rd = []
        self.fence = []

    def __getitem__(self, idx):
        return self.t[idx]


class Prog:
    ENG = ("pe", "act", "dve", "pool", "sp")

    def __init__(self, nc, n_dma_sems=32):
        self.nc = nc
        self.q = {e: [] for e in self.ENG}
        self.cnt = {}
        self.known = {e: {} for e in self.ENG}
        self.sems = {}
        self.n_dma_sems = n_dma_sems
        self.dma_i = 0
        self.stack = []
        self.ninst = 0
        self.uid = 0
        self.tot = {}
        self.regs = {}

    def enter(self, cm):
        v = cm.__enter__()
        self.stack.append(cm)
        return v

    def mark(self):
        return len(self.stack)

    def release(self, mark):
        while len(self.stack) > mark:
            self.stack.pop().__exit__(None, None, None)

    def sbuf(self, name, shape, dtype=F32):
        self.uid += 1
        t = self.enter(self.nc.sbuf_tensor("%s_%d" % (name, self.uid), list(shape), dtype))
        return Buf(name, t)

    def psum(self, name, shape, dtype=F32):
        self.uid += 1
        t = self.enter(self.nc.psum_tensor("%s_%d" % (name, self.uid), list(shape), dtype))
        return Buf(name, t)

    def dram(self, name, shape, dtype=F32):
        t = self.nc.dram_tensor(name, list(shape), dtype, kind="Internal")
        return Buf(name, t.ap())

    def sem(self, key):
        if key not in self.sems:
            nm = "s_" + "_".join(str(k) for k in (key if isinstance(key, tuple) else (key,)))
            self.sems[key] = self.enter(self.nc.semaphore(nm))
            self.cnt[key] = 0
        return self.sems[key]

    def _deps(self, eng, reads, writes, shared=()):
        deps = {}

        def add(d):
            k, v = d
            if deps.get(k, 0) < v:
                deps[k] = v
        for b in reads:
            for d in b.lw:
                add(d)
            for d in b.fence:
                add(d)
        for b in writes:
            for d in b.lw:
                add(d)
            for d in b.fence:
                add(d)
            for d in b.rd:
                add(d)
        for b in shared:
            for d in b.fence:
                add(d)
            for d in b.rd:
                add(d)
        out = []
        for k, v in deps.items():
            if eng == "pe" and isinstance(k, tuple) and k[0] == "pe":
                continue
            if self.known[eng].get(k, 0) >= v:
                continue
            self.known[eng][k] = v
            out.append((k, v))
        return out

    @staticmethod
    def _compact(lst):
        mx = {}
        for k, v in lst:
            if mx.get(k, 0) < v:
                mx[k] = v
        return list(mx.items())

    def _mark(self, key, val, reads, writes, shared=()):
        for b in reads:
            b.rd.append((key, val))
            if len(b.rd) > 64:
                b.rd = self._compact(b.rd)
        for b in writes:
            b.lw = [(key, val)]
            b.rd = []
            b.fence = []
        for b in shared:
            b.lw.append((key, val))
            if len(b.lw) > 64:
                b.lw = self._compact(b.lw)

    def fence(self, b):
        b.fence = self._compact(b.fence + b.lw)
        b.lw = []

    EPOCH = 4000

    def op(self, eng, fn, reads=(), writes=()):
        n = self.tot.get(eng, 0)
        self.tot[eng] = n + 1
        key = (eng, n // self.EPOCH)
        self.sem(key)
        waits = self._deps(eng, reads, writes)
        self.cnt[key] += 1
        val = self.cnt[key]
        self.q[eng].append(("op", fn, waits, key, 1))
        self._mark(key, val, reads, writes)
        self.ninst += 1

    def dma(self, eng, out, in_, reads=(), writes=(), shared=(), ind=None):
        i = self.dma_i % self.n_dma_sems
        self.dma_i += 1
        key = ("dma", i)
        self.sem(key)
        waits = self._deps(eng, reads, writes, shared)
        prev = self.cnt[key]
        if prev > 0 and self.known[eng].get(key, 0) < prev:
            self.known[eng][key] = prev
            waits.append((key, prev))
        self.cnt[key] += 16
        val = self.cnt[key]
        self.q[eng].append(("dma", (out, in_, ind), waits, key, 16))
        self._mark(key, val, reads, writes, shared)
        self.ninst += 1

    def reg(self, e, val):
        k = (id(e), val)
        if k not in self.regs:
            self.regs[k] = e.to_reg(val)
        return self.regs[k]

    def barrier(self):
        snap = dict(self.cnt)
        for e in self.ENG:
            waits = []
            for k, v in snap.items():
                if v > 0 and self.known[e].get(k, 0) < v:
                    self.known[e][k] = v
                    waits.append((k, v))
            if waits:
                self.q[e].append(("wait", None, waits, None, 0))

    def emit(self):
        nc = self.nc
        block = self.enter(nc.Block())
        engmap = {"pe": "tensor", "act": "scalar", "dve": "vector", "pool": "gpsimd", "sp": "sync"}
        prog = self

        def make(ename):
            items = prog.q[ename]

            def body(e):
                for kind, payload, waits, key, inc in items:
                    for (k, v) in waits:
                        e.wait_ge(prog.sems[k], v)
                    if kind == "op":
                        payload(e).then_inc(prog.sems[key], inc)
                    elif kind == "dma":
                        o, i, ind = payload
                        if ind is None:
                            e.dma_start(out=o, in_=i).then_inc(prog.sems[key], inc)
                        else:
                            idx_ap, on_out, bound = ind
                            off = bass.IndirectOffsetOnAxis(ap=idx_ap, axis=0)
                            try:
                                ins = e.indirect_dma_start(out=o, out_offset=off if on_out else None, in_=i,
                                                           in_offset=None if on_out else off, bounds_check=prog.reg(e, bound),
                                                           oob_is_err=False)
                            except Exception:
                                print("INDIRECT FAIL", o.shape, o.ap, i.shape, i.ap, idx_ap.shape, idx_ap.ap, on_out, bound)
                                raise
                            ins.then_inc(prog.sems[key], inc)
            return body

        for ename in self.ENG:
            if self.q[ename]:
                getattr(block, engmap[ename])(make(ename))

    def close(self):
        self.release(0)


def tt(P, eng, out, i0, i1, op, reads, writes):
    P.op(eng, lambda e: e.tensor_tensor(out=out, in0=i0, in1=i1, op=op), reads=reads, writes=writes)


def act(P, out, in_, func, reads, writes, bias=None, scale=None):
    kw = {}
    if bias is not None:
        kw["bias"] = bias
    if scale is not None:
        kw["scale"] = scale
    P.op("act", lambda e: e.activation(out=out, in_=in_, func=func, **kw), reads=reads, writes=writes)


def mm(P, out, lhsT, rhs, start, stop, reads, writes):
    P.op("pe", lambda e: e.matmul(out, lhsT=lhsT, rhs=rhs, start=start, stop=stop), reads=reads, writes=writes)


def tr(P, out, in_, ident, reads, writes):
    P.op("pe", lambda e: e.transpose(out, in_, ident), reads=reads, writes=writes)


def make_consts(P):
    cs = {}
    ones = P.sbuf("ones128", [128, 128])
    P.op("pool", lambda e: e.memset(ones[:], 1.0), writes=[ones])
    cs["ones"] = ones

    def sel(name, cmp, base, cm, pat):
        b = P.sbuf(name, [128, 128])
        P.op("pool", lambda e: e.affine_select(out=b[:], in_=ones[:], pattern=[[pat, 128]], compare_op=cmp,
                                               fill=P.reg(e, 0.0), base=base, channel_multiplier=cm),
             reads=[ones], writes=[b])
        cs[name] = b
    sel("ident", ALU.is_equal, 0, 1, -1)
    sel("Lstrict", ALU.is_gt, 0, -1, 1)
    sel("Lincl", ALU.is_ge, 0, -1, 1)
    sel("Ustrict", ALU.is_gt, 0, 1, -1)
    sel("Uincl", ALU.is_ge, 0, 1, -1)
    return cs


P64 = {}
_o = 0
for _n, _w in [("mup3", 24), ("mun3", 24), ("mup_wl", 2), ("mun_wl", 2), ("mup_al", 2), ("mun_al", 2),
               ("k_k", 8), ("k_a", 8), ("r_k", 8), ("w0_f", 8), ("w0_b", 8), ("a0_f", 8), ("a0_b", 8),
               ("ln_w", 8), ("ln_b", 8)]:
    P64[_n] = (_o, _w)
    _o += _w
NP64 = _o


def pack64(inp):
    def hj(v):
        return np.ascontiguousarray(np.asarray(v, np.float32).reshape(-1, 64).T)
    mp, mn = inp["mu_prev"][0], inp["mu_next"][0]
    parts = {
        "mup3": hj(mp[0:1536]), "mun3": hj(mn[0:1536]),
        "mup_wl": hj(mp[1536:1664]), "mun_wl": hj(mn[1536:1664]),
        "mup_al": hj(mp[1664:1792]), "mun_al": hj(mn[1664:1792]),
        "k_k": hj(inp["k_k"][0]), "k_a": hj(inp["k_a"][0]), "r_k": hj(inp["r_k"][0].reshape(-1)),
        "w0_f": hj(inp["w0_f"][0]), "w0_b": hj(inp["w0_b"][0]),
        "a0_f": hj(inp["a0_f"][0]), "a0_b": hj(inp["a0_b"][0]),
        "ln_w": hj(inp["ln_x_w"][0]), "ln_b": hj(inp["ln_x_b"][0]),
    }
    out = np.zeros((64, NP64), np.float32)
    for n, (o, w) in P64.items():
        out[:, o:o + w] = parts[n]
    return out


class EW:
    def __init__(self, engines=("dve", "pool")):
        self.e = engines
        self.i = 0

    def __call__(self):
        self.i += 1
        return self.e[self.i % len(self.e)]


def rwkv_setup(P, dr):
    R = {}
    pp = P.sbuf("pp64", [64, NP64])
    P.dma("sp", pp[:], dr["pp64"][:], writes=[pp])
    R["pp"] = pp
    for n in ("w2_f", "w2_b", "a2_f", "a2_b"):
        b = P.sbuf(n, [64, 512])
        P.dma("sp", b[:], dr[n][:], writes=[b])
        R[n] = b
    g2 = P.sbuf("g2", [128, 512])
    P.dma("sp", g2[:], dr["g2"][:], writes=[g2])
    R["g2"] = g2
    mugl = P.sbuf("mugl", [128, 2])
    P.dma("sp", mugl[:], dr["mugl"][:], writes=[mugl])
    R["mugl"] = mugl
    eps18 = P.sbuf("eps18", [64, 1])
    P.op("pool", lambda e: e.memset(eps18[:], 1e-18), writes=[eps18])
    R["eps18"] = eps18
    omka = P.sbuf("omka", [64, 8])
    o, w = P64["k_a"]
    P.op("dve", lambda e: e.tensor_scalar(out=omka[:], in0=pp[:, o:o + w], scalar1=-1.0, scalar2=1.0,
                                          op0=ALU.mult, op1=ALU.add), reads=[pp], writes=[omka])
    R["omka"] = omka
    return R


def rwkv_dir(P, cs, R, dirn, dr, PB, dbg=None, max_win=None):
    nwin = TT // W
    nch = W // C
    nctx = CTX // W
    if dirn == 0:
        order = list(range(nwin))
    else:
        order = list(range(nctx - 1, -1, -1)) + list(range(nwin - 1, nctx - 1, -1))
    if max_win is not None:
        order = order[:max_win]
    pp = R["pp"]
    ident = cs["ident"]
    ones = cs["ones"]
    idn = ident[0:64, 0:64]

    def prm(n):
        o, w = P64[n]
        return pp[:, o:o + w]

    def bc(ap, shape):
        return ap.broadcast_to(shape)

    dsuf = "_f" if dirn == 0 else "_b"
    mAR = P.sbuf("mAR", [64, 128])
    mNT = P.sbuf("mNT", [64, 64])
    st, inc, ntm = ("Lstrict", "Lincl", "Ustrict") if dirn == 0 else ("Ustrict", "Uincl", "Lstrict")
    P.op("dve", lambda e: e.tensor_copy(out=mAR[:, 0:64], in_=cs[st][0:64, 0:64]), reads=[cs[st]], writes=[mAR])
    P.op("dve", lambda e: e.tensor_copy(out=mAR[:, 64:128], in_=cs[inc][0:64, 0:64]), reads=[cs[inc]], writes=[mAR])
    P.op("dve", lambda e: e.tensor_copy(out=mNT[:], in_=cs[ntm][0:64, 0:64]), reads=[cs[ntm]], writes=[mNT])

    ST = P.sbuf("ST", [64, 8, 64], BF16)
    P.op("pool", lambda e: e.memset(ST[:], 0.0), writes=[ST])
    identb = P.sbuf("identb_r", [64, 64], BF16)
    P.op("dve", lambda e: e.tensor_copy(out=identb[:], in_=ident[0:64, 0:64]), reads=[ident], writes=[identb])

    Z3 = P.sbuf("Z3", [64, 24, W + 2])
    ZW = P.sbuf("ZW", [64, 2, W + 2])
    ZA = P.sbuf("ZA", [64, 2, W + 2])
    ZG = P.sbuf("ZG", [128, W + 2])
    zs3 = P.sbuf("zs3", [64, 24, W])
    wls = P.sbuf("wls", [64, 2, W])
    als = P.sbuf("als", [64, 2, W])
    gls = P.sbuf("gls", [128, W])
    tmps = P.sbuf("tmps", [128, W])
    icl = [P.sbuf("icl0", [64, 8, W]), P.sbuf("icl1", [64, 8, W])]
    lw = P.sbuf("lw", [64, 8, W])
    kkn = P.sbuf("kkn", [64, 8, W])
    t8a = P.sbuf("t8a", [64, 8, W])
    t8b = P.sbuf("t8b", [64, 8, W])
    bd = P.sbuf("bd", [64, 8, W])
    kd = [P.sbuf("kd0", [64, 8, W]), P.sbuf("kd1", [64, 8, W])]
    cw = P.sbuf("cw", [64, 8, W])
    E1s = [P.sbuf("E1_%d" % i, [64, 8, W]) for i in range(2)]
    vHs = [P.sbuf("vH_%d" % i, [64, 8, W]) for i in range(2)]
    Einv = P.sbuf("Einv", [64, 8, W])
    Ehat = P.sbuf("Ehat", [64, 8, W])
    ARs = [P.sbuf("AR_%d" % i, [64, 8, nch, 128], BF16) for i in range(2)]
    Bts = [P.sbuf("Bt_%d" % i, [64, 8, W], BF16) for i in range(2)]
    Kts = [P.sbuf("Kt_%d" % i, [64, 8, W], BF16) for i in range(2)]
    Bhs = [P.sbuf("Bh_%d" % i, [64, 8, W], BF16) for i in range(2)]
    Khs = [P.sbuf("Kh_%d" % i, [64, 8, W], BF16) for i in range(2)]
    Yw = P.sbuf("Yw", [64, 8, W])
    CH = {n: P.sbuf(n, [64, 8, 64], BF16) for n in
          ("AtT", "BhT", "KhT", "VT", "X0T", "XA", "XB", "XTA", "XTB", "T", "AhT", "Xs", "VhT", "Gs", "Qs")}
    CH["dW"] = P.sbuf("dW", [64, 8, 64])
    MB = P.sbuf("MB", [64, 8, 128], BF16)
    MK = P.sbuf("MK", [64, 8, 128], BF16)
    banks, bbanks, bki = PB

    def bank():
        bki[0] += 1
        return banks[bki[0] % len(banks)]

    def bbank():
        bki[1] += 1
        return bbanks[bki[1] % len(bbanks)]

    ew = EW()
    S3 = [64, 24, W]
    S8 = [64, 8, W]

    def shift3():
        for q in range(3):
            qs = slice(8 * q, 8 * q + 8)
            ctr, prv, nxt = (slice(None), qs, slice(1, W + 1)), (slice(None), qs, slice(0, W)), (slice(None), qs, slice(2, W + 2))
            mup = prm("mup3")[:, qs, None]
            mun = prm("mun3")[:, qs, None]
            e1 = "dve" if q != 1 else "pool"
            tmp = t8a if q != 1 else t8b
            tt(P, e1, tmp[:], Z3[prv], Z3[ctr], ALU.subtract, [Z3], [tmp])
            tt(P, e1, tmp[:], tmp[:], bc(mup, S8), ALU.mult, [tmp, pp], [tmp])
            tt(P, e1, zs3[:, qs, :], tmp[:], Z3[ctr], ALU.add, [tmp, Z3], [zs3])
            tt(P, e1, tmp[:], Z3[nxt], Z3[ctr], ALU.subtract, [Z3], [tmp])
            tt(P, e1, tmp[:], tmp[:], bc(mun, S8), ALU.mult, [tmp, pp], [tmp])
            tt(P, e1, zs3[:, qs, :], zs3[:, qs, :], tmp[:], ALU.add, [tmp, zs3], [zs3])

    zr = dr["zrT"]
    state = {"feat": -1, "chunk": -1}

    def feat():
        for wn, wi in enumerate(order):
            while state["chunk"] < wn - 2:
                yield
            hs = wn % 2
            AR, Bt, Kt, Bh, Kh, E1, vH = ARs[hs], Bts[hs], Kts[hs], Bhs[hs], Khs[hs], E1s[hs], vHs[hs]
            t0 = wi * W
            is_ctx = t0 < CTX
            lo_d, hi_d = (0, CTX) if is_ctx else (CTX, TT)
            lo = max(t0 - 1, lo_d)
            hi = min(t0 + W + 1, hi_d)
            a = lo - (t0 - 1)
            b = a + (hi - lo)
            for Z in (Z3, ZW, ZA, ZG):
                nd = len(Z.t.shape)
                if a > 0:
                    ix = (slice(None),) * (nd - 1) + (slice(0, 1),)
                    P.op("pool", lambda e, Z=Z, ix=ix: e.memset(Z[ix], 0.0), writes=[Z])
                if b < W + 2:
                    ix = (slice(None),) * (nd - 1) + (slice(W + 1, W + 2),)
                    P.op("pool", lambda e, Z=Z, ix=ix: e.memset(Z[ix], 0.0), writes=[Z])
            P.dma("sp", Z3[:, :, a:b], zr[0:1536, lo:hi].rearrange("(q j) t -> j q t", j=64), reads=[zr], writes=[Z3])
            P.dma("sp", ZW[:, :, a:b], zr[1536:1664, lo:hi].rearrange("(q j) t -> j q t", j=64), reads=[zr], writes=[ZW])
            P.dma("sp", ZA[:, :, a:b], zr[1664:1792, lo:hi].rearrange("(q j) t -> j q t", j=64), reads=[zr], writes=[ZA])
            P.dma("sp", ZG[:, a:b], zr[1792:1920, lo:hi], reads=[zr], writes=[ZG])
            yield
            shift3()
            yield
            for (zraw, zs, mupn, munn) in ((ZW, wls, "mup_wl", "mun_wl"), (ZA, als, "mup_al", "mun_al")):
                tv = t8a[:, 0:2, :]
                shp = [64, 2, W]
                c_, p_, n_ = (slice(None), slice(None), slice(1, W + 1)), (slice(None), slice(None), slice(0, W)), (slice(None), slice(None), slice(2, W + 2))
                tt(P, "dve", tv, zraw[p_], zraw[c_], ALU.subtract, [zraw], [t8a])
                tt(P, "dve", tv, tv, bc(prm(mupn)[:, :, None], shp), ALU.mult, [t8a, pp], [t8a])
                tt(P, "dve", zs[:], tv, zraw[c_], ALU.add, [t8a, zraw], [zs])
                tt(P, "dve", tv, zraw[n_], zraw[c_], ALU.subtract, [zraw], [t8a])
                tt(P, "dve", tv, tv, bc(prm(munn)[:, :, None], shp), ALU.mult, [t8a, pp], [t8a])
                tt(P, "dve", zs[:], zs[:], tv, ALU.add, [t8a, zs], [zs])
            mugl = R["mugl"]
            c2, p2, n2 = (slice(None), slice(1, W + 1)), (slice(None), slice(0, W)), (slice(None), slice(2, W + 2))
            tt(P, "dve", tmps[:], ZG[p2], ZG[c2], ALU.subtract, [ZG], [tmps])
            P.op("dve", lambda e: e.scalar_tensor_tensor(out=gls[:], in0=tmps[:], scalar=mugl[:, 0:1], in1=ZG[c2],
                                                         op0=ALU.mult, op1=ALU.add), reads=[tmps, mugl, ZG], writes=[gls])
            tt(P, "dve", tmps[:], ZG[n2], ZG[c2], ALU.subtract, [ZG], [tmps])
            P.op("dve", lambda e: e.scalar_tensor_tensor(out=gls[:], in0=tmps[:], scalar=mugl[:, 1:2], in1=gls[:],
                                                         op0=ALU.mult, op1=ALU.add), reads=[tmps, mugl, gls], writes=[gls])
            yield
            r_ = zs3[:, 0:8, :]
            k_ = zs3[:, 8:16, :]
            v_ = zs3[:, 16:24, :]
            for d2 in ((0, 1) if (dirn == 0 and not is_ctx) else (dirn,)):
                a2 = R["a2_f" if d2 == 0 else "a2_b"]
                pb = [bank(), bank()]
                for h in range(8):
                    q = pb[h // 4]
                    mm(P, q[0:64, (h % 4) * W:(h % 4 + 1) * W], a2[:, h * 64:(h + 1) * 64], als[:, d2, :], True, True,
                       [a2, als], [q])
                a0 = prm("a0_f" if d2 == 0 else "a0_b")
                for g in range(2):
                    tt(P, "dve", icl[d2][:, 4 * g:4 * g + 4, :], pb[g][0:64, 0:4 * W].rearrange("p (h t) -> p h t", h=4),
                       bc(a0[:, 4 * g:4 * g + 4, None], [64, 4, W]), ALU.add, [pb[g], pp], [icl[d2]])
                act(P, icl[d2][:], icl[d2][:], AF.Sigmoid, [icl[d2]], [icl[d2]])
            yield
            th = tmps[0:64, :]
            act(P, th, wls[:, dirn, :], AF.Tanh, [wls], [tmps])
            w2 = R["w2_f" if dirn == 0 else "w2_b"]
            pb = [bank(), bank()]
            for h in range(8):
                q = pb[h // 4]
                mm(P, q[0:64, (h % 4) * W:(h % 4 + 1) * W], w2[:, h * 64:(h + 1) * 64], th, True, True, [w2, tmps], [q])
            w0 = prm("w0_f" if dirn == 0 else "w0_b")
            for g in range(2):
                tt(P, "dve", lw[:, 4 * g:4 * g + 4, :], pb[g][0:64, 0:4 * W].rearrange("p (h t) -> p h t", h=4),
                   bc(w0[:, 4 * g:4 * g + 4, None], [64, 4, W]), ALU.add, [pb[g], pp], [lw])
            act(P, lw[:], lw[:], AF.Sigmoid, [lw], [lw])
            P.op("dve", lambda e: e.tensor_scalar(out=lw[:], in0=lw[:], scalar1=-float(np.exp(-0.5)), scalar2=None,
                                                  op0=ALU.mult), reads=[lw], writes=[lw])
            yield
            tt(P, "pool", t8a[:], k_, bc(prm("k_k")[:, :, None], S8), ALU.mult, [zs3, pp], [t8a])
            tt(P, "pool", t8b[:], t8a[:], t8a[:], ALU.mult, [t8a], [t8b])
            pb = [bank(), bank()]
            for g in range(2):
                mm(P, pb[g][0:64, 0:4 * W], ones[0:64, 0:64], t8b[:, 4 * g:4 * g + 4, :], True, True, [ones, t8b], [pb[g]])
            for g in range(2):
                act(P, kkn[:, 4 * g:4 * g + 4, :], pb[g][0:64, 0:4 * W].rearrange("p (h t) -> p h t", h=4), AF.Ln, [pb[g]], [kkn], bias=R["eps18"][:, 0:1])
            act(P, kkn[:], kkn[:], AF.Exp, [kkn], [kkn], scale=-0.5)
            tt(P, "dve", kkn[:], kkn[:], t8a[:], ALU.mult, [kkn, t8a], [kkn])
            yield
            dirs_needed = (0, 1) if (dirn == 0 and not is_ctx) else (dirn,)
            for d2 in dirs_needed:
                e1 = ew()
                tt(P, e1, kd[d2][:], icl[d2][:], bc(prm("k_a")[:, :, None], S8), ALU.mult, [icl[d2], pp], [kd[d2]])
                tt(P, e1, kd[d2][:], kd[d2][:], bc(R["omka"][:, :, None], S8), ALU.add, [kd[d2], R["omka"]], [kd[d2]])
                tt(P, e1, kd[d2][:], kd[d2][:], k_, ALU.mult, [kd[d2], zs3], [kd[d2]])
            tt(P, "pool", bd[:], kkn[:], icl[dirn][:], ALU.mult, [kkn, icl[dirn]], [bd])
            if dirn == 0 and not is_ctx:
                tt(P, "pool", t8a[:], kd[0][:], kd[1][:], ALU.add, [kd[0], kd[1]], [t8a])
                tt(P, "pool", t8a[:], t8a[:], r_, ALU.mult, [t8a, zs3], [t8a])
                tt(P, "pool", t8a[:], t8a[:], bc(prm("r_k")[:, :, None], S8), ALU.mult, [t8a, pp], [t8a])
                pb = [bank(), bank()]
                for g in range(2):
                    mm(P, pb[g][0:64, 0:4 * W], ones[0:64, 0:64], t8a[:, 4 * g:4 * g + 4, :], True, True, [ones, t8a], [pb[g]])
                for g in range(2):
                    tt(P, "dve", t8b[:, 4 * g:4 * g + 4, :], pb[g][0:64, 0:4 * W].rearrange("p (h t) -> p h t", h=4),
                       v_[:, 4 * g:4 * g + 4, :], ALU.mult, [pb[g], zs3], [t8b])
                P.dma("sp", dr["bonusT"][:, t0 - CTX:t0 - CTX + W].rearrange("(h i) t -> i h t", i=64), t8b[:],
                      reads=[t8b], writes=[dr["bonusT"]])
                sg = tmps
                act(P, sg[:], gls[:], AF.Sigmoid, [gls], [tmps])
                pb = [bank(), bank()]
                g2 = R["g2"]
                for h in range(8):
                    q = pb[h // 4]
                    mm(P, q[0:64, (h % 4) * W:(h % 4 + 1) * W], g2[:, h * 64:(h + 1) * 64], sg[:], True, True, [g2, tmps], [q])
                for g in range(2):
                    act(P, t8a[:, 4 * g:4 * g + 4, :], pb[g][0:64, 0:4 * W].rearrange("p (h t) -> p h t", h=4), AF.Copy, [pb[g]], [t8a])
                P.dma("sp", dr["gateT"][:, t0 - CTX:t0 - CTX + W].rearrange("(h i) t -> i h t", i=64), t8a[:],
                      reads=[t8a], writes=[dr["gateT"]])
            yield
            for h in range(8):
                for c in range(nch):
                    if dirn == 0:
                        sl = slice(c * C, (c + 1) * C)
                    else:
                        sl = slice(c * C + C - 1, (c * C - 1) if c > 0 else None, -1)
                    P.op("dve", lambda e, h=h, sl=sl: e.tensor_tensor_scan(
                        out=cw[:, h, sl], data0=ones[0:64, 0:64], data1=lw[:, h, sl], initial=0.0,
                        op0=ALU.mult, op1=ALU.add), reads=[lw, ones], writes=[cw])
            yield
            act(P, E1[:], cw[:], AF.Exp, [cw], [E1])
            act(P, Einv[:], cw[:], AF.Exp, [cw], [Einv], scale=-1.0)
            tt(P, "pool", t8a[:], cw[:], lw[:], ALU.subtract, [cw, lw], [t8a])
            act(P, t8a[:], t8a[:], AF.Exp, [t8a], [t8a])
            cend = C - 1 if dirn == 0 else 0
            E1v = E1[:].rearrange("p h (c t) -> p h c t", t=C)
            Wc = E1v[:, :, :, cend:cend + 1]
            tt(P, "pool", Ehat[:].rearrange("p h (c t) -> p h c t", t=C), Einv[:].rearrange("p h (c t) -> p h c t", t=C),
               bc(Wc, [64, 8, nch, C]), ALU.mult, [Einv, E1], [Ehat])
            P.op("dve", lambda e, AR=AR: e.scalar_tensor_tensor(
                out=AR[:, :, :, 0:64], in0=kkn[:].rearrange("p h (c t) -> p h c t", t=C), scalar=-1.0,
                in1=t8a[:].rearrange("p h (c t) -> p h c t", t=C), op0=ALU.mult, op1=ALU.mult),
                reads=[kkn, t8a], writes=[AR])
            tt(P, "pool", AR[:, :, :, 64:128], r_.rearrange("p h (c t) -> p h c t", t=C), E1v, ALU.mult, [zs3, E1], [AR])
            tt(P, "dve", Bt[:], bd[:], Einv[:], ALU.mult, [bd, Einv], [Bt])
            tt(P, "pool", Kt[:], kd[dirn][:], Einv[:], ALU.mult, [kd[dirn], Einv], [Kt])
            tt(P, "dve", Bh[:], bd[:], Ehat[:], ALU.mult, [bd, Ehat], [Bh])
            tt(P, "pool", Kh[:], kd[dirn][:], Ehat[:], ALU.mult, [kd[dirn], Ehat], [Kh])

            act(P, vH[:], zs3[:, 16:24, :], AF.Copy, [zs3], [vH])
            state["feat"] = wn
            yield

    def chunk():
        for wn, wi in enumerate(order):
            while state["feat"] < wn:
                yield
            hs = wn % 2
            AR, Bt, Kt, Bh, Kh, E1, vH = ARs[hs], Bts[hs], Kts[hs], Bhs[hs], Khs[hs], E1s[hs], vHs[hs]
            t0 = wi * W
            is_ctx = t0 < CTX
            cend = C - 1 if dirn == 0 else 0
            corder = range(nch) if dirn == 0 else range(nch - 1, -1, -1)
            for c in corder:
                sl = slice(c * C, (c + 1) * C)
                for (srcb, srcf, dst) in ((AR, lambda h: AR[:, h, c, 0:64], "AtT"), (Bh, lambda h: Bh[:, h, sl], "BhT"),
                                          (Kh, lambda h: Kh[:, h, sl], "KhT")):
                    q = bbank()
                    for h in range(8):
                        tr(P, q[0:64, h * 64:(h + 1) * 64], srcf(h), identb[:], [srcb, identb], [q])
                    act(P, CH[dst][:], q[0:64, 0:512].rearrange("p (h t) -> p h t", h=8), AF.Copy, [q], [CH[dst]])
                q = bank()
                for h in range(8):
                    tr(P, q[0:64, h * 64:(h + 1) * 64], vH[:, h, sl], idn, [vH, ident], [q])
                act(P, CH["VT"][:], q[0:64, :].rearrange("p (h t) -> p h t", h=8), AF.Copy, [q], [CH["VT"]])
                yield
                for (L, dstM) in ((Bt, MB), (Kt, MK)):
                    pb = [bank(), bank()]
                    for h in range(8):
                        q = pb[h // 4]
                        mm(P, q[0:64, (h % 4) * 128:(h % 4 + 1) * 128], L[:, h, sl], AR[:, h, c, :], True, True, [L, AR], [q])
                    for g in range(2):
                        tt(P, "dve", dstM[:, 4 * g:4 * g + 4, :], pb[g][0:64, :].rearrange("p (h t) -> p h t", h=4),
                           bc(mAR[:, None, :], [64, 4, 128]), ALU.mult, [pb[g], mAR], [dstM])
                q = bank()
                for h in range(8):
                    mm(P, q[0:64, h * 64:(h + 1) * 64], AR[:, h, c, 0:64], Bt[:, h, sl], True, True, [AR, Bt], [q])
                X0T = CH["X0T"]
                tt(P, "dve", X0T[:], q[0:64, :].rearrange("p (h t) -> p h t", h=8), bc(mNT[:, None, :], [64, 8, 64]),
                   ALU.mult, [q, mNT], [X0T])
                yield
                q = bank()
                for h in range(8):
                    mm(P, q[0:64, h * 64:(h + 1) * 64], MK[:, h, 0:64], CH["VT"][:, h, :], True, True, [MK, CH["VT"]], [q])
                act(P, CH["Xs"][:], q[0:64, :].rearrange("p (h t) -> p h t", h=8), AF.Copy, [q], [CH["Xs"]])
                yield
                T = CH["T"]
                tt(P, "dve", T[:], MB[:, :, 0:64], bc(idn[:, None, :], [64, 8, 64]), ALU.add, [MB, ident], [T])
                Xc_b, XTc_b = MB, X0T
                Xc = lambda h: MB[:, h, 0:64]
                XTc = lambda h: X0T[:, h, :]
                for k in range(1, 6):
                    Xn_b = CH["XA"] if k % 2 else CH["XB"]
                    XTn_b = CH["XTA"] if k % 2 else CH["XTB"]
                    q1 = bank()
                    for h in range(8):
                        mm(P, q1[0:64, h * 64:(h + 1) * 64], Xc(h), XTc(h), True, True, [Xc_b, XTc_b], [q1])
                    act(P, XTn_b[:], q1[0:64, :].rearrange("p (h t) -> p h t", h=8), AF.Copy, [q1], [XTn_b])
                    if k < 5:
                        q2 = bank()
                        for h in range(8):
                            mm(P, q2[0:64, h * 64:(h + 1) * 64], XTc(h), Xc(h), True, True, [Xc_b, XTc_b], [q2])
                        act(P, Xn_b[:], q2[0:64, :].rearrange("p (h t) -> p h t", h=8), AF.Copy, [q2], [Xn_b])
                    yield
                    q3 = bank()
                    for h in range(8):
                        mm(P, q3[0:64, h * 64:(h + 1) * 64], XTn_b[:, h, :], T[:, h, :], True, True, [XTn_b, T], [q3])
                    tt(P, "dve", T[:], T[:], q3[0:64, :].rearrange("p (h t) -> p h t", h=8), ALU.add, [T, q3], [T])
                    yield
                    Xc_b, XTc_b = Xn_b, XTn_b
                    Xc = lambda h, b_=Xn_b: b_[:, h, :]
                    XTc = lambda h, b_=XTn_b: b_[:, h, :]
                yield
                q = bank()
                for h in range(8):
                    mm(P, q[0:64, h * 64:(h + 1) * 64], T[:, h, :], CH["AtT"][:, h, :], True, True, [T, CH["AtT"]], [q])
                act(P, CH["AhT"][:], q[0:64, :].rearrange("p (h t) -> p h t", h=8), AF.Copy, [q], [CH["AhT"]])
                q = bank()
                for h in range(8):
                    mm(P, q[0:64, h * 64:(h + 1) * 64], T[:, h, :], CH["Xs"][:, h, :], True, True, [T, CH["Xs"]], [q])
                act(P, CH["VhT"][:], q[0:64, :].rearrange("p (h t) -> p h t", h=8), AF.Copy, [q], [CH["VhT"]])
                yield
                tt(P, "pool", CH["dW"][:], bc(idn[:, None, :], [64, 8, 64]),
                   bc(E1[:, :, c * C + cend:c * C + cend + 1], [64, 8, 64]), ALU.mult, [ident, E1], [CH["dW"]])
                q = bank()
                for h in range(8):
                    mm(P, q[0:64, h * 64:(h + 1) * 64], CH["AhT"][:, h, :], CH["BhT"][:, h, :], True, True,
                       [CH["AhT"], CH["BhT"]], [q])
                tt(P, "dve", CH["Gs"][:], q[0:64, :].rearrange("p (h t) -> p h t", h=8), CH["dW"][:], ALU.add,
                   [q, CH["dW"]], [CH["Gs"]])
                if not is_ctx:
                    q = bank()
                    for h in range(8):
                        mm(P, q[0:64, h * 64:(h + 1) * 64], CH["AhT"][:, h, :], MB[:, h, 64:128], True, True,
                           [CH["AhT"], MB], [q])
                    tt(P, "dve", CH["Qs"][:], q[0:64, :].rearrange("p (h t) -> p h t", h=8), AR[:, :, c, 64:128], ALU.add,
                       [q, AR], [CH["Qs"]])
                    q = bank()
                    for h in range(8):
                        o_ = q[0:64, h * 64:(h + 1) * 64]
                        mm(P, o_, ST[:, h, :], CH["Qs"][:, h, :], True, False, [ST, CH["Qs"]], [q])
                        mm(P, o_, CH["VhT"][:, h, :], MB[:, h, 64:128], False, False, [CH["VhT"], MB], [q])
                        mm(P, o_, CH["VT"][:, h, :], MK[:, h, 64:128], False, True, [CH["VT"], MK], [q])
                    act(P, Yw[:, :, sl], q[0:64, :].rearrange("p (h t) -> p h t", h=8), AF.Copy, [q], [Yw])
                yield
                q = bank()
                for h in range(8):
                    o_ = q[0:64, h * 64:(h + 1) * 64]
                    mm(P, o_, CH["Gs"][:, h, :], ST[:, h, :], True, False, [CH["Gs"], ST], [q])
                    mm(P, o_, CH["BhT"][:, h, :], CH["VhT"][:, h, :], False, False, [CH["BhT"], CH["VhT"]], [q])
                    mm(P, o_, CH["KhT"][:, h, :], CH["VT"][:, h, :], False, True, [CH["KhT"], CH["VT"]], [q])
                act(P, ST[:], q[0:64, :].rearrange("p (h t) -> p h t", h=8), AF.Copy, [q], [ST])
            if not is_ctx:
                yT = dr["yT" + dsuf]
                P.dma("sp", yT[:, t0 - CTX:t0 - CTX + W].rearrange("(h i) t -> i h t", i=64), Yw[:], reads=[Yw], writes=[yT])
            if dbg is not None and wi == (nctx - 1 if dirn == 0 else 0) and "ST" + dsuf in dbg:
                P.dma("sp", dbg["ST" + dsuf][:].rearrange("(h j) i -> j h i", j=64), ST[:], reads=[ST], writes=[dbg["ST" + dsuf]])


            state["chunk"] = wn
            yield

    return feat(), chunk()


def phase_adaln(P, cs, dr):
    out = {}
    for n in ("Ax", "Bx", "Ac", "Bc", "Af", "Bf"):
        out[n] = P.sbuf(n, [128, 8])
    out["G2"] = P.sbuf("G2", [128, 1024])
    out["G5"] = P.sbuf("G5", [128, 1024])
    out["Arow"] = P.sbuf("Arow", [128, 1024])
    out["Brow"] = P.sbuf("Brow", [128, 1024])
    m0 = P.mark()
    ones = cs["ones"]
    zt = P.sbuf("zt", [128, 2 * 1024], BF16)
    P.op("pool", lambda e: e.memset(zt[:], 0.0), writes=[zt])
    xgv = dr["xg"].t.rearrange("(n p j) d -> n p (j d)", p=128, j=2)
    for n in range(NB * 128 // 256):
        P.dma("sp" if n % 2 else "act", xgv[n], zt[:], reads=[zt], shared=[dr["xg"]])
    P.fence(dr["xg"])
    S16 = P.sbuf("S16", [128, 16])
    P.dma("sp", S16[:], dr["ccol"][:], writes=[S16])
    act(P, S16[:], S16[:], AF.Silu, [S16], [S16])
    Sbc = P.sbuf("Sbc", [128, 8, 128])
    tt(P, "dve", Sbc[:], S16[:, 0:8, None].broadcast_to([128, 8, 128]), ones[:, None, :].broadcast_to([128, 8, 128]),
       ALU.mult, [S16, ones], [Sbc])
    bada = P.sbuf("bada", [128, 48])
    P.dma("sp", bada[:], dr["bada_col"][:], writes=[bada])
    gcols = P.sbuf("gcols", [128, 16])
    P.dma("sp", gcols[:], dr["gcols"][:], writes=[gcols])
    modT = P.sbuf("modT", [128, 48, 2])
    wst = [P.sbuf("wada0", [128, 8, 1024]), P.sbuf("wada1", [128, 8, 1024])]
    brow = P.sbuf("brow", [128, 1024])
    grow = P.sbuf("grow", [128, 1024])
    pm = P.psum("pm", [128, 512])
    pg = [P.psum("pg0", [128, 512]), P.psum("pg1", [128, 512])]
    wv = dr["w_ada"].t.rearrange("(k p) n -> p k n", p=128)
    for m in range(6):
        wb = wst[m % 2]
        for k in range(8):
            P.dma("sp" if k % 2 == 0 else "act", wb[:, k, :], wv[:, k, m * 1024:(m + 1) * 1024], reads=[dr["w_ada"]], writes=[wb])
        for jj in range(8):
            j = m * 8 + jj
            for k in range(8):
                mm(P, pm[:, 2 * jj:2 * jj + 2], wb[:, k, jj * 128:(jj + 1) * 128], S16[:, k:16:8], k == 0, k == 7, [wb, S16], [pm])
        tt(P, "dve", modT[:, m * 8:(m + 1) * 8, :], pm[:, 0:16].rearrange("p (j c) -> p j c", c=2),
           bada[:, m * 8:(m + 1) * 8, None].broadcast_to([128, 8, 2]), ALU.add, [pm, bada], [modT])
        if m in (2, 3, 4, 5):
            G = out[{2: "G2", 3: "Brow", 4: "Arow", 5: "G5"}[m]]
            gsrc = {2: dr["g_post_mix"], 5: dr["g_post_ffn"], 4: dr["g_pre_ffn"], 3: None}[m]
            P.dma("sp", brow[:], dr["b_ada"][0:1, m * 1024:(m + 1) * 1024].broadcast_to([128, 1024]), reads=[dr["b_ada"]], writes=[brow])
            if gsrc is not None:
                P.dma("sp", grow[:], gsrc[0:1, :].broadcast_to([128, 1024]), reads=[gsrc], writes=[grow])
            for hf in range(2):
                for k in range(8):
                    mm(P, pg[hf][:], Sbc[:, k, :], wb[:, k, hf * 512:(hf + 1) * 512], k == 0, k == 7, [Sbc, wb], [pg[hf]])
                tt(P, "dve", G[:, hf * 512:(hf + 1) * 512], pg[hf][:], brow[:, hf * 512:(hf + 1) * 512], ALU.add, [pg[hf], brow], [G])
            if m == 4:
                P.op("dve", lambda e, G=G: e.scalar_tensor_tensor(out=G[:], in0=G[:], scalar=1.0, in1=grow[:], op0=ALU.add, op1=ALU.mult),
                     reads=[G, grow], writes=[G])
            elif gsrc is not None:
                tt(P, "dve", G[:], G[:], grow[:], ALU.mult, [G, grow], [G])
    for (A, B, gsl, msc, msh, ci) in ((out["Ax"], out["Bx"], slice(0, 8), 1, 0, 0), (out["Ac"], out["Bc"], slice(0, 8), 1, 0, 1),
                                      (out["Af"], out["Bf"], slice(8, 16), 4, 3, 0)):
        P.op("dve", lambda e, A=A, msc=msc, ci=ci, gsl=gsl: e.scalar_tensor_tensor(
            out=A[:], in0=modT[:, msc * 8:(msc + 1) * 8, ci], scalar=1.0, in1=gcols[:, gsl], op0=ALU.add, op1=ALU.mult),
            reads=[modT, gcols], writes=[A])
        P.op("dve", lambda e, B=B, msh=msh, ci=ci: e.tensor_copy(out=B[:], in_=modT[:, msh * 8:(msh + 1) * 8, ci]),
             reads=[modT], writes=[B])
    P.barrier()
    P.release(m0)
    return out


def rms_rstd(P, ss, rstd, n, eps):
    P.op("dve", lambda e: e.tensor_scalar(out=rstd[:], in0=ss[:], scalar1=1.0 / n, scalar2=eps, op0=ALU.mult, op1=ALU.add),
         reads=[ss], writes=[rstd])
    act(P, rstd[:], rstd[:], AF.Sqrt, [rstd], [rstd])
    P.op("dve", lambda e: e.reciprocal(out=rstd[:], in_=rstd[:]), reads=[rstd], writes=[rstd])


NCH_W = 42


def phase_proj(P, cs, mod, dr):
    m0 = P.mark()
    ident = cs["ident"]
    ones = cs["ones"]
    hT = P.sbuf("hT", [128, 8, TT], BF16)
    xt = [P.sbuf("xt0", [128, 1024]), P.sbuf("xt1", [128, 1024])]
    junk = P.sbuf("junk", [128, 1024])
    ss = P.sbuf("ss", [128, 1])
    rstd = P.sbuf("rstd", [128, 1])
    pt = [P.psum("pt%d" % i, [128, 512]) for i in range(4)]
    for ti in range(TT // 128):
        X = xt[ti % 2]
        if ti < 2:
            src, srcb = dr["ctx"][ti * 128:(ti + 1) * 128, :], dr["ctx"]
            A, B = mod["Ac"], mod["Bc"]
        else:
            src, srcb = dr["x"][(ti - 2) * 128:(ti - 1) * 128, :], dr["x"]
            A, B = mod["Ax"], mod["Bx"]
        P.dma("sp", X[:], src, reads=[srcb], writes=[X])
        P.op("act", lambda e, X=X: e.activation(out=junk[:], in_=X[:], func=AF.Square, accum_out=ss[:]), reads=[X], writes=[junk, ss])
        rms_rstd(P, ss, rstd, 1024.0, 1e-6)
        P.op("dve", lambda e, X=X: e.tensor_scalar(out=X[:], in0=X[:], scalar1=rstd[:, 0:1], scalar2=None, op0=ALU.mult),
             reads=[X, rstd], writes=[X])
        for k in range(8):
            q = pt[(ti % 2) * 2 + k // 4]
            tr(P, q[:, (k % 4) * 128:(k % 4 + 1) * 128], X[:, k * 128:(k + 1) * 128], ident[:], [X, ident], [q])
        for k in range(8):
            q = pt[(ti % 2) * 2 + k // 4]
            P.op("dve" if k % 2 else "act", (lambda e, q=q, k=k, A=A, B=B, ti=ti: e.tensor_scalar(
                out=hT[:, k, ti * 128:(ti + 1) * 128], in0=q[:, (k % 4) * 128:(k % 4 + 1) * 128], scalar1=A[:, k:k + 1],
                scalar2=B[:, k:k + 1], op0=ALU.mult, op1=ALU.add)) if k % 2 else (lambda e, q=q, k=k, A=A, B=B, ti=ti: e.activation(
                    out=hT[:, k, ti * 128:(ti + 1) * 128], in_=q[:, (k % 4) * 128:(k % 4 + 1) * 128], func=AF.Identity,
                    scale=A[:, k:k + 1], bias=B[:, k:k + 1])), reads=[q, A, B], writes=[hT])
    bcol = P.sbuf("bcol", [128, NCH_W])
    P.dma("sp", bcol[:], dr["bin_col"][:], writes=[bcol])
    bv = P.sbuf("bv", [1, 128])
    P.dma("sp", bv[:], dr["bv_row"][:], writes=[bv])
    wst = [P.sbuf("wst%d" % i, [128, 8, 128]) for i in range(3)]
    wbf = [P.sbuf("wbf%d" % i, [128, 8, 128], BF16) for i in range(3)]
    cosb = P.sbuf("cosb", [128, 512])
    sinb = P.sbuf("sinb", [128, 512])
    ot = [P.sbuf("ot%d" % i, [128, 512]) for i in range(2)]
    otb = [P.sbuf("otb%d" % i, [128, 512], BF16) for i in range(2)]
    t1 = P.sbuf("rp1", [128, 512])
    pp = [P.psum("pp%d" % i, [128, 512]) for i in range(4)]
    wv = dr["w_in"].t.rearrange("(k p) n -> p k n", p=128)
    groups = [(0, 256)] + [(256 + 512 * g, 512) for g in range(8)]
    wi = [0]
    oi = [0]

    def load_w(j):
        s = wi[0] % 3
        wi[0] += 1
        P.dma("act", wst[s][:], wv[:, :, j * 128:(j + 1) * 128], reads=[dr["w_in"]], writes=[wst[s]])
        P.op("pool", lambda e: e.tensor_copy(out=wbf[s][:], in_=wst[s][:]), reads=[wst[s]], writes=[wbf[s]])
        return wbf[s]

    def proj(q, wb, t0, n):
        for k in range(8):
            mm(P, q[:, 0:n], wb[:, k, :], hT[:, k, t0:t0 + n], k == 0, k == 7, [wb, hT], [q])

    for j in list(range(0, 5)) + list(range(6, 37)):
        wb = load_w(j)
        wr = load_w(37 + j) if j < 5 else None
        for gi, (t0, n) in enumerate(groups):
            if gi == 0 and not (j == 4 or 6 <= j <= 20):
                continue
            q = pp[oi[0] % 2]
            proj(q, wb, t0, n)
            o = oi[0] % 2
            oi[0] += 1
            if j < 5 and gi > 0:
                q2 = pp[2 + o]
                proj(q2, wr, t0, n)
                xs = t0 - CTX
                P.dma("sp", cosb[:], dr["cos_t"][:, xs:xs + 512], reads=[dr["cos_t"]], writes=[cosb])
                P.dma("sp", sinb[:], dr["sin_t"][:, xs:xs + 512], reads=[dr["sin_t"]], writes=[sinb])
                P.op("dve", lambda e, q=q, j=j: e.scalar_tensor_tensor(out=t1[:], in0=q[:], scalar=bcol[:, j:j + 1], in1=cosb[:],
                                                                      op0=ALU.add, op1=ALU.mult), reads=[q, bcol, cosb], writes=[t1])
                P.op("dve", lambda e, q2=q2, j=j, o=o: e.scalar_tensor_tensor(out=ot[o][:], in0=q2[:], scalar=bcol[:, 37 + j:38 + j], in1=sinb[:],
                                                                             op0=ALU.add, op1=ALU.mult), reads=[q2, bcol, sinb], writes=[ot[o]])
                tt(P, "pool", otb[o][:], ot[o][:], t1[:], ALU.add, [ot[o], t1], [otb[o]])
                if j < 4:
                    P.dma("sp", dr["qT"][j * 128:(j + 1) * 128, xs:xs + 512], otb[o][:], reads=[otb[o]], writes=[dr["qT"]])
                else:
                    P.dma("sp", dr["kT"][:, t0:t0 + 512], otb[o][:], reads=[otb[o]], writes=[dr["kT"]])
            elif j == 4:
                act(P, otb[o][:, 0:n], q[:, 0:n], AF.Identity, [q, bcol], [otb[o]], bias=bcol[:, j:j + 1])
                P.dma("sp", dr["kT"][:, t0:t0 + n], otb[o][:, 0:n], reads=[otb[o]], writes=[dr["kT"]])
            elif j <= 20:
                act(P, ot[o][:, 0:n], q[:, 0:n], AF.Identity, [q, bcol], [ot[o]], bias=bcol[:, j:j + 1])
                P.dma("sp", dr["zrT"][(j - 6) * 128:(j - 5) * 128, t0:t0 + n], ot[o][:, 0:n], reads=[ot[o]], writes=[dr["zrT"]])
            else:
                act(P, otb[o][:], q[:], AF.Sigmoid, [q, bcol], [otb[o]], bias=bcol[:, j:j + 1])
                P.dma("sp", dr["sgT"][(j - 21) * 128:(j - 20) * 128, t0 - CTX:t0 - CTX + 512], otb[o][:], reads=[otb[o]], writes=[dr["sgT"]])
    wb = load_w(5)
    onesb = P.sbuf("onesb", [1, 128], BF16)
    bvb = P.sbuf("bvb", [1, 128], BF16)
    P.op("dve", lambda e: e.tensor_copy(out=onesb[:], in_=ones[0:1, :]), reads=[ones], writes=[onesb])
    P.op("dve", lambda e: e.tensor_copy(out=bvb[:], in_=bv[:]), reads=[bv], writes=[bvb])
    vt = [P.sbuf("vt%d" % i, [128, 128], BF16) for i in range(2)]
    for ti in range(TT // 128):
        q = pp[ti % 4]
        for k in range(8):
            mm(P, q[:, 0:128], hT[:, k, ti * 128:(ti + 1) * 128], wb[:, k, :], k == 0, False, [wb, hT], [q])
        mm(P, q[:, 0:128], onesb[:], bvb[:], False, True, [onesb, bvb], [q])
        V = vt[ti % 2]
        act(P, V[:], q[:, 0:128], AF.Copy, [q], [V])
        P.dma("sp", dr["vtok"][ti * 128:(ti + 1) * 128, :], V[:], reads=[V], writes=[dr["vtok"]])
    P.barrier()
    P.release(m0)


def phase_attn(P, cs, dr):
    m0 = P.mark()
    ones = cs["ones"]
    qT = P.sbuf("qTs", [64, 8, SEQ], BF16)
    kT = P.sbuf("kTs", [64, 2, TT], BF16)
    vt = P.sbuf("vts", [128, TT // 128, 128], BF16)
    for h in range(8):
        P.dma("sp" if h % 2 else "act", qT[:, h, :], dr["qT"][h * 64:(h + 1) * 64, :], reads=[dr["qT"]], writes=[qT])
    for g in range(2):
        P.dma("sp", kT[:, g, :], dr["kT"][g * 64:(g + 1) * 64, :], reads=[dr["kT"]], writes=[kT])
    P.dma("sp", vt[:], dr["vtok"].t.rearrange("(n p) c -> p n c", p=128), reads=[dr["vtok"]], writes=[vt])
    esr = P.sbuf("esr", [1, 1024])
    esb = P.sbuf("esb", [1, 1024], BF16)
    P.dma("sp", esr[:], dr["sink_row"][:], writes=[esr])
    act(P, esb[:], esr[:], AF.Exp, [esr], [esb])
    onesb = P.sbuf("onesb2", [128, 64], BF16)
    P.op("dve", lambda e: e.tensor_copy(out=onesb[:], in_=ones[:, 0:64]), reads=[ones], writes=[onesb])
    mL = P.sbuf("mL", [128, 128], BF16)
    mR = P.sbuf("mR", [128, 128], BF16)
    P.op("dve", lambda e: e.tensor_copy(out=mL[:], in_=cs["Uincl"][:]), reads=[cs["Uincl"]], writes=[mL])
    P.op("dve", lambda e: e.tensor_copy(out=mR[:], in_=cs["Lincl"][:]), reads=[cs["Lincl"]], writes=[mR])
    ps = [P.psum("ps%d" % i, [128, 512]) for i in range(4)]
    po = [P.psum("po%d" % i, [128, 512]) for i in range(2)]
    pd = [P.psum("pd%d" % i, [128, 512]) for i in range(2)]
    pT = [P.sbuf("pT%d" % i, [128, 512], BF16) for i in range(6)]
    rden = P.sbuf("rden", [64, 512])
    ao = [P.sbuf("ao%d" % i, [64, 512], BF16) for i in range(2)]
    si = 0
    for n in range(SEQ // 128):
        blocks = []
        if n > 0:
            blocks.append((2 + n - 1, mL))
        blocks.append((2 + n, None))
        if n < SEQ // 128 - 1:
            blocks.append((2 + n + 1, mR))
        blocks += [(0, None), (1, None)]
        for g in range(2):
            o = po[g]
            d = pd[g]
            rhs_q = qT[:, 4 * g:4 * g + 4, n * 128:(n + 1) * 128]
            for bi, (kb, msk) in enumerate(blocks):
                s = ps[si % 4]
                p = pT[si % 6]
                si += 1
                mm(P, s[:].rearrange("p (h t) -> p h t", h=4), kT[:, g, kb * 128:(kb + 1) * 128], rhs_q, True, True, [kT, qT], [s])
                act(P, p[:], s[:], AF.Exp, [s], [p], scale=0.125)
                if msk is not None:
                    tt(P, "pool", p[:].rearrange("p (h t) -> p h t", h=4), p[:].rearrange("p (h t) -> p h t", h=4),
                       msk[:, None, :].broadcast_to([128, 4, 128]), ALU.mult, [p, msk], [p])
                mm(P, o[0:64, :], vt[:, kb, g * 64:(g + 1) * 64], p[:], bi == 0, bi == len(blocks) - 1, [vt, p], [o])
                mm(P, d[0:64, :], onesb[:], p[:], bi == 0, False, [onesb, p], [d])
            mm(P, d[0:64, :], onesb[0:1, :], esb[0:1, g * 512:(g + 1) * 512], False, True, [onesb, esb], [d])
            P.op("dve", lambda e, d=d: e.reciprocal(out=rden[:], in_=d[0:64, :]), reads=[d], writes=[rden])
            A = ao[g]
            tt(P, "dve", A[:], o[0:64, :], rden[:], ALU.mult, [o, rden], [A])
            P.dma("sp", dr["attT"][g * 256:(g + 1) * 256, n * 128:(n + 1) * 128].rearrange("(h d) t -> d h t", d=64),
                  A[:].rearrange("p (h t) -> p h t", h=4), reads=[A], writes=[dr["attT"]])
    P.barrier()
    P.release(m0)


def phase_rwkv_out(P, cs, R, dr):
    m0 = P.mark()
    ones = cs["ones"]
    pp = R["pp"]
    N = 512

    def prm(n):
        o, w = P64[n]
        return pp[:, o:o + w]
    yf = P.sbuf("yf", [64, 8, N])
    yb = P.sbuf("yb", [64, 8, N])
    bo = P.sbuf("bo", [64, 8, N])
    ga = P.sbuf("ga", [64, 8, N])
    sq = P.sbuf("sq", [64, 8, N])
    ob = P.sbuf("ob", [64, 8, N], BF16)
    pb = [P.psum("pr%d" % i, [128, 512]) for i in range(8)]
    for g in range(SEQ // N):
        ts = slice(g * N, (g + 1) * N)
        for (buf, nm, q) in ((yf, "yT_f", "sp"), (yb, "yT_b", "act"), (bo, "bonusT", "sp"), (ga, "gateT", "act")):
            P.dma(q, buf[:], dr[nm][:, ts].rearrange("(h i) t -> i h t", i=64), reads=[dr[nm]], writes=[buf])
        tt(P, "pool", yf[:], yf[:], yb[:], ALU.add, [yf, yb], [yf])
        for h in range(8):
            mm(P, pb[h][0:64, :], ones[0:64, 0:64], yf[:, h, :], True, True, [ones, yf], [pb[h]])
        for h in range(8):
            P.op("dve", lambda e, h=h: e.scalar_tensor_tensor(out=yf[:, h, :], in0=pb[h][0:64, :], scalar=-1.0 / 64, in1=yf[:, h, :],
                                                             op0=ALU.mult, op1=ALU.add), reads=[pb[h], yf], writes=[yf])
        tt(P, "pool", sq[:], yf[:], yf[:], ALU.mult, [yf], [sq])
        for h in range(8):
            mm(P, pb[h][0:64, :], ones[0:64, 0:64], sq[:, h, :], True, True, [ones, sq], [pb[h]])
        for h in range(8):
            P.op("dve", lambda e, h=h: e.tensor_scalar(out=sq[:, h, :], in0=pb[h][0:64, :], scalar1=1.0 / 64, scalar2=64e-5,
                                                      op0=ALU.mult, op1=ALU.add), reads=[pb[h]], writes=[sq])
        act(P, sq[:], sq[:], AF.Ln, [sq], [sq])
        act(P, sq[:], sq[:], AF.Exp, [sq], [sq], scale=-0.5)
        tt(P, "dve", yf[:], yf[:], sq[:], ALU.mult, [yf, sq], [yf])
        tt(P, "pool", yf[:], yf[:], prm("ln_w")[:, :, None].broadcast_to([64, 8, N]), ALU.mult, [yf, pp], [yf])
        tt(P, "pool", yf[:], yf[:], prm("ln_b")[:, :, None].broadcast_to([64, 8, N]), ALU.add, [yf, pp], [yf])
        tt(P, "dve", yf[:], yf[:], bo[:], ALU.add, [yf, bo], [yf])
        tt(P, "dve", ob[:], yf[:], ga[:], ALU.mult, [yf, ga], [ob])
        P.dma("sp", dr["rwkT"][:, ts].rearrange("(h i) t -> i h t", i=64), ob[:], reads=[ob], writes=[dr["rwkT"]])
    P.barrier()
    P.release(m0)


def phase_merge(P, cs, mod, dr, rt):
    m0 = P.mark()
    ident = cs["ident"]
    ones = cs["ones"]
    N = 512
    wua = P.sbuf("wua", [64, 8, 1024], BF16)
    wur = P.sbuf("wur", [64, 8, 1024], BF16)
    wo = P.sbuf("wo", [128, 8, 1024], BF16)
    stg = P.sbuf("stg", [128, 8, 1024])
    P.dma("sp", stg[0:64, :, :], dr["w_up_att"].t.rearrange("(h d) n -> d h n", d=64), reads=[dr["w_up_att"]], writes=[stg])
    P.op("pool", lambda e: e.tensor_copy(out=wua[:], in_=stg[0:64, :, :]), reads=[stg], writes=[wua])
    P.dma("sp", stg[0:64, :, :], dr["w_up_rwkv"].t.rearrange("(h d) n -> d h n", d=64), reads=[dr["w_up_rwkv"]], writes=[stg])
    P.op("pool", lambda e: e.tensor_copy(out=wur[:], in_=stg[0:64, :, :]), reads=[stg], writes=[wur])
    P.dma("sp", stg[:], dr["w_out"].t.rearrange("(k p) n -> p k n", p=128), reads=[dr["w_out"]], writes=[stg])
    P.op("pool", lambda e: e.tensor_copy(out=wo[:], in_=stg[:]), reads=[stg], writes=[wo])
    wr = P.sbuf("wr", [128, 8, 32])
    P.dma("sp", wr[:], dr["w_router"].t.rearrange("(k p) n -> p k n", p=128), reads=[dr["w_router"]], writes=[wr])
    br = P.sbuf("br", [1, 32])
    P.dma("sp", br[:], dr["b_router"][:], writes=[br])
    aT = P.sbuf("aT", [64, 8, N], BF16)
    rT = P.sbuf("rT", [64, 8, N], BF16)
    sg = P.sbuf("sg", [128, 16, N], BF16)
    mT = P.sbuf("mT", [128, 8, N], BF16)
    m1 = P.sbuf("m1", [128, N])
    m2 = P.sbuf("m2", [128, N])
    xt = P.sbuf("xtd", [128, 1024])
    x1 = P.sbuf("x1d", [128, 1024])
    junk = P.sbuf("junkd", [128, 1024])
    ssa = P.sbuf("ssa", [128, 2])
    ss = P.sbuf("ssd", [128, 1])
    rstd = P.sbuf("rstdd", [128, 1])
    hf32 = P.sbuf("hf32", [128, 8, 128])
    hfrow = P.sbuf("hfrow", [128, 1024], BF16)
    lg = P.sbuf("lg", [128, 32])
    m8 = P.sbuf("m8", [128, 8])
    nmx = P.sbuf("nmx", [128, 1])
    msk = P.sbuf("msk", [128, 32])
    ex = P.sbuf("ex", [128, 32])
    sm = P.sbuf("sm", [128, 1])
    gts = P.sbuf("gts", [32, 128])
    pa = P.psum("pa", [128, 512]); pr = P.psum("prr", [128, 512])
    pm = [P.psum("pmx0", [128, 512]), P.psum("pmx1", [128, 512])]
    ptr = [P.psum("ptr0", [128, 512]), P.psum("ptr1", [128, 512])]
    pl = P.psum("pl", [128, 512]); pg = P.psum("pgt", [128, 512])
    for g in range(SEQ // N):
        ts = slice(g * N, (g + 1) * N)
        P.dma("sp", aT[:], dr["attT"][:, ts].rearrange("(h d) t -> d h t", d=64), reads=[dr["attT"]], writes=[aT])
        P.dma("act", rT[:], dr["rwkT"][:, ts].rearrange("(h d) t -> d h t", d=64), reads=[dr["rwkT"]], writes=[rT])
        P.dma("sp", sg[:], dr["sgT"][:, ts].rearrange("(c p) t -> p c t", p=128), reads=[dr["sgT"]], writes=[sg])
        for dc in range(8):
            for h in range(8):
                mm(P, pa[:], wua[:, h, dc * 128:(dc + 1) * 128], aT[:, h, :], h == 0, h == 7, [wua, aT], [pa])
            for h in range(8):
                mm(P, pr[:], wur[:, h, dc * 128:(dc + 1) * 128], rT[:, h, :], h == 0, h == 7, [wur, rT], [pr])
            tt(P, "dve", m1[:], pa[:], sg[:, dc, :], ALU.mult, [pa, sg], [m1])
            tt(P, "dve", m2[:], pr[:], sg[:, 8 + dc, :], ALU.mult, [pr, sg], [m2])
            tt(P, "pool", mT[:, dc, :], m1[:], m2[:], ALU.add, [m1, m2], [mT])
        for t4 in range(4):
            tok = g * N + t4 * 128
            P.dma("act", xt[:], dr["x"][tok:tok + 128, :], reads=[dr["x"]], writes=[xt])
            for hf in range(2):
                for k in range(8):
                    mm(P, pm[hf][:], mT[:, k, t4 * 128:(t4 + 1) * 128], wo[:, k, hf * 512:(hf + 1) * 512], k == 0, k == 7, [mT, wo], [pm[hf]])
                P.op("act", lambda e, hf=hf: e.activation(out=junk[:, 0:512], in_=pm[hf][:], func=AF.Square, accum_out=ssa[:, hf:hf + 1]),
                     reads=[pm[hf]], writes=[junk, ssa])
            tt(P, "dve", ss[:], ssa[:, 0:1], ssa[:, 1:2], ALU.add, [ssa], [ss])
            rms_rstd(P, ss, rstd, 1024.0, 1e-6)
            for hf in range(2):
                hs = slice(hf * 512, (hf + 1) * 512)
                P.op("dve", lambda e, hf=hf, hs=hs: e.scalar_tensor_tensor(out=x1[:, hs], in0=pm[hf][:], scalar=rstd[:, 0:1], in1=mod["G2"][:, hs],
                                                                          op0=ALU.mult, op1=ALU.mult), reads=[pm[hf], rstd, mod["G2"]], writes=[x1])
            tt(P, "pool", x1[:], x1[:], xt[:], ALU.add, [x1, xt], [x1])
            P.dma("sp", dr["x1"][tok:tok + 128, :], x1[:], reads=[x1], writes=[dr["x1"]])
            P.op("act", lambda e: e.activation(out=junk[:], in_=x1[:], func=AF.Square, accum_out=ss[:]), reads=[x1], writes=[junk, ss])
            rms_rstd(P, ss, rstd, 1024.0, 1e-6)
            P.op("dve", lambda e: e.tensor_scalar(out=xt[:], in0=x1[:], scalar1=rstd[:, 0:1], scalar2=None, op0=ALU.mult),
                 reads=[x1, rstd], writes=[xt])
            for k in range(8):
                tr(P, ptr[k // 4][:, (k % 4) * 128:(k % 4 + 1) * 128], xt[:, k * 128:(k + 1) * 128], ident[:], [xt, ident], [ptr[k // 4]])
            for k in range(8):
                q = ptr[k // 4]
                P.op("dve", lambda e, q=q, k=k: e.tensor_scalar(out=hf32[:, k, :], in0=q[:, (k % 4) * 128:(k % 4 + 1) * 128],
                                                               scalar1=mod["Af"][:, k:k + 1], scalar2=mod["Bf"][:, k:k + 1],
                                                               op0=ALU.mult, op1=ALU.add), reads=[q, mod["Af"], mod["Bf"]], writes=[hf32])
            tt(P, "pool", junk[:], xt[:], mod["Arow"][:], ALU.mult, [xt, mod["Arow"]], [junk])
            tt(P, "pool", hfrow[:], junk[:], mod["Brow"][:], ALU.add, [junk, mod["Brow"]], [hfrow])
            P.dma("sp", dr["hftok"][tok:tok + 128, :], hfrow[:], reads=[hfrow], writes=[dr["hftok"]])
            for k in range(8):
                mm(P, pl[:, 0:32], hf32[:, k, :], wr[:, k, :], k == 0, False, [hf32, wr], [pl])
            mm(P, pl[:, 0:32], ones[0:1, :], br[:], False, True, [ones, br], [pl])
            P.op("dve", lambda e: e.tensor_copy(out=lg[:], in_=pl[:, 0:32]), reads=[pl], writes=[lg])
            P.op("dve", lambda e: e.max(out=m8[:], in_=lg[:]), reads=[lg], writes=[m8])
            P.op("dve", lambda e: e.tensor_scalar(out=nmx[:], in0=m8[:, 0:1], scalar1=-1.0, scalar2=None, op0=ALU.mult), reads=[m8], writes=[nmx])
            P.op("dve", lambda e: e.tensor_scalar(out=msk[:], in0=lg[:], scalar1=m8[:, 3:4], scalar2=None, op0=ALU.is_ge), reads=[lg, m8], writes=[msk])
            act(P, ex[:], lg[:], AF.Exp, [lg, nmx], [ex], bias=nmx[:, 0:1])
            tt(P, "dve", ex[:], ex[:], msk[:], ALU.mult, [ex, msk], [ex])
            P.op("dve", lambda e: e.reduce_sum(out=sm[:], in_=ex[:], axis=mybir.AxisListType.X), reads=[ex], writes=[sm])
            P.op("dve", lambda e: e.reciprocal(out=sm[:], in_=sm[:]), reads=[sm], writes=[sm])
            P.op("dve", lambda e: e.tensor_scalar(out=ex[:], in0=ex[:], scalar1=sm[:, 0:1], scalar2=None, op0=ALU.mult), reads=[ex, sm], writes=[ex])
            ti_ = g * 4 + t4
            P.op("dve", lambda e, ti_=ti_: e.tensor_copy(out=rt["Gall"][:, ti_, :], in_=ex[:]), reads=[ex], writes=[rt["Gall"]])
            P.op("dve", lambda e, ti_=ti_: e.tensor_copy(out=rt["Mall"][:, ti_, :], in_=msk[:]), reads=[msk], writes=[rt["Mall"]])
    P.barrier()
    P.release(m0)


I32 = mybir.dt.int32
BIGI = 1.0e6


def phase_moe(P, cs, mod, rt, dr):
    m0 = P.mark()
    ones, ident = cs["ones"], cs["ident"]
    Mall, Gall = rt["Mall"], rt["Gall"]
    NT = SEQ // 128
    xg, yb = dr["xg"], dr["yb"]
    IDX = P.sbuf("IDX", [128, NB, 8], I32)
    DEST = P.sbuf("DEST", [128, NT, 4], I32)
    GK = P.sbuf("GK", [128, NT, 4])
    m1 = P.mark()
    it = P.sbuf("it", [128, 192], I32)
    itf = P.sbuf("itf", [128, 192])
    P.op("pool", lambda e: e.iota(it[:], pattern=[[128, 192]], base=0, channel_multiplier=0), writes=[it])
    P.op("dve", lambda e: e.tensor_copy(out=itf[:], in_=it[:]), reads=[it], writes=[itf])
    pi = P.sbuf("pi", [128, 1], I32)
    pif = P.sbuf("pif", [128, 1])
    P.op("pool", lambda e: e.iota(pi[:], pattern=[[0, 1]], base=0, channel_multiplier=1), writes=[pi])
    P.op("dve", lambda e: e.tensor_copy(out=pif[:], in_=pi[:]), reads=[pi], writes=[pif])
    pc = P.psum("pc", [128, 512])
    pq = P.psum("pq", [128, 512])
    for i in range(NT):
        mm(P, pc[:, 0:32], ones[:], Mall[:, i, :], i == 0, i == NT - 1, [ones, Mall], [pc])
    cnt = P.sbuf("cnt", [128, 32])
    P.op("dve", lambda e: e.tensor_copy(out=cnt[:], in_=pc[:, 0:32]), reads=[pc], writes=[cnt])
    cmp = P.sbuf("cmp", [128, 32, 32])
    tt(P, "dve", cmp[:], cnt[:, :, None].broadcast_to([128, 32, 32]), itf[:, None, 0:32].broadcast_to([128, 32, 32]),
       ALU.is_gt, [cnt, itf], [cmp])
    padded = P.sbuf("padded", [128, 32])
    P.op("dve", lambda e: e.reduce_sum(out=padded[:], in_=cmp[:], axis=mybir.AxisListType.X), reads=[cmp], writes=[padded])
    P.op("dve", lambda e: e.tensor_scalar(out=padded[:], in0=padded[:], scalar1=128.0, scalar2=None, op0=ALU.mult), reads=[padded], writes=[padded])
    p_end = P.sbuf("p_end", [128, 32])
    P.op("dve", lambda e: e.tensor_tensor_scan(out=p_end[:], data0=ones[:, 0:32], data1=padded[:], initial=0.0, op0=ALU.mult, op1=ALU.add),
         reads=[ones, padded], writes=[p_end])
    base0 = P.sbuf("base0", [128, 32])
    tt(P, "dve", base0[:], p_end[:], padded[:], ALU.subtract, [p_end, padded], [base0])
    ebc = P.sbuf("ebc", [128, NB, 32])
    tt(P, "dve", ebc[:], p_end[:, None, :].broadcast_to([128, NB, 32]), itf[:, 0:NB, None].broadcast_to([128, NB, 32]),
       ALU.is_le, [p_end, itf], [ebc])
    eb = P.sbuf("eb", [128, NB])
    P.op("dve", lambda e: e.reduce_sum(out=eb[:], in_=ebc[:], axis=mybir.AxisListType.X), reads=[ebc], writes=[eb])
    P.op("dve", lambda e: e.tensor_scalar(out=eb[:], in0=eb[:], scalar1=31.0, scalar2=None, op0=ALU.min), reads=[eb], writes=[eb])
    sk = P.sbuf("sk", [128, NB])
    P.op("pool", lambda e: e.memset(sk[:], 0.0), writes=[sk])
    tt(P, "dve", sk[:, 1:NB], eb[:, 1:NB], eb[:, 0:NB - 1], ALU.is_equal, [eb], [sk])
    P.op("dve", lambda e: e.tensor_scalar(out=sk[:], in0=sk[:], scalar1=BIGI, scalar2=None, op0=ALU.mult), reads=[sk], writes=[sk])
    basef = P.sbuf("basef", [128, NB])
    P.op("dve", lambda e: e.tensor_scalar(out=basef[:], in0=eb[:], scalar1=128.0, scalar2=pif[:, 0:1], op0=ALU.mult, op1=ALU.add),
         reads=[eb, pif], writes=[basef])
    idxf = P.sbuf("idxf", [128, NB, 8])
    for pc_ in range(6):
        mul, add = (4.0, float(pc_)) if pc_ < 4 else (2.0, float(pc_ - 4))
        P.op("dve", lambda e, pc_=pc_, mul=mul, add=add: e.tensor_scalar(out=idxf[:, :, pc_], in0=basef[:], scalar1=mul, scalar2=add,
                                                                         op0=ALU.mult, op1=ALU.add), reads=[basef], writes=[idxf])
        tt(P, "dve", idxf[:, :, pc_], idxf[:, :, pc_], sk[:], ALU.add, [idxf, sk], [idxf])
    tt(P, "dve", idxf[:, :, 6], eb[:], sk[:], ALU.add, [eb, sk], [idxf])
    P.op("dve", lambda e: e.tensor_copy(out=IDX[:, :, 0:7], in_=idxf[:, :, 0:7]), reads=[idxf], writes=[IDX])
    DESTf = P.sbuf("DESTf", [128, NT, 4])
    Dt = P.sbuf("Dt", [128, 32])
    Vt = P.sbuf("Vt", [128, 32])
    oh = P.sbuf("oh", [128, 32])
    m8 = P.sbuf("m8s", [128, 8])
    for i in range(NT):
        mm(P, pq[:, 0:32], cs["Lstrict"][:], Mall[:, i, :], True, True, [cs["Lstrict"], Mall], [pq])
        tt(P, "dve", Dt[:], pq[:, 0:32], base0[:], ALU.add, [pq, base0], [Dt])
        mm(P, pc[:, 0:32], ones[:], Mall[:, i, :], True, True, [ones, Mall], [pc])
        tt(P, "dve", base0[:], base0[:], pc[:, 0:32], ALU.add, [base0, pc], [base0])
        P.op("dve", lambda e: e.tensor_scalar(out=Vt[:], in0=Dt[:], scalar1=-1.0, scalar2=32768.0, op0=ALU.mult, op1=ALU.add), reads=[Dt], writes=[Vt])
        tt(P, "dve", Vt[:], Vt[:], Mall[:, i, :], ALU.mult, [Vt, Mall], [Vt])
        P.op("dve", lambda e: e.max(out=m8[:], in_=Vt[:]), reads=[Vt], writes=[m8])
        P.op("dve", lambda e, i=i: e.tensor_scalar(out=DESTf[:, i, :], in0=m8[:, 0:4], scalar1=-1.0, scalar2=32768.0, op0=ALU.mult, op1=ALU.add),
             reads=[m8], writes=[DESTf])
        for k in range(4):
            P.op("dve", lambda e, k=k: e.tensor_scalar(out=oh[:], in0=Vt[:], scalar1=m8[:, k:k + 1], scalar2=None, op0=ALU.is_equal),
                 reads=[Vt, m8], writes=[oh])
            tt(P, "dve", oh[:], oh[:], Gall[:, i, :], ALU.mult, [oh, Gall], [oh])
            P.op("dve", lambda e, i=i, k=k: e.reduce_sum(out=GK[:, i, k:k + 1], in_=oh[:], axis=mybir.AxisListType.X), reads=[oh], writes=[GK])
    P.op("dve", lambda e: e.tensor_copy(out=DEST[:], in_=DESTf[:]), reads=[DESTf], writes=[DEST])
    hr = [P.sbuf("hr%d" % i, [128, 1024], BF16) for i in range(2)]
    for i in range(NT):
        H = hr[i % 2]
        P.dma("sp", H[:], dr["hftok"][i * 128:(i + 1) * 128, :], reads=[dr["hftok"]], writes=[H])
        for k in range(4):
            P.dma("pool", xg.t[:, :], H[:], reads=[H, DEST], shared=[xg], ind=(DEST[:, i, k:k + 1], True, NB * 128 - 1))
    P.barrier()
    P.release(m1)
    m2 = P.mark()
    identb = P.sbuf("identb", [128, 128], BF16)
    P.op("dve", lambda e: e.tensor_copy(out=identb[:], in_=ident[:]), reads=[ident], writes=[identb])
    wgu_p = [P.sbuf("wgu%d" % i, [128, 8, 512], BF16) for i in range(4)]
    wdn_p = [P.sbuf("wdn%d" % i, [128, 8, 512], BF16) for i in range(2)]
    bgu_b = P.sbuf("bgu_b", [128, 2048])
    bdn_b = P.sbuf("bdn_b", [128, 1024])
    NS = 2
    xs = [P.sbuf("xs%d" % i, [128, 1024], BF16) for i in range(3)]
    xgT = [P.sbuf("xgT%d" % i, [128, 8, 128], BF16) for i in range(NS)]
    hgu = [P.sbuf("hgu%d" % i, [128, 2048]) for i in range(NS)]
    gcb = [P.sbuf("gcb%d" % i, [128, 1024]) for i in range(NS)]
    sgb = [P.sbuf("sgb%d" % i, [128, 1024]) for i in range(NS)]
    u1b = [P.sbuf("u1b%d" % i, [128, 1024]) for i in range(NS)]
    actb = [P.sbuf("actb%d" % i, [128, 1024], BF16) for i in range(NS)]
    actT = [P.sbuf("actT%d" % i, [128, 8, 128], BF16) for i in range(NS)]
    ysb = [P.sbuf("ysb%d" % i, [128, 1024]) for i in range(NS)]
    ptx = P.psum("ptx", [128, 1024], BF16)
    pta = P.psum("pta", [128, 1024], BF16)
    pgu = [P.psum("pgu%d" % i, [128, 512]) for i in range(2)]
    pdn = [P.psum("pdn%d" % i, [128, 512]) for i in range(2)]
    wgu2d, wdn2d = dr["wgu2d"], dr["wdn2d"]
    def stage1(b):
        s_ = b % NS
        for ng in range(4):
            P.dma("pool", wgu_p[ng][:].rearrange("p k n -> p (k n)"), wgu2d.t[:, :], reads=[wgu2d, IDX], writes=[wgu_p[ng]],
                  ind=(IDX[:, b, ng:ng + 1], False, NEXP * 128 * 4 - 1))
        P.dma("pool", bgu_b[:], dr["b_gate_up"].t[:, :], reads=[dr["b_gate_up"], IDX], writes=[bgu_b], ind=(IDX[:, b, 6:7], False, NEXP - 1))
        X = xs[b % 3]
        for k in range(8):
            tr(P, ptx[:, k * 128:(k + 1) * 128], X[:, k * 128:(k + 1) * 128], identb[:], [X, identb], [ptx])
        act(P, xgT[s_][:].rearrange("p k t -> p (k t)"), ptx[:], AF.Copy, [ptx], [xgT[s_]])
        H = hgu[s_]
        for ng in range(4):
            q = pgu[ng % 2]
            for k in range(8):
                mm(P, q[:], xgT[s_][:, k, :], wgu_p[ng][:, k, :], k == 0, k == 7, [xgT[s_], wgu_p[ng]], [q])
            tt(P, "dve", H[:, ng * 512:(ng + 1) * 512], q[:], bgu_b[:, ng * 512:(ng + 1) * 512], ALU.add, [q, bgu_b], [H])

    def stage1b(b):
        s_ = b % NS
        H = hgu[s_]
        P.op("dve", lambda e, H=H, s_=s_: e.tensor_scalar(out=gcb[s_][:], in0=H[:, 0:2048:2], scalar1=7.0, scalar2=None, op0=ALU.min),
             reads=[H], writes=[gcb[s_]])
        act(P, sgb[s_][:], gcb[s_][:], AF.Sigmoid, [gcb[s_]], [sgb[s_]], scale=1.702)
        P.op("dve", lambda e, H=H, s_=s_: e.tensor_scalar(out=u1b[s_][:], in0=H[:, 1:2048:2], scalar1=7.0, scalar2=-7.0, op0=ALU.min, op1=ALU.max),
             reads=[H], writes=[u1b[s_]])
        tt(P, "dve", gcb[s_][:], gcb[s_][:], sgb[s_][:], ALU.mult, [gcb[s_], sgb[s_]], [gcb[s_]])
        P.op("dve", lambda e, s_=s_: e.scalar_tensor_tensor(out=actb[s_][:], in0=u1b[s_][:], scalar=1.0, in1=gcb[s_][:], op0=ALU.add, op1=ALU.mult),
             reads=[u1b[s_], gcb[s_]], writes=[actb[s_]])

    def stage2(b):
        s_ = b % NS
        for hf in range(2):
            P.dma("pool", wdn_p[hf][:].rearrange("p k n -> p (k n)"), wdn2d.t[:, :], reads=[wdn2d, IDX], writes=[wdn_p[hf]],
                  ind=(IDX[:, b, 4 + hf:5 + hf], False, NEXP * 128 * 2 - 1))
        P.dma("pool", bdn_b[:], dr["b_down"].t[:, :], reads=[dr["b_down"], IDX], writes=[bdn_b], ind=(IDX[:, b, 6:7], False, NEXP - 1))
        for k in range(8):
            tr(P, pta[:, k * 128:(k + 1) * 128], actb[s_][:, k * 128:(k + 1) * 128], identb[:], [actb[s_], identb], [pta])
        act(P, actT[s_][:].rearrange("p k t -> p (k t)"), pta[:], AF.Copy, [pta], [actT[s_]])
        Y = ysb[s_]
        for hf in range(2):
            q = pdn[hf]
            for k in range(8):
                mm(P, q[:], actT[s_][:, k, :], wdn_p[hf][:, k, :], k == 0, k == 7, [actT[s_], wdn_p[hf]], [q])
            tt(P, "dve", Y[:, hf * 512:(hf + 1) * 512], q[:], bdn_b[:, hf * 512:(hf + 1) * 512], ALU.add, [q, bdn_b], [Y])
        P.dma("sp", yb.t[b * 128:(b + 1) * 128, :], Y[:], reads=[Y], shared=[yb])

    def loadx(b):
        P.dma("sp", xs[b % 3][:], xg.t[b * 128:(b + 1) * 128, :], reads=[xg], writes=[xs[b % 3]])

    loadx(0)
    loadx(1)
    stage1(0)
    stage1b(0)
    for b in range(NB):
        if b + 2 < NB:
            loadx(b + 2)
        if b + 1 < NB:
            stage1(b + 1)
        stage2(b)
        if b + 1 < NB:
            stage1b(b + 1)
    P.barrier()
    P.release(m2)
    yg = [P.sbuf("yg%d" % i, [128, 1024]) for i in range(4)]
    xt = P.sbuf("xte", [128, 1024])
    ot = P.sbuf("ote", [128, 1024])
    ya = P.sbuf("ya", [128, 1024])
    ss = P.sbuf("sse", [128, 1])
    rstd = P.sbuf("rstde", [128, 1])
    for i in range(NT):
        tok = i * 128
        for k in range(4):
            P.dma("pool", yg[k][:], yb.t[:, :], reads=[yb, DEST], writes=[yg[k]], ind=(DEST[:, i, k:k + 1], False, NB * 128 - 1))
        P.dma("act", xt[:], dr["x1"][tok:tok + 128, :], reads=[dr["x1"]], writes=[xt])
        P.op("dve", lambda e, i=i: e.tensor_scalar(out=ya[:], in0=yg[0][:], scalar1=GK[:, i, 0:1], scalar2=None, op0=ALU.mult),
             reads=[yg[0], GK], writes=[ya])
        for k in range(1, 4):
            P.op("dve", lambda e, i=i, k=k: e.scalar_tensor_tensor(out=ya[:], in0=yg[k][:], scalar=GK[:, i, k:k + 1], in1=ya[:], op0=ALU.mult, op1=ALU.add),
                 reads=[yg[k], GK, ya], writes=[ya])
        P.op("act", lambda e: e.activation(out=ot[:], in_=ya[:], func=AF.Square, accum_out=ss[:]), reads=[ya], writes=[ot, ss])
        rms_rstd(P, ss, rstd, 1024.0, 1e-6)
        P.op("dve", lambda e: e.scalar_tensor_tensor(out=ot[:], in0=ya[:], scalar=rstd[:, 0:1], in1=mod["G5"][:], op0=ALU.mult, op1=ALU.mult),
             reads=[ya, rstd, mod["G5"]], writes=[ot])
        tt(P, "pool", ot[:], ot[:], xt[:], ALU.add, [ot, xt], [ot])
        P.dma("sp", dr["out"][tok:tok + 128, :], ot[:], reads=[ot], writes=[dr["out"]])
    P.barrier()
    P.release(m0)


IN_SPECS = [
    ("x", [SEQ, D], F32), ("ctx", [CTX, D], F32), ("ccol", [128, 16], F32), ("w_ada", [D, 6 * D], F32),
    ("b_ada", [1, 6 * D], F32), ("bada_col", [128, 48], F32), ("gcols", [128, 16], F32),
    ("g_post_mix", [1, D], F32), ("g_post_ffn", [1, D], F32), ("g_pre_ffn", [1, D], F32), ("w_in", [D, NCH_W * 128], F32),
    ("bin_col", [128, NCH_W], F32), ("bv_row", [1, 128], F32), ("cos_t", [128, SEQ], F32), ("sin_t", [128, SEQ], F32),
    ("sink_row", [1, 1024], F32), ("pp64", [64, NP64], F32), ("w2_f", [64, 512], F32), ("w2_b", [64, 512], F32),
    ("a2_f", [64, 512], F32), ("a2_b", [64, 512], F32), ("g2", [128, 512], F32), ("mugl", [128, 2], F32),
    ("w_up_att", [512, D], F32), ("w_up_rwkv", [512, D], F32), ("w_out", [D, D], F32), ("w_router", [D, 32], F32),
    ("b_router", [1, 32], F32), ("wgu2d", [NEXP * 128 * 4, 4096], F32), ("wdn2d", [NEXP * 128 * 2, 4096], F32),
    ("b_down", [NEXP, D], F32), ("b_gate_up", [NEXP, 2 * D], F32),
]
SCRATCH = [
    ("zrT", [1920, TT], F32), ("qT", [512, SEQ], BF16), ("kT", [128, TT], BF16), ("vtok", [TT, 128], BF16),
    ("sgT", [2048, SEQ], BF16), ("attT", [512, SEQ], BF16), ("yT_f", [512, SEQ], F32), ("yT_b", [512, SEQ], F32),
    ("bonusT", [512, SEQ], F32), ("gateT", [512, SEQ], F32), ("rwkT", [512, SEQ], BF16), ("x1", [SEQ, D], F32),
    ("hftok", [SEQ, D], BF16), ("xg", [NB * 128, D], BF16), ("yb", [NB * 128, D], F32),
]


def build(debug=False, phases=None, nexp=NEXP):
    nc = bass.Bass("TRN2", target_bir_lowering=False)
    P = Prog(nc)
    dr = {}
    for n, shp, dt in IN_SPECS:
        dr[n] = Buf(n, nc.dram_tensor(n, list(shp), dt, kind="ExternalInput").ap())
    for n, shp, dt in SCRATCH:
        kind = "ExternalOutput" if debug else "Internal"
        dr[n] = Buf(n, nc.dram_tensor(n, list(shp), dt, kind=kind).ap())
    dr["out"] = Buf("out", nc.dram_tensor("out", [SEQ, D], F32, kind="ExternalOutput").ap())
    ph = phases or ("adaln", "proj", "attn", "rwkv", "rwkv_out", "merge", "moe")
    cs = make_consts(P)
    mod = phase_adaln(P, cs, dr)
    if "proj" in ph:
        phase_proj(P, cs, mod, dr)
    if "attn" in ph:
        phase_attn(P, cs, dr)
    mR = P.mark()
    R = rwkv_setup(P, dr)
    if "rwkv" in ph:
        mW = P.mark()
        PB = ([P.psum("bk%d" % i, [128, 512]) for i in range(6)], [P.psum("bb%d" % i, [128, 1024], BF16) for i in range(2)], [0, 0])
        gens = list(rwkv_dir(P, cs, R, 0, dr, PB)) + list(rwkv_dir(P, cs, R, 1, dr, PB))
        while gens:
            for g_ in list(gens):
                try:
                    next(g_)
                except StopIteration:
                    gens.remove(g_)
        P.barrier()
        P.release(mW)
    if "rwkv_out" in ph:
        phase_rwkv_out(P, cs, R, dr)
    P.barrier()
    P.release(mR)
    rt = {"Mall": P.sbuf("Mall", [128, SEQ // 128, 32]), "Gall": P.sbuf("Gall", [128, SEQ // 128, 32])}
    if "merge" in ph:
        phase_merge(P, cs, mod, dr, rt)
    if "moe" in ph:
        phase_moe(P, cs, mod, rt, dr)
    P.barrier()
    P.emit()
    P.close()
    return nc, P


def host_layout(inp):
    f = lambda a: np.ascontiguousarray(np.asarray(a, np.float32))
    col = lambda v: f(np.asarray(v).reshape(-1, 128).T)
    sh = {}
    sh["w_ada"] = f(inp["w_ada"][0]); sh["b_ada"] = f(inp["b_ada"][0][None])
    sh["bada_col"] = col(inp["b_ada"][0])
    sh["gcols"] = f(np.concatenate([col(inp["g_pre_mix"][0]), col(inp["g_pre_ffn"][0])], 1))
    sh["g_post_mix"] = f(inp["g_post_mix"][0][None]); sh["g_post_ffn"] = f(inp["g_post_ffn"][0][None]); sh["g_pre_ffn"] = f(inp["g_pre_ffn"][0][None])
    w_in = np.asarray(inp["w_in"][0], np.float32); b_in = np.asarray(inp["b_in"][0], np.float32)
    d = np.arange(64)
    partner = np.where((d % 32) < 16, d + 16, d - 16)
    qperm = (np.arange(8)[:, None] * 64 + partner[None, :]).reshape(-1)
    kperm = 512 + (np.arange(2)[:, None] * 64 + partner[None, :]).reshape(-1)
    cols = np.concatenate([np.arange(4736), qperm, kperm])
    sh["w_in"] = f(w_in[:, cols])
    sh["bin_col"] = col(b_in[cols])
    sh["bv_row"] = f(b_in[640:768][None])
    half = 32
    inv_freq = (np.float32(10000.0) ** (-np.arange(0, half, 2, dtype=np.float32) / np.float32(half))).astype(np.float32)
    t = np.arange(SEQ)
    row = (t // 64).astype(np.float32); colp = (t % 64).astype(np.float32)
    dd = np.arange(128) % 64
    pos = np.where((dd < 32)[:, None], row[None, :], colp[None, :]).astype(np.float32)
    ang = (pos * inv_freq[dd % 16][:, None]).astype(np.float32)
    sign = np.where((dd % 32) < 16, -1.0, 1.0).astype(np.float32)[:, None]
    sh["cos_t"] = f(np.cos(ang)); sh["sin_t"] = f(np.sin(ang) * sign)
    sh["sink_row"] = f(np.repeat(np.asarray(inp["att_sinks"][0], np.float32), 128)[None])
    sh["pp64"] = pack64(inp)
    for n in ("w2_f", "w2_b", "a2_f", "a2_b", "g2", "w_up_att", "w_up_rwkv", "w_out", "w_router", "b_down", "b_gate_up"):
        sh[n] = f(inp[n][0])
    wgu = np.asarray(inp["w_gate_up"][0], np.float32).reshape(NEXP, 8, 128, 4, 512)
    sh["wgu2d"] = np.ascontiguousarray(wgu.transpose(0, 2, 3, 1, 4)).reshape(NEXP * 128 * 4, 4096)
    wdn = np.asarray(inp["w_down"][0], np.float32).reshape(NEXP, 8, 128, 2, 512)
    sh["wdn2d"] = np.ascontiguousarray(wdn.transpose(0, 2, 3, 1, 4)).reshape(NEXP * 128 * 2, 4096)
    sh["mugl"] = f(np.stack([inp["mu_prev"][0][1792:1920], inp["mu_next"][0][1792:1920]], 1))
    sh["b_router"] = f(inp["b_router"][0][None])
    cc = np.asarray(inp["c_ctx"], np.float32)
    percore = []
    for b in range(inp["x"].shape[0]):
        m = dict(sh)
        m["x"] = f(inp["x"][b]); m["ctx"] = f(inp["ctx"][b])
        m["ccol"] = f(np.concatenate([col(inp["c"][b]), col(cc)], 1))
        percore.append(m)
    return percore


_NC = {}


def kernel(**inputs):
    if "nc" not in _NC:
        _NC["nc"] = build()[0]
    in_maps = host_layout(inputs)
    res = run_bass_kernel_spmd(_NC["nc"], in_maps, core_ids=list(range(len(in_maps))))
    return np.stack([np.asarray(r["out"], np.float32) for r in res.results], 0)
```

```python
import numpy as np
import concourse.bass as bass
import concourse.mybir as mybir
from concourse.bass_utils import run_bass_kernel_spmd

F32 = mybir.dt.float32
BF16 = mybir.dt.bfloat16
AF = mybir.ActivationFunctionType
ALU = mybir.AluOpType

SEQ = 4096
CTX = 256
TT = SEQ + CTX
D = 1024
HD = 64
NH = 8
C = 64
W = 64
NEXP = 32


class Buf:
    __slots__ = ("name", "t", "lw", "rd", "fence")

    def __init__(self, name, t):
        self.name = name
        self.t = t
        self.lw = []
        self.rd = []
        self.fence = []

    def __getitem__(self, idx):
        return self.t[idx]


class Prog:
    ENG = ("pe", "act", "dve", "pool", "sp")

    def __init__(self, nc, n_dma_sems=32):
        self.nc = nc
        self.q = {e: [] for e in self.ENG}
        self.cnt = {}
        self.known = {e: {} for e in self.ENG}
        self.sems = {}
        self.n_dma_sems = n_dma_sems
        self.dma_i = 0
        self.stack = []
        self.ninst = 0
        self.uid = 0
        self.tot = {}
        self.regs = {}

    def enter(self, cm):
        v = cm.__enter__()
        self.stack.append(cm)
        return v

    def mark(self):
        return len(self.stack)

    def release(self, mark):
        while len(self.stack) > mark:
            self.stack.pop().__exit__(None, None, None)

    def sbuf(self, name, shape, dtype=F32):
        self.uid += 1
        t = self.enter(self.nc.sbuf_tensor("%s_%d" % (name, self.uid), list(shape), dtype))
        return Buf(name, t)

    def psum(self, name, shape, dtype=F32):
        self.uid += 1
        t = self.enter(self.nc.psum_tensor("%s_%d" % (name, self.uid), list(shape), dtype))
        return Buf(name, t)

    def dram(self, name, shape, dtype=F32):
        t = self.nc.dram_tensor(name, list(shape), dtype, kind="Internal")
        return Buf(name, t.ap())

    def sem(self, key):
        if key not in self.sems:
            nm = "s_" + "_".join(str(k) for k in (key if isinstance(key, tuple) else (key,)))
            self.sems[key] = self.enter(self.nc.semaphore(nm))
            self.cnt[key] = 0
        return self.sems[key]

    def _deps(self, eng, reads, writes, shared=()):
        deps = {}

        def add(d):
            k, v = d
            if deps.get(k, 0) < v:
                deps[k] = v
        for b in reads:
            for d in b.lw:
                add(d)
            for d in b.fence:
                add(d)
        for b in writes:
            for d in b.lw:
                add(d)
            for d in b.fence:
                add(d)
            for d in b.rd:
                add(d)
        for b in shared:
            for d in b.fence:
                add(d)
            for d in b.rd:
                add(d)
        out = []
        for k, v in deps.items():
            if eng == "pe" and isinstance(k, tuple) and k[0] == "pe":
                continue
            if self.known[eng].get(k, 0) >= v:
                continue
            self.known[eng][k] = v
            out.append((k, v))
        return out

    @staticmethod
    def _compact(lst):
        mx = {}
        for k, v in lst:
            if mx.get(k, 0) < v:
                mx[k] = v
        return list(mx.items())

    def _mark(self, key, val, reads, writes, shared=()):
        for b in reads:
            b.rd.append((key, val))
            if len(b.rd) > 64:
                b.rd = self._compact(b.rd)
        for b in writes:
            b.lw = [(key, val)]
            b.rd = []
            b.fence = []
        for b in shared:
            b.lw.append((key, val))
            if len(b.lw) > 64:
                b.lw = self._compact(b.lw)

    def fence(self, b):
        b.fence = self._compact(b.fence + b.lw)
        b.lw = []

    EPOCH = 4000

    def op(self, eng, fn, reads=(), writes=()):
        n = self.tot.get(eng, 0)
        self.tot[eng] = n + 1
        key = (eng, n // self.EPOCH)
        self.sem(key)
        waits = self._deps(eng, reads, writes)
        self.cnt[key] += 1
        val = self.cnt[key]
        self.q[eng].append(("op", fn, waits, key, 1))
        self._mark(key, val, reads, writes)
        self.ninst += 1

    def dma(self, eng, out, in_, reads=(), writes=(), shared=(), ind=None):
        i = self.dma_i % self.n_dma_sems
        self.dma_i += 1
        key = ("dma", i)
        self.sem(key)
        waits = self._deps(eng, reads, writes, shared)
        prev = self.cnt[key]
        if prev > 0 and self.known[eng].get(key, 0) < prev:
            self.known[eng][key] = prev
            waits.append((key, prev))
        self.cnt[key] += 16
        val = self.cnt[key]
        self.q[eng].append(("dma", (out, in_, ind), waits, key, 16))
        self._mark(key, val, reads, writes, shared)
        self.ninst += 1

    def reg(self, e, val):
        k = (id(e), val)
        if k not in self.regs:
            self.regs[k] = e.to_reg(val)
        return self.regs[k]

    def barrier(self):
        snap = dict(self.cnt)
        for e in self.ENG:
            waits = []
            for k, v in snap.items():
                if v > 0 and self.known[e].get(k, 0) < v:
                    self.known[e][k] = v
                    waits.append((k, v))
            if waits:
                self.q[e].append(("wait", None, waits, None, 0))

    def emit(self):
        nc = self.nc
        block = self.enter(nc.Block())
        engmap = {"pe": "tensor", "act": "scalar", "dve": "vector", "pool": "gpsimd", "sp": "sync"}
        prog = self

        def make(ename):
            items = prog.q[ename]

            def body(e):
                for kind, payload, waits, key, inc in items:
                    for (k, v) in waits:
                        e.wait_ge(prog.sems[k], v)
                    if kind == "op":
                        payload(e).then_inc(prog.sems[key], inc)
                    elif kind == "dma":
                        o, i, ind = payload
                        if ind is None:
                            e.dma_start(out=o, in_=i).then_inc(prog.sems[key], inc)
                        else:
                            idx_ap, on_out, bound = ind
                            off = bass.IndirectOffsetOnAxis(ap=idx_ap, axis=0)
                            try:
                                ins = e.indirect_dma_start(out=o, out_offset=off if on_out else None, in_=i,
                                                           in_offset=None if on_out else off, bounds_check=prog.reg(e, bound),
                                                           oob_is_err=False)
                            except Exception:
                                print("INDIRECT FAIL", o.shape, o.ap, i.shape, i.ap, idx_ap.shape, idx_ap.ap, on_out, bound)
                                raise
                            ins.then_inc(prog.sems[key], inc)
            return body

        for ename in self.ENG:
            if self.q[ename]:
                getattr(block, engmap[ename])(make(ename))

    def close(self):
        self.release(0)


def tt(P, eng, out, i0, i1, op, reads, writes):
    P.op(eng, lambda e: e.tensor_tensor(out=out, in0=i0, in1=i1, op=op), reads=reads, writes=writes)


def act(P, out, in_, func, reads, writes, bias=None, scale=None):
    kw = {}
    if bias is not None:
        kw["bias"] = bias
    if scale is not None:
        kw["scale"] = scale
    P.op("act", lambda e: e.activation(out=out, in_=in_, func=func, **kw), reads=reads, writes=writes)


def mm(P, out, lhsT, rhs, start, stop, reads, writes):
    P.op("pe", lambda e: e.matmul(out, lhsT=lhsT, rhs=rhs, start=start, stop=stop), reads=reads, writes=writes)


def tr(P, out, in_, ident, reads, writes):
    P.op("pe", lambda e: e.transpose(out, in_, ident), reads=reads, writes=writes)


def make_consts(P):
    cs = {}
    ones = P.sbuf("ones128", [128, 128])
    P.op("pool", lambda e: e.memset(ones[:], 1.0), writes=[ones])
    cs["ones"] = ones

    def sel(name, cmp, base, cm, pat):
        b = P.sbuf(name, [128, 128])
        P.op("pool", lambda e: e.affine_select(out=b[:], in_=ones[:], pattern=[[pat, 128]], compare_op=cmp,
                                               fill=P.reg(e, 0.0), base=base, channel_multiplier=cm),
             reads=[ones], writes=[b])
        cs[name] = b
    sel("ident", ALU.is_equal, 0, 1, -1)
    sel("Lstrict", ALU.is_gt, 0, -1, 1)
    sel("Lincl", ALU.is_ge, 0, -1, 1)
    sel("Ustrict", ALU.is_gt, 0, 1, -1)
    sel("Uincl", ALU.is_ge, 0, 1, -1)
    return cs


P64 = {}
_o = 0
for _n, _w in [("mup3", 24), ("mun3", 24), ("mup_wl", 2), ("mun_wl", 2), ("mup_al", 2), ("mun_al", 2),
               ("k_k", 8), ("k_a", 8), ("r_k", 8), ("w0_f", 8), ("w0_b", 8), ("a0_f", 8), ("a0_b", 8),
               ("ln_w", 8), ("ln_b", 8)]:
    P64[_n] = (_o, _w)
    _o += _w
NP64 = _o


def pack64(inp):
    def hj(v):
        return np.ascontiguousarray(np.asarray(v, np.float32).reshape(-1, 64).T)
    mp, mn = inp["mu_prev"][0], inp["mu_next"][0]
    parts = {
        "mup3": hj(mp[0:1536]), "mun3": hj(mn[0:1536]),
        "mup_wl": hj(mp[1536:1664]), "mun_wl": hj(mn[1536:1664]),
        "mup_al": hj(mp[1664:1792]), "mun_al": hj(mn[1664:1792]),
        "k_k": hj(inp["k_k"][0]), "k_a": hj(inp["k_a"][0]), "r_k": hj(inp["r_k"][0].reshape(-1)),
        "w0_f": hj(inp["w0_f"][0]), "w0_b": hj(inp["w0_b"][0]),
        "a0_f": hj(inp["a0_f"][0]), "a0_b": hj(inp["a0_b"][0]),
        "ln_w": hj(inp["ln_x_w"][0]), "ln_b": hj(inp["ln_x_b"][0]),
    }
    out = np.zeros((64, NP64), np.float32)
    for n, (o, w) in P64.items():
        out[:, o:o + w] = parts[n]
    return out


class EW:
    def __init__(self, engines=("dve", "pool")):
        self.e = engines
        self.i = 0

    def __call__(self):
        self.i += 1
        return self.e[self.i % len(self.e)]


def rwkv_setup(P, dr):
    R = {}
    pp = P.sbuf("pp64", [64, NP64])
    P.dma("sp", pp[:], dr["pp64"][:], writes=[pp])
    R["pp"] = pp
    for n in ("w2_f", "w2_b", "a2_f", "a2_b"):
        b = P.sbuf(n, [64, 512])
        P.dma("sp", b[:], dr[n][:], writes=[b])
        R[n] = b
    g2 = P.sbuf("g2", [128, 512])
    P.dma("sp", g2[:], dr["g2"][:], writes=[g2])
    R["g2"] = g2
    mugl = P.sbuf("mugl", [128, 2])
    P.dma("sp", mugl[:], dr["mugl"][:], writes=[mugl])
    R["mugl"] = mugl
    eps18 = P.sbuf("eps18", [64, 1])
    P.op("pool", lambda e: e.memset(eps18[:], 1e-18), writes=[eps18])
    R["eps18"] = eps18
    omka = P.sbuf("omka", [64, 8])
    o, w = P64["k_a"]
    P.op("dve", lambda e: e.tensor_scalar(out=omka[:], in0=pp[:, o:o + w], scalar1=-1.0, scalar2=1.0,
                                          op0=ALU.mult, op1=ALU.add), reads=[pp], writes=[omka])
    R["omka"] = omka
    return R


def rwkv_dir(P, cs, R, dirn, dr, PB, dbg=None, max_win=None):
    nwin = TT // W
    nch = W // C
    nctx = CTX // W
    if dirn == 0:
        order = list(range(nwin))
    else:
        order = list(range(nctx - 1, -1, -1)) + list(range(nwin - 1, nctx - 1, -1))
    if max_win is not None:
        order = order[:max_win]
    pp = R["pp"]
    ident = cs["ident"]
    ones = cs["ones"]
    idn = ident[0:64, 0:64]

    def prm(n):
        o, w = P64[n]
        return pp[:, o:o + w]

    def bc(ap, shape):
        return ap.broadcast_to(shape)

    dsuf = "_f" if dirn == 0 else "_b"
    mAR = P.sbuf("mAR", [64, 128])
    mNT = P.sbuf("mNT", [64, 64])
    st, inc, ntm = ("Lstrict", "Lincl", "Ustrict") if dirn == 0 else ("Ustrict", "Uincl", "Lstrict")
    P.op("dve", lambda e: e.tensor_copy(out=mAR[:, 0:64], in_=cs[st][0:64, 0:64]), reads=[cs[st]], writes=[mAR])
    P.op("dve", lambda e: e.tensor_copy(out=mAR[:, 64:128], in_=cs[inc][0:64, 0:64]), reads=[cs[inc]], writes=[mAR])
    P.op("dve", lambda e: e.tensor_copy(out=mNT[:], in_=cs[ntm][0:64, 0:64]), reads=[cs[ntm]], writes=[mNT])

    ST = P.sbuf("ST", [64, 8, 64], BF16)
    P.op("pool", lambda e: e.memset(ST[:], 0.0), writes=[ST])
    identb = P.sbuf("identb_r", [64, 64], BF16)
    P.op("dve", lambda e: e.tensor_copy(out=identb[:], in_=ident[0:64, 0:64]), reads=[ident], writes=[identb])

    Z3 = P.sbuf("Z3", [64, 24, W + 2])
    ZW = P.sbuf("ZW", [64, 2, W + 2])
    ZA = P.sbuf("ZA", [64, 2, W + 2])
    ZG = P.sbuf("ZG", [128, W + 2])
    zs3 = P.sbuf("zs3", [64, 24, W])
    wls = P.sbuf("wls", [64, 2, W])
    als = P.sbuf("als", [64, 2, W])
    gls = P.sbuf("gls", [128, W])
    tmps = P.sbuf("tmps", [128, W])
    icl = [P.sbuf("icl0", [64, 8, W]), P.sbuf("icl1", [64, 8, W])]
    lw = P.sbuf("lw", [64, 8, W])
    kkn = P.sbuf("kkn", [64, 8, W])
    t8a = P.sbuf("t8a", [64, 8, W])
    t8b = P.sbuf("t8b", [64, 8, W])
    bd = P.sbuf("bd", [64, 8, W])
    kd = [P.sbuf("kd0", [64, 8, W]), P.sbuf("kd1", [64, 8, W])]
    cw = P.sbuf("cw", [64, 8, W])
    E1s = [P.sbuf("E1_%d" % i, [64, 8, W]) for i in range(2)]
    vHs = [P.sbuf("vH_%d" % i, [64, 8, W]) for i in range(2)]
    Einv = P.sbuf("Einv", [64, 8, W])
    Ehat = P.sbuf("Ehat", [64, 8, W])
    ARs = [P.sbuf("AR_%d" % i, [64, 8, nch, 128], BF16) for i in range(2)]
    Bts = [P.sbuf("Bt_%d" % i, [64, 8, W], BF16) for i in range(2)]
    Kts = [P.sbuf("Kt_%d" % i, [64, 8, W], BF16) for i in range(2)]
    Bhs = [P.sbuf("Bh_%d" % i, [64, 8, W], BF16) for i in range(2)]
    Khs = [P.sbuf("Kh_%d" % i, [64, 8, W], BF16) for i in range(2)]
    Yw = P.sbuf("Yw", [64, 8, W])
    CH = {n: P.sbuf(n, [64, 8, 64], BF16) for n in
          ("AtT", "BhT", "KhT", "VT", "X0T", "XA", "XB", "XTA", "XTB", "T", "AhT", "Xs", "VhT", "Gs", "Qs")}
    CH["dW"] = P.sbuf("dW", [64, 8, 64])
    MB = P.sbuf("MB", [64, 8, 128], BF16)
    MK = P.sbuf("MK", [64, 8, 128], BF16)
    banks, bbanks, bki = PB

    def bank():
        bki[0] += 1
        return banks[bki[0] % len(banks)]

    def bbank():
        bki[1] += 1
        return bbanks[bki[1] % len(bbanks)]

    ew = EW()
    S3 = [64, 24, W]
    S8 = [64, 8, W]

    def shift3():
        for q in range(3):
            qs = slice(8 * q, 8 * q + 8)
            ctr, prv, nxt = (slice(None), qs, slice(1, W + 1)), (slice(None), qs, slice(0, W)), (slice(None), qs, slice(2, W + 2))
            mup = prm("mup3")[:, qs, None]
            mun = prm("mun3")[:, qs, None]
            e1 = "dve" if q != 1 else "pool"
            tmp = t8a if q != 1 else t8b
            tt(P, e1, tmp[:], Z3[prv], Z3[ctr], ALU.subtract, [Z3], [tmp])
            tt(P, e1, tmp[:], tmp[:], bc(mup, S8), ALU.mult, [tmp, pp], [tmp])
            tt(P, e1, zs3[:, qs, :], tmp[:], Z3[ctr], ALU.add, [tmp, Z3], [zs3])
            tt(P, e1, tmp[:], Z3[nxt], Z3[ctr], ALU.subtract, [Z3], [tmp])
            tt(P, e1, tmp[:], tmp[:], bc(mun, S8), ALU.mult, [tmp, pp], [tmp])
            tt(P, e1, zs3[:, qs, :], zs3[:, qs, :], tmp[:], ALU.add, [tmp, zs3], [zs3])

    zr = dr["zrT"]
    state = {"feat": -1, "chunk": -1}

    def feat():
        for wn, wi in enumerate(order):
            while state["chunk"] < wn - 2:
                yield
            hs = wn % 2
            AR, Bt, Kt, Bh, Kh, E1, vH = ARs[hs], Bts[hs], Kts[hs], Bhs[hs], Khs[hs], E1s[hs], vHs[hs]
            t0 = wi * W
            is_ctx = t0 < CTX
            lo_d, hi_d = (0, CTX) if is_ctx else (CTX, TT)
            lo = max(t0 - 1, lo_d)
            hi = min(t0 + W + 1, hi_d)
            a = lo - (t0 - 1)
            b = a + (hi - lo)
            for Z in (Z3, ZW, ZA, ZG):
                nd = len(Z.t.shape)
                if a > 0:
                    ix = (slice(None),) * (nd - 1) + (slice(0, 1),)
                    P.op("pool", lambda e, Z=Z, ix=ix: e.memset(Z[ix], 0.0), writes=[Z])
                if b < W + 2:
                    ix = (slice(None),) * (nd - 1) + (slice(W + 1, W + 2),)
                    P.op("pool", lambda e, Z=Z, ix=ix: e.memset(Z[ix], 0.0), writes=[Z])
            P.dma("sp", Z3[:, :, a:b], zr[0:1536, lo:hi].rearrange("(q j) t -> j q t", j=64), reads=[zr], writes=[Z3])
            P.dma("sp", ZW[:, :, a:b], zr[1536:1664, lo:hi].rearrange("(q j) t -> j q t", j=64), reads=[zr], writes=[ZW])
            P.dma("sp", ZA[:, :, a:b], zr[1664:1792, lo:hi].rearrange("(q j) t -> j q t", j=64), reads=[zr], writes=[ZA])
            P.dma("sp", ZG[:, a:b], zr[1792:1920, lo:hi], reads=[zr], writes=[ZG])
            yield
            shift3()
            yield
            for (zraw, zs, mupn, munn) in ((ZW, wls, "mup_wl", "mun_wl"), (ZA, als, "mup_al", "mun_al")):
                tv = t8a[:, 0:2, :]
                shp = [64, 2, W]
                c_, p_, n_ = (slice(None), slice(None), slice(1, W + 1)), (slice(None), slice(None), slice(0, W)), (slice(None), slice(None), slice(2, W + 2))
                tt(P, "dve", tv, zraw[p_], zraw[c_], ALU.subtract, [zraw], [t8a])
                tt(P, "dve", tv, tv, bc(prm(mupn)[:, :, None], shp), ALU.mult, [t8a, pp], [t8a])
                tt(P, "dve", zs[:], tv, zraw[c_], ALU.add, [t8a, zraw], [zs])
                tt(P, "dve", tv, zraw[n_], zraw[c_], ALU.subtract, [zraw], [t8a])
                tt(P, "dve", tv, tv, bc(prm(munn)[:, :, None], shp), ALU.mult, [t8a, pp], [t8a])
                tt(P, "dve", zs[:], zs[:], tv, ALU.add, [t8a, zs], [zs])
            mugl = R["mugl"]
            c2, p2, n2 = (slice(None), slice(1, W + 1)), (slice(None), slice(0, W)), (slice(None), slice(2, W + 2))
            tt(P, "dve", tmps[:], ZG[p2], ZG[c2], ALU.subtract, [ZG], [tmps])
            P.op("dve", lambda e: e.scalar_tensor_tensor(out=gls[:], in0=tmps[:], scalar=mugl[:, 0:1], in1=ZG[c2],
                                                         op0=ALU.mult, op1=ALU.add), reads=[tmps, mugl, ZG], writes=[gls])
            tt(P, "dve", tmps[:], ZG[n2], ZG[c2], ALU.subtract, [ZG], [tmps])
            P.op("dve", lambda e: e.scalar_tensor_tensor(out=gls[:], in0=tmps[:], scalar=mugl[:, 1:2], in1=gls[:],
                                                         op0=ALU.mult, op1=ALU.add), reads=[tmps, mugl, gls], writes=[gls])
            yield
            r_ = zs3[:, 0:8, :]
            k_ = zs3[:, 8:16, :]
            v_ = zs3[:, 16:24, :]
            for d2 in ((0, 1) if (dirn == 0 and not is_ctx) else (dirn,)):
                a2 = R["a2_f" if d2 == 0 else "a2_b"]
                pb = [bank(), bank()]
                for h in range(8):
                    q = pb[h // 4]
                    mm(P, q[0:64, (h % 4) * W:(h % 4 + 1) * W], a2[:, h * 64:(h + 1) * 64], als[:, d2, :], True, True,
                       [a2, als], [q])
                a0 = prm("a0_f" if d2 == 0 else "a0_b")
                for g in range(2):
                    tt(P, "dve", icl[d2][:, 4 * g:4 * g + 4, :], pb[g][0:64, 0:4 * W].rearrange("p (h t) -> p h t", h=4),
                       bc(a0[:, 4 * g:4 * g + 4, None], [64, 4, W]), ALU.add, [pb[g], pp], [icl[d2]])
                act(P, icl[d2][:], icl[d2][:], AF.Sigmoid, [icl[d2]], [icl[d2]])
            yield
            th = tmps[0:64, :]
            act(P, th, wls[:, dirn, :], AF.Tanh, [wls], [tmps])
            w2 = R["w2_f" if dirn == 0 else "w2_b"]
            pb = [bank(), bank()]
            for h in range(8):
                q = pb[h // 4]
                mm(P, q[0:64, (h % 4) * W:(h % 4 + 1) * W], w2[:, h * 64:(h + 1) * 64], th, True, True, [w2, tmps], [q])
            w0 = prm("w0_f" if dirn == 0 else "w0_b")
            for g in range(2):
                tt(P, "dve", lw[:, 4 * g:4 * g + 4, :], pb[g][0:64, 0:4 * W].rearrange("p (h t) -> p h t", h=4),
                   bc(w0[:, 4 * g:4 * g + 4, None], [64, 4, W]), ALU.add, [pb[g], pp], [lw])
            act(P, lw[:], lw[:], AF.Sigmoid, [lw], [lw])
            P.op("dve", lambda e: e.tensor_scalar(out=lw[:], in0=lw[:], scalar1=-float(np.exp(-0.5)), scalar2=None,
                                                  op0=ALU.mult), reads=[lw], writes=[lw])
            yield
            tt(P, "pool", t8a[:], k_, bc(prm("k_k")[:, :, None], S8), ALU.mult, [zs3, pp], [t8a])
            tt(P, "pool", t8b[:], t8a[:], t8a[:], ALU.mult, [t8a], [t8b])
            pb = [bank(), bank()]
            for g in range(2):
                mm(P, pb[g][0:64, 0:4 * W], ones[0:64, 0:64], t8b[:, 4 * g:4 * g + 4, :], True, True, [ones, t8b], [pb[g]])
            for g in range(2):
                act(P, kkn[:, 4 * g:4 * g + 4, :], pb[g][0:64, 0:4 * W].rearrange("p (h t) -> p h t", h=4), AF.Ln, [pb[g]], [kkn], bias=R["eps18"][:, 0:1])
            act(P, kkn[:], kkn[:], AF.Exp, [kkn], [kkn], scale=-0.5)
            tt(P, "dve", kkn[:], kkn[:], t8a[:], ALU.mult, [kkn, t8a], [kkn])
            yield
            dirs_needed = (0, 1) if (dirn == 0 and not is_ctx) else (dirn,)
            for d2 in dirs_needed:
                e1 = ew()
                tt(P, e1, kd[d2][:], icl[d2][:], bc(prm("k_a")[:, :, None], S8), ALU.mult, [icl[d2], pp], [kd[d2]])
                tt(P, e1, kd[d2][:], kd[d2][:], bc(R["omka"][:, :, None], S8), ALU.add, [kd[d2], R["omka"]], [kd[d2]])
                tt(P, e1, kd[d2][:], kd[d2][:], k_, ALU.mult, [kd[d2], zs3], [kd[d2]])
            tt(P, "pool", bd[:], kkn[:], icl[dirn][:], ALU.mult, [kkn, icl[dirn]], [bd])
            if dirn == 0 and not is_ctx:
                tt(P, "pool", t8a[:], kd[0][:], kd[1][:], ALU.add, [kd[0], kd[1]], [t8a])
                tt(P, "pool", t8a[:], t8a[:], r_, ALU.mult, [t8a, zs3], [t8a])
                tt(P, "pool", t8a[:], t8a[:], bc(prm("r_k")[:, :, None], S8), ALU.mult, [t8a, pp], [t8a])
                pb = [bank(), bank()]
                for g in range(2):
                    mm(P, pb[g][0:64, 0:4 * W], ones[0:64, 0:64], t8a[:, 4 * g:4 * g + 4, :], True, True, [ones, t8a], [pb[g]])
                for g in range(2):
                    tt(P, "dve", t8b[:, 4 * g:4 * g + 4, :], pb[g][0:64, 0:4 * W].rearrange("p (h t) -> p h t", h=4),
                       v_[:, 4 * g:4 * g + 4, :], ALU.mult, [pb[g], zs3], [t8b])
                P.dma("sp", dr["bonusT"][:, t0 - CTX:t0 - CTX + W].rearrange("(h i) t -> i h t", i=64), t8b[:],
                      reads=[t8b], writes=[dr["bonusT"]])
                sg = tmps
                act(P, sg[:], gls[:], AF.Sigmoid, [gls], [tmps])
                pb = [bank(), bank()]
                g2 = R["g2"]
                for h in range(8):
                    q = pb[h // 4]
                    mm(P, q[0:64, (h % 4) * W:(h % 4 + 1) * W], g2[:, h * 64:(h + 1) * 64], sg[:], True, True, [g2, tmps], [q])
                for g in range(2):
                    act(P, t8a[:, 4 * g:4 * g + 4, :], pb[g][0:64, 0:4 * W].rearrange("p (h t) -> p h t", h=4), AF.Copy, [pb[g]], [t8a])
                P.dma("sp", dr["gateT"][:, t0 - CTX:t0 - CTX + W].rearrange("(h i) t -> i h t", i=64), t8a[:],
                      reads=[t8a], writes=[dr["gateT"]])
            yield
            for h in range(8):
                for c in range(nch):
                    if dirn == 0:
                        sl = slice(c * C, (c + 1) * C)
                    else:
                        sl = slice(c * C + C - 1, (c * C - 1) if c > 0 else None, -1)
                    P.op("dve", lambda e, h=h, sl=sl: e.tensor_tensor_scan(
                        out=cw[:, h, sl], data0=ones[0:64, 0:64], data1=lw[:, h, sl], initial=0.0,
                        op0=ALU.mult, op1=ALU.add), reads=[lw, ones], writes=[cw])
            yield
            act(P, E1[:], cw[:], AF.Exp, [cw], [E1])
            act(P, Einv[:], cw[:], AF.Exp, [cw], [Einv], scale=-1.0)
            tt(P, "pool", t8a[:], cw[:], lw[:], ALU.subtract, [cw, lw], [t8a])
            act(P, t8a[:], t8a[:], AF.Exp, [t8a], [t8a])
            cend = C - 1 if dirn == 0 else 0
            E1v = E1[:].rearrange("p h (c t) -> p h c t", t=C)
            Wc = E1v[:, :, :, cend:cend + 1]
            tt(P, "pool", Ehat[:].rearrange("p h (c t) -> p h c t", t=C), Einv[:].rearrange("p h (c t) -> p h c t", t=C),
               bc(Wc, [64, 8, nch, C]), ALU.mult, [Einv, E1], [Ehat])
            P.op("dve", lambda e, AR=AR: e.scalar_tensor_tensor(
                out=AR[:, :, :, 0:64], in0=kkn[:].rearrange("p h (c t) -> p h c t", t=C), scalar=-1.0,
                in1=t8a[:].rearrange("p h (c t) -> p h c t", t=C), op0=ALU.mult, op1=ALU.mult),
                reads=[kkn, t8a], writes=[AR])
            tt(P, "pool", AR[:, :, :, 64:128], r_.rearrange("p h (c t) -> p h c t", t=C), E1v, ALU.mult, [zs3, E1], [AR])
            tt(P, "dve", Bt[:], bd[:], Einv[:], ALU.mult, [bd, Einv], [Bt])
            tt(P, "pool", Kt[:], kd[dirn][:], Einv[:], ALU.mult, [kd[dirn], Einv], [Kt])
            tt(P, "dve", Bh[:], bd[:], Ehat[:], ALU.mult, [bd, Ehat], [Bh])
            tt(P, "pool", Kh[:], kd[dirn][:], Ehat[:], ALU.mult, [kd[dirn], Ehat], [Kh])

            act(P, vH[:], zs3[:, 16:24, :], AF.Copy, [zs3], [vH])
            state["feat"] = wn
            yield

    def chunk():
        for wn, wi in enumerate(order):
            while state["feat"] < wn:
                yield
            hs = wn % 2
            AR, Bt, Kt, Bh, Kh, E1, vH = ARs[hs], Bts[hs], Kts[hs], Bhs[hs], Khs[hs], E1s[hs], vHs[hs]
            t0 = wi * W
            is_ctx = t0 < CTX
            cend = C - 1 if dirn == 0 else 0
            corder = range(nch) if dirn == 0 else range(nch - 1, -1, -1)
            for c in corder:
                sl = slice(c * C, (c + 1) * C)
                for (srcb, srcf, dst) in ((AR, lambda h: AR[:, h, c, 0:64], "AtT"), (Bh, lambda h: Bh[:, h, sl], "BhT"),
                                          (Kh, lambda h: Kh[:, h, sl], "KhT")):
                    q = bbank()
                    for h in range(8):
                        tr(P, q[0:64, h * 64:(h + 1) * 64], srcf(h), identb[:], [srcb, identb], [q])
                    act(P, CH[dst][:], q[0:64, 0:512].rearrange("p (h t) -> p h t", h=8), AF.Copy, [q], [CH[dst]])
                q = bank()
                for h in range(8):
                    tr(P, q[0:64, h * 64:(h + 1) * 64], vH[:, h, sl], idn, [vH, ident], [q])
                act(P, CH["VT"][:], q[0:64, :].rearrange("p (h t) -> p h t", h=8), AF.Copy, [q], [CH["VT"]])
                yield
                for (L, dstM) in ((Bt, MB), (Kt, MK)):
                    pb = [bank(), bank()]
                    for h in range(8):
                        q = pb[h // 4]
                        mm(P, q[0:64, (h % 4) * 128:(h % 4 + 1) * 128], L[:, h, sl], AR[:, h, c, :], True, True, [L, AR], [q])
                    for g in range(2):
                        tt(P, "dve", dstM[:, 4 * g:4 * g + 4, :], pb[g][0:64, :].rearrange("p (h t) -> p h t", h=4),
                           bc(mAR[:, None, :], [64, 4, 128]), ALU.mult, [pb[g], mAR], [dstM])
                q = bank()
                for h in range(8):
                    mm(P, q[0:64, h * 64:(h + 1) * 64], AR[:, h, c, 0:64], Bt[:, h, sl], True, True, [AR, Bt], [q])
                X0T = CH["X0T"]
                tt(P, "dve", X0T[:], q[0:64, :].rearrange("p (h t) -> p h t", h=8), bc(mNT[:, None, :], [64, 8, 64]),
                   ALU.mult, [q, mNT], [X0T])
                yield
                q = bank()
                for h in range(8):
                    mm(P, q[0:64, h * 64:(h + 1) * 64], MK[:, h, 0:64], CH["VT"][:, h, :], True, True, [MK, CH["VT"]], [q])
                act(P, CH["Xs"][:], q[0:64, :].rearrange("p (h t) -> p h t", h=8), AF.Copy, [q], [CH["Xs"]])
                yield
                T = CH["T"]
                tt(P, "dve", T[:], MB[:, :, 0:64], bc(idn[:, None, :], [64, 8, 64]), ALU.add, [MB, ident], [T])
                Xc_b, XTc_b = MB, X0T
                Xc = lambda h: MB[:, h, 0:64]
                XTc = lambda h: X0T[:, h, :]
                for k in range(1, 6):
                    Xn_b = CH["XA"] if k % 2 else CH["XB"]
                    XTn_b = CH["XTA"] if k % 2 else CH["XTB"]
                    q1 = bank()
                    for h in range(8):
                        mm(P, q1[0:64, h * 64:(h + 1) * 64], Xc(h), XTc(h), True, True, [Xc_b, XTc_b], [q1])
                    act(P, XTn_b[:], q1[0:64, :].rearrange("p (h t) -> p h t", h=8), AF.Copy, [q1], [XTn_b])
                    if k < 5:
                        q2 = bank()
                        for h in range(8):
                            mm(P, q2[0:64, h * 64:(h + 1) * 64], XTc(h), Xc(h), True, True, [Xc_b, XTc_b], [q2])
                        act(P, Xn_b[:], q2[0:64, :].rearrange("p (h t) -> p h t", h=8), AF.Copy, [q2], [Xn_b])
                    yield
                    q3 = bank()
                    for h in range(8):
                        mm(P, q3[0:64, h * 64:(h + 1) * 64], XTn_b[:, h, :], T[:, h, :], True, True, [XTn_b, T], [q3])
                    tt(P, "dve", T[:], T[:], q3[0:64, :].rearrange("p (h t) -> p h t", h=8), ALU.add, [T, q3], [T])
                    yield
                    Xc_b, XTc_b = Xn_b, XTn_b
                    Xc = lambda h, b_=Xn_b: b_[:, h, :]
                    XTc = lambda h, b_=XTn_b: b_[:, h, :]
                yield
                q = bank()
                for h in range(8):
                    mm(P, q[0:64, h * 64:(h + 1) * 64], T[:, h, :], CH["AtT"][:, h, :], True, True, [T, CH["AtT"]], [q])
                act(P, CH["AhT"][:], q[0:64, :].rearrange("p (h t) -> p h t", h=8), AF.Copy, [q], [CH["AhT"]])
                q = bank()
                for h in range(8):
                    mm(P, q[0:64, h * 64:(h + 1) * 64], T[:, h, :], CH["Xs"][:, h, :], True, True, [T, CH["Xs"]], [q])
                act(P, CH["VhT"][:], q[0:64, :].rearrange("p (h t) -> p h t", h=8), AF.Copy, [q], [CH["VhT"]])
                yield
                tt(P, "pool", CH["dW"][:], bc(idn[:, None, :], [64, 8, 64]),
                   bc(E1[:, :, c * C + cend:c * C + cend + 1], [64, 8, 64]), ALU.mult, [ident, E1], [CH["dW"]])
                q = bank()
                for h in range(8):
                    mm(P, q[0:64, h * 64:(h + 1) * 64], CH["AhT"][:, h, :], CH["BhT"][:, h, :], True, True,
                       [CH["AhT"], CH["BhT"]], [q])
                tt(P, "dve", CH["Gs"][:], q[0:64, :].rearrange("p (h t) -> p h t", h=8), CH["dW"][:], ALU.add,
                   [q, CH["dW"]], [CH["Gs"]])
                if not is_ctx:
                    q = bank()
                    for h in range(8):
                        mm(P, q[0:64, h * 64:(h + 1) * 64], CH["AhT"][:, h, :], MB[:, h, 64:128], True, True,
                           [CH["AhT"], MB], [q])
                    tt(P, "dve", CH["Qs"][:], q[0:64, :].rearrange("p (h t) -> p h t", h=8), AR[:, :, c, 64:128], ALU.add,
                       [q, AR], [CH["Qs"]])
                    q = bank()
                    for h in range(8):
                        o_ = q[0:64, h * 64:(h + 1) * 64]
                        mm(P, o_, ST[:, h, :], CH["Qs"][:, h, :], True, False, [ST, CH["Qs"]], [q])
                        mm(P, o_, CH["VhT"][:, h, :], MB[:, h, 64:128], False, False, [CH["VhT"], MB], [q])
                        mm(P, o_, CH["VT"][:, h, :], MK[:, h, 64:128], False, True, [CH["VT"], MK], [q])
                    act(P, Yw[:, :, sl], q[0:64, :].rearrange("p (h t) -> p h t", h=8), AF.Copy, [q], [Yw])
                yield
                q = bank()
                for h in range(8):
                    o_ = q[0:64, h * 64:(h + 1) * 64]
                    mm(P, o_, CH["Gs"][:, h, :], ST[:, h, :], True, False, [CH["Gs"], ST], [q])
                    mm(P, o_, CH["BhT"][:, h, :], CH["VhT"][:, h, :], False, False, [CH["BhT"], CH["VhT"]], [q])
                    mm(P, o_, CH["KhT"][:, h, :], CH["VT"][:, h, :], False, True, [CH["KhT"], CH["VT"]], [q])
                act(P, ST[:], q[0:64, :].rearrange("p (h t) -> p h t", h=8), AF.Copy, [q], [ST])
            if not is_ctx:
                yT = dr["yT" + dsuf]
                P.dma("sp", yT[:, t0 - CTX:t0 - CTX + W].rearrange("(h i) t -> i h t", i=64), Yw[:], reads=[Yw], writes=[yT])
            if dbg is not None and wi == (nctx - 1 if dirn == 0 else 0) and "ST" + dsuf in dbg:
                P.dma("sp", dbg["ST" + dsuf][:].rearrange("(h j) i -> j h i", j=64), ST[:], reads=[ST], writes=[dbg["ST" + dsuf]])


            state["chunk"] = wn
            yield

    return feat(), chunk()


def phase_adaln(P, cs, dr):
    out = {}
    for n in ("Ax", "Bx", "Ac", "Bc", "Af", "Bf"):
        out[n] = P.sbuf(n, [128, 8])
    out["G2"] = P.sbuf("G2", [128, 1024])
    out["G5"] = P.sbuf("G5", [128, 1024])
    out["Arow"] = P.sbuf("Arow", [128, 1024])
    out["Brow"] = P.sbuf("Brow", [128, 1024])
    m0 = P.mark()
    ones = cs["ones"]
    S16 = P.sbuf("S16", [128, 16])
    P.dma("sp", S16[:], dr["ccol"][:], writes=[S16])
    act(P, S16[:], S16[:], AF.Silu, [S16], [S16])
    Sbc = P.sbuf("Sbc", [128, 8, 128])
    tt(P, "dve", Sbc[:], S16[:, 0:8, None].broadcast_to([128, 8, 128]), ones[:, None, :].broadcast_to([128, 8, 128]),
       ALU.mult, [S16, ones], [Sbc])
    bada = P.sbuf("bada", [128, 48])
    P.dma("sp", bada[:], dr["bada_col"][:], writes=[bada])
    gcols = P.sbuf("gcols", [128, 16])
    P.dma("sp", gcols[:], dr["gcols"][:], writes=[gcols])
    modT = P.sbuf("modT", [128, 48, 2])
    wst = [P.sbuf("wada0", [128, 8, 1024]), P.sbuf("wada1", [128, 8, 1024])]
    brow = P.sbuf("brow", [128, 1024])
    grow = P.sbuf("grow", [128, 1024])
    pm = P.psum("pm", [128, 512])
    pg = [P.psum("pg0", [128, 512]), P.psum("pg1", [128, 512])]
    wv = dr["w_ada"].t.rearrange("(k p) n -> p k n", p=128)
    for m in range(6):
        wb = wst[m % 2]
        for k in range(8):
            P.dma("sp" if k % 2 == 0 else "act", wb[:, k, :], wv[:, k, m * 1024:(m + 1) * 1024], reads=[dr["w_ada"]], writes=[wb])
        for jj in range(8):
            j = m * 8 + jj
            for k in range(8):
                mm(P, pm[:, 2 * jj:2 * jj + 2], wb[:, k, jj * 128:(jj + 1) * 128], S16[:, k:16:8], k == 0, k == 7, [wb, S16], [pm])
        tt(P, "dve", modT[:, m * 8:(m + 1) * 8, :], pm[:, 0:16].rearrange("p (j c) -> p j c", c=2),
           bada[:, m * 8:(m + 1) * 8, None].broadcast_to([128, 8, 2]), ALU.add, [pm, bada], [modT])
        if m in (2, 3, 4, 5):
            G = out[{2: "G2", 3: "Brow", 4: "Arow", 5: "G5"}[m]]
            gsrc = {2: dr["g_post_mix"], 5: dr["g_post_ffn"], 4: dr["g_pre_ffn"], 3: None}[m]
            P.dma("sp", brow[:], dr["b_ada"][0:1, m * 1024:(m + 1) * 1024].broadcast_to([128, 1024]), reads=[dr["b_ada"]], writes=[brow])
            if gsrc is not None:
                P.dma("sp", grow[:], gsrc[0:1, :].broadcast_to([128, 1024]), reads=[gsrc], writes=[grow])
            for hf in range(2):
                for k in range(8):
                    mm(P, pg[hf][:], Sbc[:, k, :], wb[:, k, hf * 512:(hf + 1) * 512], k == 0, k == 7, [Sbc, wb], [pg[hf]])
                tt(P, "dve", G[:, hf * 512:(hf + 1) * 512], pg[hf][:], brow[:, hf * 512:(hf + 1) * 512], ALU.add, [pg[hf], brow], [G])
            if m == 4:
                P.op("dve", lambda e, G=G: e.scalar_tensor_tensor(out=G[:], in0=G[:], scalar=1.0, in1=grow[:], op0=ALU.add, op1=ALU.mult),
                     reads=[G, grow], writes=[G])
            elif gsrc is not None:
                tt(P, "dve", G[:], G[:], grow[:], ALU.mult, [G, grow], [G])
    for (A, B, gsl, msc, msh, ci) in ((out["Ax"], out["Bx"], slice(0, 8), 1, 0, 0), (out["Ac"], out["Bc"], slice(0, 8), 1, 0, 1),
                                      (out["Af"], out["Bf"], slice(8, 16), 4, 3, 0)):
        P.op("dve", lambda e, A=A, msc=msc, ci=ci, gsl=gsl: e.scalar_tensor_tensor(
            out=A[:], in0=modT[:, msc * 8:(msc + 1) * 8, ci], scalar=1.0, in1=gcols[:, gsl], op0=ALU.add, op1=ALU.mult),
            reads=[modT, gcols], writes=[A])
        P.op("dve", lambda e, B=B, msh=msh, ci=ci: e.tensor_copy(out=B[:], in_=modT[:, msh * 8:(msh + 1) * 8, ci]),
             reads=[modT], writes=[B])
    P.barrier()
    P.release(m0)
    return out


def rms_rstd(P, ss, rstd, n, eps):
    P.op("dve", lambda e: e.tensor_scalar(out=rstd[:], in0=ss[:], scalar1=1.0 / n, scalar2=eps, op0=ALU.mult, op1=ALU.add),
         reads=[ss], writes=[rstd])
    act(P, rstd[:], rstd[:], AF.Sqrt, [rstd], [rstd])
    P.op("dve", lambda e: e.reciprocal(out=rstd[:], in_=rstd[:]), reads=[rstd], writes=[rstd])


NCH_W = 42


def phase_proj(P, cs, mod, dr):
    m0 = P.mark()
    ident = cs["ident"]
    ones = cs["ones"]
    hT = P.sbuf("hT", [128, 8, TT], BF16)
    xt = [P.sbuf("xt0", [128, 1024]), P.sbuf("xt1", [128, 1024])]
    junk = P.sbuf("junk", [128, 1024])
    ss = P.sbuf("ss", [128, 1])
    rstd = P.sbuf("rstd", [128, 1])
    pt = [P.psum("pt%d" % i, [128, 512]) for i in range(4)]
    for ti in range(TT // 128):
        X = xt[ti % 2]
        if ti < 2:
            src, srcb = dr["ctx"][ti * 128:(ti + 1) * 128, :], dr["ctx"]
            A, B = mod["Ac"], mod["Bc"]
        else:
            src, srcb = dr["x"][(ti - 2) * 128:(ti - 1) * 128, :], dr["x"]
            A, B = mod["Ax"], mod["Bx"]
        P.dma("sp", X[:], src, reads=[srcb], writes=[X])
        P.op("act", lambda e, X=X: e.activation(out=junk[:], in_=X[:], func=AF.Square, accum_out=ss[:]), reads=[X], writes=[junk, ss])
        rms_rstd(P, ss, rstd, 1024.0, 1e-6)
        P.op("dve", lambda e, X=X: e.tensor_scalar(out=X[:], in0=X[:], scalar1=rstd[:, 0:1], scalar2=None, op0=ALU.mult),
             reads=[X, rstd], writes=[X])
        for k in range(8):
            q = pt[(ti % 2) * 2 + k // 4]
            tr(P, q[:, (k % 4) * 128:(k % 4 + 1) * 128], X[:, k * 128:(k + 1) * 128], ident[:], [X, ident], [q])
        for k in range(8):
            q = pt[(ti % 2) * 2 + k // 4]
            P.op("dve" if k % 2 else "act", (lambda e, q=q, k=k, A=A, B=B, ti=ti: e.tensor_scalar(
                out=hT[:, k, ti * 128:(ti + 1) * 128], in0=q[:, (k % 4) * 128:(k % 4 + 1) * 128], scalar1=A[:, k:k + 1],
                scalar2=B[:, k:k + 1], op0=ALU.mult, op1=ALU.add)) if k % 2 else (lambda e, q=q, k=k, A=A, B=B, ti=ti: e.activation(
                    out=hT[:, k, ti * 128:(ti + 1) * 128], in_=q[:, (k % 4) * 128:(k % 4 + 1) * 128], func=AF.Identity,
                    scale=A[:, k:k + 1], bias=B[:, k:k + 1])), reads=[q, A, B], writes=[hT])
    bcol = P.sbuf("bcol", [128, NCH_W])
    P.dma("sp", bcol[:], dr["bin_col"][:], writes=[bcol])
    bv = P.sbuf("bv", [1, 128])
    P.dma("sp", bv[:], dr["bv_row"][:], writes=[bv])
    NWS = 5
    wst = [P.sbuf("wst%d" % i, [128, 8, 128]) for i in range(NWS)]
    wbf = [P.sbuf("wbf%d" % i, [128, 8, 128], BF16) for i in range(NWS)]
    cosb = P.sbuf("cosb", [128, 512])
    sinb = P.sbuf("sinb", [128, 512])
    ot = [P.sbuf("ot%d" % i, [128, 512]) for i in range(2)]
    otb = [P.sbuf("otb%d" % i, [128, 512], BF16) for i in range(2)]
    t1 = P.sbuf("rp1", [128, 512])
    pp = [P.psum("pp%d" % i, [128, 512]) for i in range(4)]
    wv = dr["w_in"].t.rearrange("(k p) n -> p k n", p=128)
    groups = [(0, 256)] + [(256 + 512 * g, 512) for g in range(8)]
    wi = [0]
    oi = [0]

    def load_w(j):
        s = wi[0] % NWS
        wi[0] += 1
        P.dma("act", wst[s][:], wv[:, :, j * 128:(j + 1) * 128], reads=[dr["w_in"]], writes=[wst[s]])
        P.op("pool", lambda e: e.tensor_copy(out=wbf[s][:], in_=wst[s][:]), reads=[wst[s]], writes=[wbf[s]])
        return wbf[s]

    def proj(q, wb, t0, n):
        for k in range(8):
            mm(P, q[:, 0:n], wb[:, k, :], hT[:, k, t0:t0 + n], k == 0, k == 7, [wb, hT], [q])

    jobs = list(range(0, 5)) + list(range(6, 37))

    def load_job(j):
        return load_w(j), (load_w(37 + j) if j < 5 else None)

    nxt = load_job(jobs[0])
    for ji, j in enumerate(jobs):
        wb, wr = nxt
        if ji + 1 < len(jobs):
            nxt = load_job(jobs[ji + 1])
        for gi, (t0, n) in enumerate(groups):
            if gi == 0 and not (j == 4 or 6 <= j <= 20):
                continue
            q = pp[oi[0] % 2]
            proj(q, wb, t0, n)
            o = oi[0] % 2
            oi[0] += 1
            if j < 5 and gi > 0:
                q2 = pp[2 + o]
                proj(q2, wr, t0, n)
                xs = t0 - CTX
                P.dma("sp", cosb[:], dr["cos_t"][:, xs:xs + 512], reads=[dr["cos_t"]], writes=[cosb])
                P.dma("sp", sinb[:], dr["sin_t"][:, xs:xs + 512], reads=[dr["sin_t"]], writes=[sinb])
                P.op("dve", lambda e, q=q, j=j: e.scalar_tensor_tensor(out=t1[:], in0=q[:], scalar=bcol[:, j:j + 1], in1=cosb[:],
                                                                      op0=ALU.add, op1=ALU.mult), reads=[q, bcol, cosb], writes=[t1])
                P.op("dve", lambda e, q2=q2, j=j, o=o: e.scalar_tensor_tensor(out=ot[o][:], in0=q2[:], scalar=bcol[:, 37 + j:38 + j], in1=sinb[:],
                                                                             op0=ALU.add, op1=ALU.mult), reads=[q2, bcol, sinb], writes=[ot[o]])
                tt(P, "pool", otb[o][:], ot[o][:], t1[:], ALU.add, [ot[o], t1], [otb[o]])
                if j < 4:
                    P.dma("sp", dr["qT"][j * 128:(j + 1) * 128, xs:xs + 512], otb[o][:], reads=[otb[o]], writes=[dr["qT"]])
                else:
                    P.dma("sp", dr["kT"][:, t0:t0 + 512], otb[o][:], reads=[otb[o]], writes=[dr["kT"]])
            elif j == 4:
                act(P, otb[o][:, 0:n], q[:, 0:n], AF.Identity, [q, bcol], [otb[o]], bias=bcol[:, j:j + 1])
                P.dma("sp", dr["kT"][:, t0:t0 + n], otb[o][:, 0:n], reads=[otb[o]], writes=[dr["kT"]])
            elif j <= 20:
                act(P, ot[o][:, 0:n], q[:, 0:n], AF.Identity, [q, bcol], [ot[o]], bias=bcol[:, j:j + 1])
                P.dma("sp", dr["zrT"][(j - 6) * 128:(j - 5) * 128, t0:t0 + n], ot[o][:, 0:n], reads=[ot[o]], writes=[dr["zrT"]])
            else:
                act(P, otb[o][:], q[:], AF.Sigmoid, [q, bcol], [otb[o]], bias=bcol[:, j:j + 1])
                P.dma("sp", dr["sgT"][(j - 21) * 128:(j - 20) * 128, t0 - CTX:t0 - CTX + 512], otb[o][:], reads=[otb[o]], writes=[dr["sgT"]])
    wb = load_w(5)
    onesb = P.sbuf("onesb", [1, 128], BF16)
    bvb = P.sbuf("bvb", [1, 128], BF16)
    P.op("dve", lambda e: e.tensor_copy(out=onesb[:], in_=ones[0:1, :]), reads=[ones], writes=[onesb])
    P.op("dve", lambda e: e.tensor_copy(out=bvb[:], in_=bv[:]), reads=[bv], writes=[bvb])
    vt = [P.sbuf("vt%d" % i, [128, 128], BF16) for i in range(2)]
    for ti in range(TT // 128):
        q = pp[ti % 4]
        for k in range(8):
            mm(P, q[:, 0:128], hT[:, k, ti * 128:(ti + 1) * 128], wb[:, k, :], k == 0, False, [wb, hT], [q])
        mm(P, q[:, 0:128], onesb[:], bvb[:], False, True, [onesb, bvb], [q])
        V = vt[ti % 2]
        act(P, V[:], q[:, 0:128], AF.Copy, [q], [V])
        P.dma("sp", dr["vtok"][ti * 128:(ti + 1) * 128, :], V[:], reads=[V], writes=[dr["vtok"]])
    P.barrier()
    P.release(m0)


def phase_attn(P, cs, dr):
    m0 = P.mark()
    ones = cs["ones"]
    qT = P.sbuf("qTs", [64, 8, SEQ], BF16)
    kT = P.sbuf("kTs", [64, 2, TT], BF16)
    vt = P.sbuf("vts", [128, TT // 128, 128], BF16)
    for h in range(8):
        P.dma("sp" if h % 2 else "act", qT[:, h, :], dr["qT"][h * 64:(h + 1) * 64, :], reads=[dr["qT"]], writes=[qT])
    for g in range(2):
        P.dma("sp", kT[:, g, :], dr["kT"][g * 64:(g + 1) * 64, :], reads=[dr["kT"]], writes=[kT])
    P.dma("sp", vt[:], dr["vtok"].t.rearrange("(n p) c -> p n c", p=128), reads=[dr["vtok"]], writes=[vt])
    esr = P.sbuf("esr", [1, 1024])
    esb = P.sbuf("esb", [1, 1024], BF16)
    P.dma("sp", esr[:], dr["sink_row"][:], writes=[esr])
    act(P, esb[:], esr[:], AF.Exp, [esr], [esb])
    onesb = P.sbuf("onesb2", [128, 64], BF16)
    P.op("dve", lambda e: e.tensor_copy(out=onesb[:], in_=ones[:, 0:64]), reads=[ones], writes=[onesb])
    mL = P.sbuf("mL", [128, 128], BF16)
    mR = P.sbuf("mR", [128, 128], BF16)
    P.op("dve", lambda e: e.tensor_copy(out=mL[:], in_=cs["Uincl"][:]), reads=[cs["Uincl"]], writes=[mL])
    P.op("dve", lambda e: e.tensor_copy(out=mR[:], in_=cs["Lincl"][:]), reads=[cs["Lincl"]], writes=[mR])
    ps = [P.psum("ps%d" % i, [128, 512]) for i in range(4)]
    po = [P.psum("po%d" % i, [128, 512]) for i in range(2)]
    pd = [P.psum("pd%d" % i, [128, 512]) for i in range(2)]
    pT = [P.sbuf("pT%d" % i, [128, 512], BF16) for i in range(6)]
    rden = P.sbuf("rden", [64, 512])
    ao = [P.sbuf("ao%d" % i, [64, 512], BF16) for i in range(2)]
    si = 0
    for n in range(SEQ // 128):
        blocks = []
        if n > 0:
            blocks.append((2 + n - 1, mL))
        blocks.append((2 + n, None))
        if n < SEQ // 128 - 1:
            blocks.append((2 + n + 1, mR))
        blocks += [(0, None), (1, None)]
        for g in range(2):
            o = po[g]
            d = pd[g]
            rhs_q = qT[:, 4 * g:4 * g + 4, n * 128:(n + 1) * 128]
            for bi, (kb, msk) in enumerate(blocks):
                s = ps[si % 4]
                p = pT[si % 6]
                si += 1
                mm(P, s[:].rearrange("p (h t) -> p h t", h=4), kT[:, g, kb * 128:(kb + 1) * 128], rhs_q, True, True, [kT, qT], [s])
                act(P, p[:], s[:], AF.Exp, [s], [p], scale=0.125)
                if msk is not None:
                    tt(P, "pool", p[:].rearrange("p (h t) -> p h t", h=4), p[:].rearrange("p (h t) -> p h t", h=4),
                       msk[:, None, :].broadcast_to([128, 4, 128]), ALU.mult, [p, msk], [p])
                mm(P, o[0:64, :], vt[:, kb, g * 64:(g + 1) * 64], p[:], bi == 0, bi == len(blocks) - 1, [vt, p], [o])
                mm(P, d[0:64, :], onesb[:], p[:], bi == 0, False, [onesb, p], [d])
            mm(P, d[0:64, :], onesb[0:1, :], esb[0:1, g * 512:(g + 1) * 512], False, True, [onesb, esb], [d])
            P.op("dve", lambda e, d=d: e.reciprocal(out=rden[:], in_=d[0:64, :]), reads=[d], writes=[rden])
            A = ao[g]
            tt(P, "dve", A[:], o[0:64, :], rden[:], ALU.mult, [o, rden], [A])
            P.dma("sp", dr["attT"][g * 256:(g + 1) * 256, n * 128:(n + 1) * 128].rearrange("(h d) t -> d h t", d=64),
                  A[:].rearrange("p (h t) -> p h t", h=4), reads=[A], writes=[dr["attT"]])
    P.barrier()
    P.release(m0)


def phase_rwkv_out(P, cs, R, dr):
    m0 = P.mark()
    ones = cs["ones"]
    pp = R["pp"]
    N = 512

    def prm(n):
        o, w = P64[n]
        return pp[:, o:o + w]
    yf = P.sbuf("yf", [64, 8, N])
    yb = P.sbuf("yb", [64, 8, N])
    bo = P.sbuf("bo", [64, 8, N])
    ga = P.sbuf("ga", [64, 8, N])
    sq = P.sbuf("sq", [64, 8, N])
    ob = P.sbuf("ob", [64, 8, N], BF16)
    pb = [P.psum("pr%d" % i, [128, 512]) for i in range(8)]
    for g in range(SEQ // N):
        ts = slice(g * N, (g + 1) * N)
        for (buf, nm, q) in ((yf, "yT_f", "sp"), (yb, "yT_b", "act"), (bo, "bonusT", "sp"), (ga, "gateT", "act")):
            P.dma(q, buf[:], dr[nm][:, ts].rearrange("(h i) t -> i h t", i=64), reads=[dr[nm]], writes=[buf])
        tt(P, "pool", yf[:], yf[:], yb[:], ALU.add, [yf, yb], [yf])
        for h in range(8):
            mm(P, pb[h][0:64, :], ones[0:64, 0:64], yf[:, h, :], True, True, [ones, yf], [pb[h]])
        for h in range(8):
            P.op("dve", lambda e, h=h: e.scalar_tensor_tensor(out=yf[:, h, :], in0=pb[h][0:64, :], scalar=-1.0 / 64, in1=yf[:, h, :],
                                                             op0=ALU.mult, op1=ALU.add), reads=[pb[h], yf], writes=[yf])
        tt(P, "pool", sq[:], yf[:], yf[:], ALU.mult, [yf], [sq])
        for h in range(8):
            mm(P, pb[h][0:64, :], ones[0:64, 0:64], sq[:, h, :], True, True, [ones, sq], [pb[h]])
        for h in range(8):
            P.op("dve", lambda e, h=h: e.tensor_scalar(out=sq[:, h, :], in0=pb[h][0:64, :], scalar1=1.0 / 64, scalar2=64e-5,
                                                      op0=ALU.mult, op1=ALU.add), reads=[pb[h]], writes=[sq])
        act(P, sq[:], sq[:], AF.Ln, [sq], [sq])
        act(P, sq[:], sq[:], AF.Exp, [sq], [sq], scale=-0.5)
        tt(P, "dve", yf[:], yf[:], sq[:], ALU.mult, [yf, sq], [yf])
        tt(P, "pool", yf[:], yf[:], prm("ln_w")[:, :, None].broadcast_to([64, 8, N]), ALU.mult, [yf, pp], [yf])
        tt(P, "pool", yf[:], yf[:], prm("ln_b")[:, :, None].broadcast_to([64, 8, N]), ALU.add, [yf, pp], [yf])
        tt(P, "dve", yf[:], yf[:], bo[:], ALU.add, [yf, bo], [yf])
        tt(P, "dve", ob[:], yf[:], ga[:], ALU.mult, [yf, ga], [ob])
        P.dma("sp", dr["rwkT"][:, ts].rearrange("(h i) t -> i h t", i=64), ob[:], reads=[ob], writes=[dr["rwkT"]])
    P.barrier()
    P.release(m0)


def phase_merge(P, cs, mod, dr, rt):
    m0 = P.mark()
    ident = cs["ident"]
    ones = cs["ones"]
    N = 512
    wua = P.sbuf("wua", [64, 8, 1024], BF16)
    wur = P.sbuf("wur", [64, 8, 1024], BF16)
    wo = P.sbuf("wo", [128, 8, 1024], BF16)
    stg = P.sbuf("stg", [128, 8, 1024])
    P.dma("sp", stg[0:64, :, :], dr["w_up_att"].t.rearrange("(h d) n -> d h n", d=64), reads=[dr["w_up_att"]], writes=[stg])
    P.op("pool", lambda e: e.tensor_copy(out=wua[:], in_=stg[0:64, :, :]), reads=[stg], writes=[wua])
    P.dma("sp", stg[0:64, :, :], dr["w_up_rwkv"].t.rearrange("(h d) n -> d h n", d=64), reads=[dr["w_up_rwkv"]], writes=[stg])
    P.op("pool", lambda e: e.tensor_copy(out=wur[:], in_=stg[0:64, :, :]), reads=[stg], writes=[wur])
    P.dma("sp", stg[:], dr["w_out"].t.rearrange("(k p) n -> p k n", p=128), reads=[dr["w_out"]], writes=[stg])
    P.op("pool", lambda e: e.tensor_copy(out=wo[:], in_=stg[:]), reads=[stg], writes=[wo])
    wr = P.sbuf("wr", [128, 8, 32])
    P.dma("sp", wr[:], dr["w_router"].t.rearrange("(k p) n -> p k n", p=128), reads=[dr["w_router"]], writes=[wr])
    br = P.sbuf("br", [1, 32])
    P.dma("sp", br[:], dr["b_router"][:], writes=[br])
    aT = P.sbuf("aT", [64, 8, N], BF16)
    rT = P.sbuf("rT", [64, 8, N], BF16)
    sg = P.sbuf("sg", [128, 16, N], BF16)
    mT = P.sbuf("mT", [128, 8, N], BF16)
    m1 = P.sbuf("m1", [128, N])
    m2 = P.sbuf("m2", [128, N])
    xt = P.sbuf("xtd", [128, 1024])
    x1 = P.sbuf("x1d", [128, 1024])
    junk = P.sbuf("junkd", [128, 1024])
    ssa = P.sbuf("ssa", [128, 2])
    ss = P.sbuf("ssd", [128, 1])
    rstd = P.sbuf("rstdd", [128, 1])
    hf32 = P.sbuf("hf32", [128, 8, 128])
    hfrow = P.sbuf("hfrow", [128, 1024], BF16)
    lg = P.sbuf("lg", [128, 32])
    m8 = P.sbuf("m8", [128, 8])
    nmx = P.sbuf("nmx", [128, 1])
    msk = P.sbuf("msk", [128, 32])
    ex = P.sbuf("ex", [128, 32])
    sm = P.sbuf("sm", [128, 1])
    gts = P.sbuf("gts", [32, 128])
    pa = P.psum("pa", [128, 512]); pr = P.psum("prr", [128, 512])
    pm = [P.psum("pmx0", [128, 512]), P.psum("pmx1", [128, 512])]
    ptr = [P.psum("ptr0", [128, 512]), P.psum("ptr1", [128, 512])]
    pl = P.psum("pl", [128, 512]); pg = P.psum("pgt", [128, 512])
    for g in range(SEQ // N):
        ts = slice(g * N, (g + 1) * N)
        P.dma("sp", aT[:], dr["attT"][:, ts].rearrange("(h d) t -> d h t", d=64), reads=[dr["attT"]], writes=[aT])
        P.dma("act", rT[:], dr["rwkT"][:, ts].rearrange("(h d) t -> d h t", d=64), reads=[dr["rwkT"]], writes=[rT])
        P.dma("sp", sg[:], dr["sgT"][:, ts].rearrange("(c p) t -> p c t", p=128), reads=[dr["sgT"]], writes=[sg])
        for dc in range(8):
            for h in range(8):
                mm(P, pa[:], wua[:, h, dc * 128:(dc + 1) * 128], aT[:, h, :], h == 0, h == 7, [wua, aT], [pa])
            for h in range(8):
                mm(P, pr[:], wur[:, h, dc * 128:(dc + 1) * 128], rT[:, h, :], h == 0, h == 7, [wur, rT], [pr])
            tt(P, "dve", m1[:], pa[:], sg[:, dc, :], ALU.mult, [pa, sg], [m1])
            tt(P, "dve", m2[:], pr[:], sg[:, 8 + dc, :], ALU.mult, [pr, sg], [m2])
            tt(P, "pool", mT[:, dc, :], m1[:], m2[:], ALU.add, [m1, m2], [mT])
        for t4 in range(4):
            tok = g * N + t4 * 128
            P.dma("act", xt[:], dr["x"][tok:tok + 128, :], reads=[dr["x"]], writes=[xt])
            for hf in range(2):
                for k in range(8):
                    mm(P, pm[hf][:], mT[:, k, t4 * 128:(t4 + 1) * 128], wo[:, k, hf * 512:(hf + 1) * 512], k == 0, k == 7, [mT, wo], [pm[hf]])
                P.op("act", lambda e, hf=hf: e.activation(out=junk[:, 0:512], in_=pm[hf][:], func=AF.Square, accum_out=ssa[:, hf:hf + 1]),
                     reads=[pm[hf]], writes=[junk, ssa])
            tt(P, "dve", ss[:], ssa[:, 0:1], ssa[:, 1:2], ALU.add, [ssa], [ss])
            rms_rstd(P, ss, rstd, 1024.0, 1e-6)
            for hf in range(2):
                hs = slice(hf * 512, (hf + 1) * 512)
                P.op("dve", lambda e, hf=hf, hs=hs: e.scalar_tensor_tensor(out=x1[:, hs], in0=pm[hf][:], scalar=rstd[:, 0:1], in1=mod["G2"][:, hs],
                                                                          op0=ALU.mult, op1=ALU.mult), reads=[pm[hf], rstd, mod["G2"]], writes=[x1])
            tt(P, "pool", x1[:], x1[:], xt[:], ALU.add, [x1, xt], [x1])
            P.dma("sp", dr["x1"][tok:tok + 128, :], x1[:], reads=[x1], writes=[dr["x1"]])
            P.op("act", lambda e: e.activation(out=junk[:], in_=x1[:], func=AF.Square, accum_out=ss[:]), reads=[x1], writes=[junk, ss])
            rms_rstd(P, ss, rstd, 1024.0, 1e-6)
            P.op("dve", lambda e: e.tensor_scalar(out=xt[:], in0=x1[:], scalar1=rstd[:, 0:1], scalar2=None, op0=ALU.mult),
                 reads=[x1, rstd], writes=[xt])
            for k in range(8):
                tr(P, ptr[k // 4][:, (k % 4) * 128:(k % 4 + 1) * 128], xt[:, k * 128:(k + 1) * 128], ident[:], [xt, ident], [ptr[k // 4]])
            for k in range(8):
                q = ptr[k // 4]
                P.op("dve", lambda e, q=q, k=k: e.tensor_scalar(out=hf32[:, k, :], in0=q[:, (k % 4) * 128:(k % 4 + 1) * 128],
                                                               scalar1=mod["Af"][:, k:k + 1], scalar2=mod["Bf"][:, k:k + 1],
                                                               op0=ALU.mult, op1=ALU.add), reads=[q, mod["Af"], mod["Bf"]], writes=[hf32])
            tt(P, "pool", junk[:], xt[:], mod["Arow"][:], ALU.mult, [xt, mod["Arow"]], [junk])
            tt(P, "pool", hfrow[:], junk[:], mod["Brow"][:], ALU.add, [junk, mod["Brow"]], [hfrow])
            P.dma("sp", dr["hftok"][tok:tok + 128, :], hfrow[:], reads=[hfrow], writes=[dr["hftok"]])
            for k in range(8):
                mm(P, pl[:, 0:32], hf32[:, k, :], wr[:, k, :], k == 0, False, [hf32, wr], [pl])
            mm(P, pl[:, 0:32], ones[0:1, :], br[:], False, True, [ones, br], [pl])
            P.op("dve", lambda e: e.tensor_copy(out=lg[:], in_=pl[:, 0:32]), reads=[pl], writes=[lg])
            P.op("dve", lambda e: e.max(out=m8[:], in_=lg[:]), reads=[lg], writes=[m8])
            P.op("dve", lambda e: e.tensor_scalar(out=nmx[:], in0=m8[:, 0:1], scalar1=-1.0, scalar2=None, op0=ALU.mult), reads=[m8], writes=[nmx])
            P.op("dve", lambda e: e.tensor_scalar(out=msk[:], in0=lg[:], scalar1=m8[:, 3:4], scalar2=None, op0=ALU.is_ge), reads=[lg, m8], writes=[msk])
            act(P, ex[:], lg[:], AF.Exp, [lg, nmx], [ex], bias=nmx[:, 0:1])
            tt(P, "dve", ex[:], ex[:], msk[:], ALU.mult, [ex, msk], [ex])
            P.op("dve", lambda e: e.reduce_sum(out=sm[:], in_=ex[:], axis=mybir.AxisListType.X), reads=[ex], writes=[sm])
            P.op("dve", lambda e: e.reciprocal(out=sm[:], in_=sm[:]), reads=[sm], writes=[sm])
            P.op("dve", lambda e: e.tensor_scalar(out=ex[:], in0=ex[:], scalar1=sm[:, 0:1], scalar2=None, op0=ALU.mult), reads=[ex, sm], writes=[ex])
            ti_ = g * 4 + t4
            P.op("dve", lambda e, ti_=ti_: e.tensor_copy(out=rt["Gall"][:, ti_, :], in_=ex[:]), reads=[ex], writes=[rt["Gall"]])
            P.op("dve", lambda e, ti_=ti_: e.tensor_copy(out=rt["Mall"][:, ti_, :], in_=msk[:]), reads=[msk], writes=[rt["Mall"]])
    P.barrier()
    P.release(m0)


NB = SEQ * 4 // 128 + NEXP
I32 = mybir.dt.int32
BIGI = 1.0e6


def phase_moe(P, cs, mod, rt, dr):
    m0 = P.mark()
    ones, ident = cs["ones"], cs["ident"]
    Mall, Gall = rt["Mall"], rt["Gall"]
    NT = SEQ // 128
    xg, yb = dr["xg"], dr["yb"]
    IDX = P.sbuf("IDX", [128, NB, 8], I32)
    DEST = P.sbuf("DEST", [128, NT, 4], I32)
    GK = P.sbuf("GK", [128, NT, 4])
    m1 = P.mark()
    zt = P.sbuf("zt", [128, 2 * 1024], BF16)
    P.op("pool", lambda e: e.memset(zt[:], 0.0), writes=[zt])
    xgv = xg.t.rearrange("(n p j) d -> n p (j d)", p=128, j=2)
    for n in range(NB * 128 // 256):
        P.dma("sp" if n % 2 else "act", xgv[n], zt[:], reads=[zt], shared=[xg])
    P.fence(xg)
    it = P.sbuf("it", [128, 192], I32)
    itf = P.sbuf("itf", [128, 192])
    P.op("pool", lambda e: e.iota(it[:], pattern=[[128, 192]], base=0, channel_multiplier=0), writes=[it])
    P.op("dve", lambda e: e.tensor_copy(out=itf[:], in_=it[:]), reads=[it], writes=[itf])
    pi = P.sbuf("pi", [128, 1], I32)
    pif = P.sbuf("pif", [128, 1])
    P.op("pool", lambda e: e.iota(pi[:], pattern=[[0, 1]], base=0, channel_multiplier=1), writes=[pi])
    P.op("dve", lambda e: e.tensor_copy(out=pif[:], in_=pi[:]), reads=[pi], writes=[pif])
    pc = P.psum("pc", [128, 512])
    pq = P.psum("pq", [128, 512])
    for i in range(NT):
        mm(P, pc[:, 0:32], ones[:], Mall[:, i, :], i == 0, i == NT - 1, [ones, Mall], [pc])
    cnt = P.sbuf("cnt", [128, 32])
    P.op("dve", lambda e: e.tensor_copy(out=cnt[:], in_=pc[:, 0:32]), reads=[pc], writes=[cnt])
    cmp = P.sbuf("cmp", [128, 32, 32])
    tt(P, "dve", cmp[:], cnt[:, :, None].broadcast_to([128, 32, 32]), itf[:, None, 0:32].broadcast_to([128, 32, 32]),
       ALU.is_gt, [cnt, itf], [cmp])
    padded = P.sbuf("padded", [128, 32])
    P.op("dve", lambda e: e.reduce_sum(out=padded[:], in_=cmp[:], axis=mybir.AxisListType.X), reads=[cmp], writes=[padded])
    P.op("dve", lambda e: e.tensor_scalar(out=padded[:], in0=padded[:], scalar1=128.0, scalar2=None, op0=ALU.mult), reads=[padded], writes=[padded])
    p_end = P.sbuf("p_end", [128, 32])
    P.op("dve", lambda e: e.tensor_tensor_scan(out=p_end[:], data0=ones[:, 0:32], data1=padded[:], initial=0.0, op0=ALU.mult, op1=ALU.add),
         reads=[ones, padded], writes=[p_end])
    base0 = P.sbuf("base0", [128, 32])
    tt(P, "dve", base0[:], p_end[:], padded[:], ALU.subtract, [p_end, padded], [base0])
    ebc = P.sbuf("ebc", [128, NB, 32])
    tt(P, "dve", ebc[:], p_end[:, None, :].broadcast_to([128, NB, 32]), itf[:, 0:NB, None].broadcast_to([128, NB, 32]),
       ALU.is_le, [p_end, itf], [ebc])
    eb = P.sbuf("eb", [128, NB])
    P.op("dve", lambda e: e.reduce_sum(out=eb[:], in_=ebc[:], axis=mybir.AxisListType.X), reads=[ebc], writes=[eb])
    P.op("dve", lambda e: e.tensor_scalar(out=eb[:], in0=eb[:], scalar1=31.0, scalar2=None, op0=ALU.min), reads=[eb], writes=[eb])
    sk = P.sbuf("sk", [128, NB])
    P.op("pool", lambda e: e.memset(sk[:], 0.0), writes=[sk])
    tt(P, "dve", sk[:, 1:NB], eb[:, 1:NB], eb[:, 0:NB - 1], ALU.is_equal, [eb], [sk])
    P.op("dve", lambda e: e.tensor_scalar(out=sk[:], in0=sk[:], scalar1=BIGI, scalar2=None, op0=ALU.mult), reads=[sk], writes=[sk])
    basef = P.sbuf("basef", [128, NB])
    P.op("dve", lambda e: e.tensor_scalar(out=basef[:], in0=eb[:], scalar1=128.0, scalar2=pif[:, 0:1], op0=ALU.mult, op1=ALU.add),
         reads=[eb, pif], writes=[basef])
    idxf = P.sbuf("idxf", [128, NB, 8])
    for pc_ in range(6):
        mul, add = (4.0, float(pc_)) if pc_ < 4 else (2.0, float(pc_ - 4))
        P.op("dve", lambda e, pc_=pc_, mul=mul, add=add: e.tensor_scalar(out=idxf[:, :, pc_], in0=basef[:], scalar1=mul, scalar2=add,
                                                                         op0=ALU.mult, op1=ALU.add), reads=[basef], writes=[idxf])
        tt(P, "dve", idxf[:, :, pc_], idxf[:, :, pc_], sk[:], ALU.add, [idxf, sk], [idxf])
    tt(P, "dve", idxf[:, :, 6], eb[:], sk[:], ALU.add, [eb, sk], [idxf])
    P.op("dve", lambda e: e.tensor_copy(out=IDX[:, :, 0:7], in_=idxf[:, :, 0:7]), reads=[idxf], writes=[IDX])
    DESTf = P.sbuf("DESTf", [128, NT, 4])
    Dt = P.sbuf("Dt", [128, 32])
    Vt = P.sbuf("Vt", [128, 32])
    oh = P.sbuf("oh", [128, 32])
    m8 = P.sbuf("m8s", [128, 8])
    for i in range(NT):
        mm(P, pq[:, 0:32], cs["Lstrict"][:], Mall[:, i, :], True, True, [cs["Lstrict"], Mall], [pq])
        tt(P, "dve", Dt[:], pq[:, 0:32], base0[:], ALU.add, [pq, base0], [Dt])
        mm(P, pc[:, 0:32], ones[:], Mall[:, i, :], True, True, [ones, Mall], [pc])
        tt(P, "dve", base0[:], base0[:], pc[:, 0:32], ALU.add, [base0, pc], [base0])
        P.op("dve", lambda e: e.tensor_scalar(out=Vt[:], in0=Dt[:], scalar1=-1.0, scalar2=32768.0, op0=ALU.mult, op1=ALU.add), reads=[Dt], writes=[Vt])
        tt(P, "dve", Vt[:], Vt[:], Mall[:, i, :], ALU.mult, [Vt, Mall], [Vt])
        P.op("dve", lambda e: e.max(out=m8[:], in_=Vt[:]), reads=[Vt], writes=[m8])
        P.op("dve", lambda e, i=i: e.tensor_scalar(out=DESTf[:, i, :], in0=m8[:, 0:4], scalar1=-1.0, scalar2=32768.0, op0=ALU.mult, op1=ALU.add),
             reads=[m8], writes=[DESTf])
        for k in range(4):
            P.op("dve", lambda e, k=k: e.tensor_scalar(out=oh[:], in0=Vt[:], scalar1=m8[:, k:k + 1], scalar2=None, op0=ALU.is_equal),
                 reads=[Vt, m8], writes=[oh])
            tt(P, "dve", oh[:], oh[:], Gall[:, i, :], ALU.mult, [oh, Gall], [oh])
            P.op("dve", lambda e, i=i, k=k: e.reduce_sum(out=GK[:, i, k:k + 1], in_=oh[:], axis=mybir.AxisListType.X), reads=[oh], writes=[GK])
    P.op("dve", lambda e: e.tensor_copy(out=DEST[:], in_=DESTf[:]), reads=[DESTf], writes=[DEST])
    hr = [P.sbuf("hr%d" % i, [128, 1024], BF16) for i in range(2)]
    for i in range(NT):
        H = hr[i % 2]
        P.dma("sp", H[:], dr["hftok"][i * 128:(i + 1) * 128, :], reads=[dr["hftok"]], writes=[H])
        for k in range(4):
            P.dma("pool", xg.t[:, :], H[:], reads=[H, DEST], shared=[xg], ind=(DEST[:, i, k:k + 1], True, NB * 128 - 1))
    P.barrier()
    P.release(m1)
    m2 = P.mark()
    identb = P.sbuf("identb", [128, 128], BF16)
    P.op("dve", lambda e: e.tensor_copy(out=identb[:], in_=ident[:]), reads=[ident], writes=[identb])
    wgu_p = [P.sbuf("wgu%d" % i, [128, 8, 512], BF16) for i in range(4)]
    wdn_p = [P.sbuf("wdn%d" % i, [128, 8, 512], BF16) for i in range(2)]
    bgu_b = P.sbuf("bgu_b", [128, 2048])
    bdn_b = P.sbuf("bdn_b", [128, 1024])
    NS = 2
    xs = [P.sbuf("xs%d" % i, [128, 1024], BF16) for i in range(3)]
    xgT = [P.sbuf("xgT%d" % i, [128, 8, 128], BF16) for i in range(NS)]
    hgu = [P.sbuf("hgu%d" % i, [128, 2048]) for i in range(NS)]
    gcb = [P.sbuf("gcb%d" % i, [128, 1024]) for i in range(NS)]
    sgb = [P.sbuf("sgb%d" % i, [128, 1024]) for i in range(NS)]
    u1b = [P.sbuf("u1b%d" % i, [128, 1024]) for i in range(NS)]
    actb = [P.sbuf("actb%d" % i, [128, 1024], BF16) for i in range(NS)]
    actT = [P.sbuf("actT%d" % i, [128, 8, 128], BF16) for i in range(NS)]
    ysb = [P.sbuf("ysb%d" % i, [128, 1024]) for i in range(NS)]
    ptx = P.psum("ptx", [128, 1024], BF16)
    pta = P.psum("pta", [128, 1024], BF16)
    pgu = [P.psum("pgu%d" % i, [128, 512]) for i in range(2)]
    pdn = [P.psum("pdn%d" % i, [128, 512]) for i in range(2)]
    wgu2d, wdn2d = dr["wgu2d"], dr["wdn2d"]
    def stage1(b):
        s_ = b % NS
        for ng in range(4):
            P.dma("pool", wgu_p[ng][:].rearrange("p k n -> p (k n)"), wgu2d.t[:, :], reads=[wgu2d, IDX], writes=[wgu_p[ng]],
                  ind=(IDX[:, b, ng:ng + 1], False, NEXP * 128 * 4 - 1))
        P.dma("pool", bgu_b[:], dr["b_gate_up"].t[:, :], reads=[dr["b_gate_up"], IDX], writes=[bgu_b], ind=(IDX[:, b, 6:7], False, NEXP - 1))
        X = xs[b % 3]
        for k in range(8):
            tr(P, ptx[:, k * 128:(k + 1) * 128], X[:, k * 128:(k + 1) * 128], identb[:], [X, identb], [ptx])
        act(P, xgT[s_][:].rearrange("p k t -> p (k t)"), ptx[:], AF.Copy, [ptx], [xgT[s_]])
        H = hgu[s_]
        for ng in range(4):
            q = pgu[ng % 2]
            for k in range(8):
                mm(P, q[:], xgT[s_][:, k, :], wgu_p[ng][:, k, :], k == 0, k == 7, [xgT[s_], wgu_p[ng]], [q])
            tt(P, "dve", H[:, ng * 512:(ng + 1) * 512], q[:], bgu_b[:, ng * 512:(ng + 1) * 512], ALU.add, [q, bgu_b], [H])

    def stage1b(b):
        s_ = b % NS
        H = hgu[s_]
        P.op("dve", lambda e, H=H, s_=s_: e.tensor_scalar(out=gcb[s_][:], in0=H[:, 0:2048:2], scalar1=7.0, scalar2=None, op0=ALU.min),
             reads=[H], writes=[gcb[s_]])
        act(P, sgb[s_][:], gcb[s_][:], AF.Sigmoid, [gcb[s_]], [sgb[s_]], scale=1.702)
        P.op("dve", lambda e, H=H, s_=s_: e.tensor_scalar(out=u1b[s_][:], in0=H[:, 1:2048:2], scalar1=7.0, scalar2=-7.0, op0=ALU.min, op1=ALU.max),
             reads=[H], writes=[u1b[s_]])
        tt(P, "dve", gcb[s_][:], gcb[s_][:], sgb[s_][:], ALU.mult, [gcb[s_], sgb[s_]], [gcb[s_]])
        P.op("dve", lambda e, s_=s_: e.scalar_tensor_tensor(out=actb[s_][:], in0=u1b[s_][:], scalar=1.0, in1=gcb[s_][:], op0=ALU.add, op1=ALU.mult),
             reads=[u1b[s_], gcb[s_]], writes=[actb[s_]])

    def stage2(b):
        s_ = b % NS
        for hf in range(2):
            P.dma("pool", wdn_p[hf][:].rearrange("p k n -> p (k n)"), wdn2d.t[:, :], reads=[wdn2d, IDX], writes=[wdn_p[hf]],
                  ind=(IDX[:, b, 4 + hf:5 + hf], False, NEXP * 128 * 2 - 1))
        P.dma("pool", bdn_b[:], dr["b_down"].t[:, :], reads=[dr["b_down"], IDX], writes=[bdn_b], ind=(IDX[:, b, 6:7], False, NEXP - 1))
        for k in range(8):
            tr(P, pta[:, k * 128:(k + 1) * 128], actb[s_][:, k * 128:(k + 1) * 128], identb[:], [actb[s_], identb], [pta])
        act(P, actT[s_][:].rearrange("p k t -> p (k t)"), pta[:], AF.Copy, [pta], [actT[s_]])
        Y = ysb[s_]
        for hf in range(2):
            q = pdn[hf]
            for k in range(8):
                mm(P, q[:], actT[s_][:, k, :], wdn_p[hf][:, k, :], k == 0, k == 7, [actT[s_], wdn_p[hf]], [q])
            tt(P, "dve", Y[:, hf * 512:(hf + 1) * 512], q[:], bdn_b[:, hf * 512:(hf + 1) * 512], ALU.add, [q, bdn_b], [Y])
        P.dma("sp", yb.t[b * 128:(b + 1) * 128, :], Y[:], reads=[Y], shared=[yb])

    def loadx(b):
        P.dma("sp", xs[b % 3][:], xg.t[b * 128:(b + 1) * 128, :], reads=[xg], writes=[xs[b % 3]])

    loadx(0)
    loadx(1)
    stage1(0)
    stage1b(0)
    for b in range(NB):
        if b + 2 < NB:
            loadx(b + 2)
        if b + 1 < NB:
            stage1(b + 1)
        stage2(b)
        if b + 1 < NB:
            stage1b(b + 1)
    P.barrier()
    P.release(m2)
    yg = [P.sbuf("yg%d" % i, [128, 1024]) for i in range(4)]
    xt = P.sbuf("xte", [128, 1024])
    ot = P.sbuf("ote", [128, 1024])
    ya = P.sbuf("ya", [128, 1024])
    ss = P.sbuf("sse", [128, 1])
    rstd = P.sbuf("rstde", [128, 1])
    for i in range(NT):
        tok = i * 128
        for k in range(4):
            P.dma("pool", yg[k][:], yb.t[:, :], reads=[yb, DEST], writes=[yg[k]], ind=(DEST[:, i, k:k + 1], False, NB * 128 - 1))
        P.dma("act", xt[:], dr["x1"][tok:tok + 128, :], reads=[dr["x1"]], writes=[xt])
        P.op("dve", lambda e, i=i: e.tensor_scalar(out=ya[:], in0=yg[0][:], scalar1=GK[:, i, 0:1], scalar2=None, op0=ALU.mult),
             reads=[yg[0], GK], writes=[ya])
        for k in range(1, 4):
            P.op("dve", lambda e, i=i, k=k: e.scalar_tensor_tensor(out=ya[:], in0=yg[k][:], scalar=GK[:, i, k:k + 1], in1=ya[:], op0=ALU.mult, op1=ALU.add),
                 reads=[yg[k], GK, ya], writes=[ya])
        P.op("act", lambda e: e.activation(out=ot[:], in_=ya[:], func=AF.Square, accum_out=ss[:]), reads=[ya], writes=[ot, ss])
        rms_rstd(P, ss, rstd, 1024.0, 1e-6)
        P.op("dve", lambda e: e.scalar_tensor_tensor(out=ot[:], in0=ya[:], scalar=rstd[:, 0:1], in1=mod["G5"][:], op0=ALU.mult, op1=ALU.mult),
             reads=[ya, rstd, mod["G5"]], writes=[ot])
        tt(P, "pool", ot[:], ot[:], xt[:], ALU.add, [ot, xt], [ot])
        P.dma("sp", dr["out"][tok:tok + 128, :], ot[:], reads=[ot], writes=[dr["out"]])
    P.barrier()
    P.release(m0)


IN_SPECS = [
    ("x", [SEQ, D], F32), ("ctx", [CTX, D], F32), ("ccol", [128, 16], F32), ("w_ada", [D, 6 * D], F32),
    ("b_ada", [1, 6 * D], F32), ("bada_col", [128, 48], F32), ("gcols", [128, 16], F32),
    ("g_post_mix", [1, D], F32), ("g_post_ffn", [1, D], F32), ("g_pre_ffn", [1, D], F32), ("w_in", [D, NCH_W * 128], F32),
    ("bin_col", [128, NCH_W], F32), ("bv_row", [1, 128], F32), ("cos_t", [128, SEQ], F32), ("sin_t", [128, SEQ], F32),
    ("sink_row", [1, 1024], F32), ("pp64", [64, NP64], F32), ("w2_f", [64, 512], F32), ("w2_b", [64, 512], F32),
    ("a2_f", [64, 512], F32), ("a2_b", [64, 512], F32), ("g2", [128, 512], F32), ("mugl", [128, 2], F32),
    ("w_up_att", [512, D], F32), ("w_up_rwkv", [512, D], F32), ("w_out", [D, D], F32), ("w_router", [D, 32], F32),
    ("b_router", [1, 32], F32), ("wgu2d", [NEXP * 128 * 4, 4096], F32), ("wdn2d", [NEXP * 128 * 2, 4096], F32),
    ("b_down", [NEXP, D], F32), ("b_gate_up", [NEXP, 2 * D], F32),
]
SCRATCH = [
    ("zrT", [1920, TT], F32), ("qT", [512, SEQ], BF16), ("kT", [128, TT], BF16), ("vtok", [TT, 128], BF16),
    ("sgT", [2048, SEQ], BF16), ("attT", [512, SEQ], BF16), ("yT_f", [512, SEQ], F32), ("yT_b", [512, SEQ], F32),
    ("bonusT", [512, SEQ], F32), ("gateT", [512, SEQ], F32), ("rwkT", [512, SEQ], BF16), ("x1", [SEQ, D], F32),
    ("hftok", [SEQ, D], BF16), ("xg", [NB * 128, D], BF16), ("yb", [NB * 128, D], F32),
]


def build(debug=False, phases=None, nexp=NEXP):
    nc = bass.Bass("TRN2", target_bir_lowering=False)
    P = Prog(nc)
    dr = {}
    for n, shp, dt in IN_SPECS:
        dr[n] = Buf(n, nc.dram_tensor(n, list(shp), dt, kind="ExternalInput").ap())
    for n, shp, dt in SCRATCH:
        kind = "ExternalOutput" if debug else "Internal"
        dr[n] = Buf(n, nc.dram_tensor(n, list(shp), dt, kind=kind).ap())
    dr["out"] = Buf("out", nc.dram_tensor("out", [SEQ, D], F32, kind="ExternalOutput").ap())
    ph = phases or ("adaln", "proj", "attn", "rwkv", "rwkv_out", "merge", "moe")
    cs = make_consts(P)
    mod = phase_adaln(P, cs, dr)
    if "proj" in ph:
        phase_proj(P, cs, mod, dr)
    if "attn" in ph:
        phase_attn(P, cs, dr)
    mR = P.mark()
    R = rwkv_setup(P, dr)
    if "rwkv" in ph:
        mW = P.mark()
        PB = ([P.psum("bk%d" % i, [128, 512]) for i in range(6)], [P.psum("bb%d" % i, [128, 1024], BF16) for i in range(2)], [0, 0])
        gens = list(rwkv_dir(P, cs, R, 0, dr, PB)) + list(rwkv_dir(P, cs, R, 1, dr, PB))
        while gens:
            for g_ in list(gens):
                try:
                    next(g_)
                except StopIteration:
                    gens.remove(g_)
        P.barrier()
        P.release(mW)
    if "rwkv_out" in ph:
        phase_rwkv_out(P, cs, R, dr)
    P.barrier()
    P.release(mR)
    rt = {"Mall": P.sbuf("Mall", [128, SEQ // 128, 32]), "Gall": P.sbuf("Gall", [128, SEQ // 128, 32])}
    if "merge" in ph:
        phase_merge(P, cs, mod, dr, rt)
    if "moe" in ph:
        phase_moe(P, cs, mod, rt, dr)
    P.barrier()
    P.emit()
    P.close()
    return nc, P


def host_layout(inp):
    f = lambda a: np.ascontiguousarray(np.asarray(a, np.float32))
    col = lambda v: f(np.asarray(v).reshape(-1, 128).T)
    sh = {}
    sh["w_ada"] = f(inp["w_ada"][0]); sh["b_ada"] = f(inp["b_ada"][0][None])
    sh["bada_col"] = col(inp["b_ada"][0])
    sh["gcols"] = f(np.concatenate([col(inp["g_pre_mix"][0]), col(inp["g_pre_ffn"][0])], 1))
    sh["g_post_mix"] = f(inp["g_post_mix"][0][None]); sh["g_post_ffn"] = f(inp["g_post_ffn"][0][None]); sh["g_pre_ffn"] = f(inp["g_pre_ffn"][0][None])
    w_in = np.asarray(inp["w_in"][0], np.float32); b_in = np.asarray(inp["b_in"][0], np.float32)
    d = np.arange(64)
    partner = np.where((d % 32) < 16, d + 16, d - 16)
    qperm = (np.arange(8)[:, None] * 64 + partner[None, :]).reshape(-1)
    kperm = 512 + (np.arange(2)[:, None] * 64 + partner[None, :]).reshape(-1)
    cols = np.concatenate([np.arange(4736), qperm, kperm])
    sh["w_in"] = f(w_in[:, cols])
    sh["bin_col"] = col(b_in[cols])
    sh["bv_row"] = f(b_in[640:768][None])
    half = 32
    inv_freq = (np.float32(10000.0) ** (-np.arange(0, half, 2, dtype=np.float32) / np.float32(half))).astype(np.float32)
    t = np.arange(SEQ)
    row = (t // 64).astype(np.float32); colp = (t % 64).astype(np.float32)
    dd = np.arange(128) % 64
    pos = np.where((dd < 32)[:, None], row[None, :], colp[None, :]).astype(np.float32)
    ang = (pos * inv_freq[dd % 16][:, None]).astype(np.float32)
    sign = np.where((dd % 32) < 16, -1.0, 1.0).astype(np.float32)[:, None]
    sh["cos_t"] = f(np.cos(ang)); sh["sin_t"] = f(np.sin(ang) * sign)
    sh["sink_row"] = f(np.repeat(np.asarray(inp["att_sinks"][0], np.float32), 128)[None])
    sh["pp64"] = pack64(inp)
    for n in ("w2_f", "w2_b", "a2_f", "a2_b", "g2", "w_up_att", "w_up_rwkv", "w_out", "w_router", "b_down", "b_gate_up"):
        sh[n] = f(inp[n][0])
    wgu = np.asarray(inp["w_gate_up"][0], np.float32).reshape(NEXP, 8, 128, 4, 512)
    sh["wgu2d"] = np.ascontiguousarray(wgu.transpose(0, 2, 3, 1, 4)).reshape(NEXP * 128 * 4, 4096)
    wdn = np.asarray(inp["w_down"][0], np.float32).reshape(NEXP, 8, 128, 2, 512)
    sh["wdn2d"] = np.ascontiguousarray(wdn.transpose(0, 2, 3, 1, 4)).reshape(NEXP * 128 * 2, 4096)
    sh["mugl"] = f(np.stack([inp["mu_prev"][0][1792:1920], inp["mu_next"][0][1792:1920]], 1))
    sh["b_router"] = f(inp["b_router"][0][None])
    cc = np.asarray(inp["c_ctx"], np.float32)
    percore = []
    for b in range(inp["x"].shape[0]):
        m = dict(sh)
        m["x"] = f(inp["x"][b]); m["ctx"] = f(inp["ctx"][b])
        m["ccol"] = f(np.concatenate([col(inp["c"][b]), col(cc)], 1))
        percore.append(m)
    return percore


_NC = {}


def kernel(**inputs):
    if "nc" not in _NC:
        _NC["nc"] = build()[0]
    in_maps = host_layout(inputs)
    res = run_bass_kernel_spmd(_NC["nc"], in_maps, core_ids=list(range(len(in_maps))))
    return np.stack([np.asarray(r["out"], np.float32) for r in res.results], 0)
```

```python
import numpy as np
import concourse.bass as bass
import concourse.mybir as mybir
from concourse.bass_utils import run_bass_kernel_spmd

F32 = mybir.dt.float32
BF16 = mybir.dt.bfloat16
AF = mybir.ActivationFunctionType
ALU = mybir.AluOpType

SEQ = 4096
CTX = 256
TT = SEQ + CTX
D = 1024
HD = 64
NH = 8
C = 64
W = 64
NEXP = 32


class Buf:
    __slots__ = ("name", "t", "lw", "rd", "fence")

    def __init__(self, name, t):
        self.name = name
        self.t = t
        self.lw = []
        self.rd = []
        self.fence = []

    def __getitem__(self, idx):
        return self.t[idx]


class Prog:
    ENG = ("pe", "act", "dve", "pool", "sp")

    def __init__(self, nc, n_dma_sems=32):
        self.nc = nc
        self.q = {e: [] for e in self.ENG}
        self.cnt = {}
        self.known = {e: {} for e in self.ENG}
        self.sems = {}
        self.n_dma_sems = n_dma_sems
        self.dma_i = 0
        self.stack = []
        self.ninst = 0
        self.uid = 0
        self.tot = {}
        self.regs = {}

    def enter(self, cm):
        v = cm.__enter__()
        self.stack.append(cm)
        return v

    def mark(self):
        return len(self.stack)

    def release(self, mark):
        while len(self.stack) > mark:
            self.stack.pop().__exit__(None, None, None)

    def sbuf(self, name, shape, dtype=F32):
        self.uid += 1
        t = self.enter(self.nc.sbuf_tensor("%s_%d" % (name, self.uid), list(shape), dtype))
        return Buf(name, t)

    def psum(self, name, shape, dtype=F32):
        self.uid += 1
        t = self.enter(self.nc.psum_tensor("%s_%d" % (name, self.uid), list(shape), dtype))
        return Buf(name, t)

    def dram(self, name, shape, dtype=F32):
        t = self.nc.dram_tensor(name, list(shape), dtype, kind="Internal")
        return Buf(name, t.ap())

    def sem(self, key):
        if key not in self.sems:
            nm = "s_" + "_".join(str(k) for k in (key if isinstance(key, tuple) else (key,)))
            self.sems[key] = self.enter(self.nc.semaphore(nm))
            self.cnt[key] = 0
        return self.sems[key]

    def _deps(self, eng, reads, writes, shared=()):
        deps = {}

        def add(d):
            k, v = d
            if deps.get(k, 0) < v:
                deps[k] = v
        for b in reads:
            for d in b.lw:
                add(d)
            for d in b.fence:
                add(d)
        for b in writes:
            for d in b.lw:
                add(d)
            for d in b.fence:
                add(d)
            for d in b.rd:
                add(d)
        for b in shared:
            for d in b.fence:
                add(d)
            for d in b.rd:
                add(d)
        out = []
        for k, v in deps.items():
            if eng == "pe" and isinstance(k, tuple) and k[0] == "pe":
                continue
            if self.known[eng].get(k, 0) >= v:
                continue
            self.known[eng][k] = v
            out.append((k, v))
        return out

    @staticmethod
    def _compact(lst):
        mx = {}
        for k, v in lst:
            if mx.get(k, 0) < v:
                mx[k] = v
        return list(mx.items())

    def _mark(self, key, val, reads, writes, shared=()):
        for b in reads:
            b.rd.append((key, val))
            if len(b.rd) > 64:
                b.rd = self._compact(b.rd)
        for b in writes:
            b.lw = [(key, val)]
            b.rd = []
            b.fence = []
        for b in shared:
            b.lw.append((key, val))
            if len(b.lw) > 64:
                b.lw = self._compact(b.lw)

    def fence(self, b):
        b.fence = self._compact(b.fence + b.lw)
        b.lw = []

    EPOCH = 4000

    def op(self, eng, fn, reads=(), writes=()):
        n = self.tot.get(eng, 0)
        self.tot[eng] = n + 1
        key = (eng, n // self.EPOCH)
        self.sem(key)
        waits = self._deps(eng, reads, writes)
        self.cnt[key] += 1
        val = self.cnt[key]
        self.q[eng].append(("op", fn, waits, key, 1))
        self._mark(key, val, reads, writes)
        self.ninst += 1

    def dma(self, eng, out, in_, reads=(), writes=(), shared=(), ind=None):
        i = self.dma_i % self.n_dma_sems
        self.dma_i += 1
        key = ("dma", i)
        self.sem(key)
        waits = self._deps(eng, reads, writes, shared)
        prev = self.cnt[key]
        if prev > 0 and self.known[eng].get(key, 0) < prev:
            self.known[eng][key] = prev
            waits.append((key, prev))
        self.cnt[key] += 16
        val = self.cnt[key]
        self.q[eng].append(("dma", (out, in_, ind), waits, key, 16))
        self._mark(key, val, reads, writes, shared)
        self.ninst += 1

    def reg(self, e, val):
        k = (id(e), val)
        if k not in self.regs:
            self.regs[k] = e.to_reg(val)
        return self.regs[k]

    def barrier(self):
        snap = dict(self.cnt)
        for e in self.ENG:
            waits = []
            for k, v in snap.items():
                if v > 0 and self.known[e].get(k, 0) < v:
                    self.known[e][k] = v
                    waits.append((k, v))
            if waits:
                self.q[e].append(("wait", None, waits, None, 0))

    def emit(self):
        nc = self.nc
        block = self.enter(nc.Block())
        engmap = {"pe": "tensor", "act": "scalar", "dve": "vector", "pool": "gpsimd", "sp": "sync"}
        prog = self

        def make(ename):
            items = prog.q[ename]

            def body(e):
                for kind, payload, waits, key, inc in items:
                    for (k, v) in waits:
                        e.wait_ge(prog.sems[k], v)
                    if kind == "op":
                        payload(e).then_inc(prog.sems[key], inc)
                    elif kind == "dma":
                        o, i, ind = payload
                        if ind is None:
                            e.dma_start(out=o, in_=i).then_inc(prog.sems[key], inc)
                        else:
                            idx_ap, on_out, bound = ind
                            off = bass.IndirectOffsetOnAxis(ap=idx_ap, axis=0)
                            try:
                                ins = e.indirect_dma_start(out=o, out_offset=off if on_out else None, in_=i,
                                                           in_offset=None if on_out else off, bounds_check=prog.reg(e, bound),
                                                           oob_is_err=False)
                            except Exception:
                                print("INDIRECT FAIL", o.shape, o.ap, i.shape, i.ap, idx_ap.shape, idx_ap.ap, on_out, bound)
                                raise
                            ins.then_inc(prog.sems[key], inc)
            return body

        for ename in self.ENG:
            if self.q[ename]:
                getattr(block, engmap[ename])(make(ename))

    def close(self):
        self.release(0)


def tt(P, eng, out, i0, i1, op, reads, writes):
    P.op(eng, lambda e: e.tensor_tensor(out=out, in0=i0, in1=i1, op=op), reads=reads, writes=writes)


def act(P, out, in_, func, reads, writes, bias=None, scale=None):
    kw = {}
    if bias is not None:
        kw["bias"] = bias
    if scale is not None:
        kw["scale"] = scale
    P.op("act", lambda e: e.activation(out=out, in_=in_, func=func, **kw), reads=reads, writes=writes)


def mm(P, out, lhsT, rhs, start, stop, reads, writes):
    P.op("pe", lambda e: e.matmul(out, lhsT=lhsT, rhs=rhs, start=start, stop=stop), reads=reads, writes=writes)


def tr(P, out, in_, ident, reads, writes):
    P.op("pe", lambda e: e.transpose(out, in_, ident), reads=reads, writes=writes)


def make_consts(P):
    cs = {}
    ones = P.sbuf("ones128", [128, 128])
    P.op("pool", lambda e: e.memset(ones[:], 1.0), writes=[ones])
    cs["ones"] = ones

    def sel(name, cmp, base, cm, pat):
        b = P.sbuf(name, [128, 128])
        P.op("pool", lambda e: e.affine_select(out=b[:], in_=ones[:], pattern=[[pat, 128]], compare_op=cmp,
                                               fill=P.reg(e, 0.0), base=base, channel_multiplier=cm),
             reads=[ones], writes=[b])
        cs[name] = b
    sel("ident", ALU.is_equal, 0, 1, -1)
    sel("Lstrict", ALU.is_gt, 0, -1, 1)
    sel("Lincl", ALU.is_ge, 0, -1, 1)
    sel("Ustrict", ALU.is_gt, 0, 1, -1)
    sel("Uincl", ALU.is_ge, 0, 1, -1)
    return cs


P64 = {}
_o = 0
for _n, _w in [("mup3", 24), ("mun3", 24), ("mup_wl", 2), ("mun_wl", 2), ("mup_al", 2), ("mun_al", 2),
               ("k_k", 8), ("k_a", 8), ("r_k", 8), ("w0_f", 8), ("w0_b", 8), ("a0_f", 8), ("a0_b", 8),
               ("ln_w", 8), ("ln_b", 8)]:
    P64[_n] = (_o, _w)
    _o += _w
NP64 = _o


def pack64(inp):
    def hj(v):
        return np.ascontiguousarray(np.asarray(v, np.float32).reshape(-1, 64).T)
    mp, mn = inp["mu_prev"][0], inp["mu_next"][0]
    parts = {
        "mup3": hj(mp[0:1536]), "mun3": hj(mn[0:1536]),
        "mup_wl": hj(mp[1536:1664]), "mun_wl": hj(mn[1536:1664]),
        "mup_al": hj(mp[1664:1792]), "mun_al": hj(mn[1664:1792]),
        "k_k": hj(inp["k_k"][0]), "k_a": hj(inp["k_a"][0]), "r_k": hj(inp["r_k"][0].reshape(-1)),
        "w0_f": hj(inp["w0_f"][0]), "w0_b": hj(inp["w0_b"][0]),
        "a0_f": hj(inp["a0_f"][0]), "a0_b": hj(inp["a0_b"][0]),
        "ln_w": hj(inp["ln_x_w"][0]), "ln_b": hj(inp["ln_x_b"][0]),
    }
    out = np.zeros((64, NP64), np.float32)
    for n, (o, w) in P64.items():
        out[:, o:o + w] = parts[n]
    return out


class EW:
    def __init__(self, engines=("dve", "pool")):
        self.e = engines
        self.i = 0

    def __call__(self):
        self.i += 1
        return self.e[self.i % len(self.e)]


def rwkv_setup(P, dr):
    R = {}
    pp = P.sbuf("pp64", [64, NP64])
    P.dma("sp", pp[:], dr["pp64"][:], writes=[pp])
    R["pp"] = pp
    for n in ("w2_f", "w2_b", "a2_f", "a2_b"):
        b = P.sbuf(n, [64, 512])
        P.dma("sp", b[:], dr[n][:], writes=[b])
        R[n] = b
    g2 = P.sbuf("g2", [128, 512])
    P.dma("sp", g2[:], dr["g2"][:], writes=[g2])
    R["g2"] = g2
    mugl = P.sbuf("mugl", [128, 2])
    P.dma("sp", mugl[:], dr["mugl"][:], writes=[mugl])
    R["mugl"] = mugl
    eps18 = P.sbuf("eps18", [64, 1])
    P.op("pool", lambda e: e.memset(eps18[:], 1e-18), writes=[eps18])
    R["eps18"] = eps18
    omka = P.sbuf("omka", [64, 8])
    o, w = P64["k_a"]
    P.op("dve", lambda e: e.tensor_scalar(out=omka[:], in0=pp[:, o:o + w], scalar1=-1.0, scalar2=1.0,
                                          op0=ALU.mult, op1=ALU.add), reads=[pp], writes=[omka])
    R["omka"] = omka
    return R


def rwkv_dir(P, cs, R, dirn, dr, PB, dbg=None, max_win=None):
    nwin = TT // W
    nch = W // C
    nctx = CTX // W
    if dirn == 0:
        order = list(range(nwin))
    else:
        order = list(range(nctx - 1, -1, -1)) + list(range(nwin - 1, nctx - 1, -1))
    if max_win is not None:
        order = order[:max_win]
    pp = R["pp"]
    ident = cs["ident"]
    ones = cs["ones"]
    idn = ident[0:64, 0:64]

    def prm(n):
        o, w = P64[n]
        return pp[:, o:o + w]

    def bc(ap, shape):
        return ap.broadcast_to(shape)

    dsuf = "_f" if dirn == 0 else "_b"
    mAR = P.sbuf("mAR", [64, 128])
    mNT = P.sbuf("mNT", [64, 64])
    st, inc, ntm = ("Lstrict", "Lincl", "Ustrict") if dirn == 0 else ("Ustrict", "Uincl", "Lstrict")
    P.op("dve", lambda e: e.tensor_copy(out=mAR[:, 0:64], in_=cs[st][0:64, 0:64]), reads=[cs[st]], writes=[mAR])
    P.op("dve", lambda e: e.tensor_copy(out=mAR[:, 64:128], in_=cs[inc][0:64, 0:64]), reads=[cs[inc]], writes=[mAR])
    P.op("dve", lambda e: e.tensor_copy(out=mNT[:], in_=cs[ntm][0:64, 0:64]), reads=[cs[ntm]], writes=[mNT])

    ST = P.sbuf("ST", [64, 8, 64], BF16)
    P.op("pool", lambda e: e.memset(ST[:], 0.0), writes=[ST])
    identb = P.sbuf("identb_r", [64, 64], BF16)
    P.op("dve", lambda e: e.tensor_copy(out=identb[:], in_=ident[0:64, 0:64]), reads=[ident], writes=[identb])

    Z3 = P.sbuf("Z3", [64, 24, W + 2])
    ZW = P.sbuf("ZW", [64, 2, W + 2])
    ZA = P.sbuf("ZA", [64, 2, W + 2])
    ZG = P.sbuf("ZG", [128, W + 2])
    zs3 = P.sbuf("zs3", [64, 24, W])
    wls = P.sbuf("wls", [64, 2, W])
    als = P.sbuf("als", [64, 2, W])
    gls = P.sbuf("gls", [128, W])
    tmps = P.sbuf("tmps", [128, W])
    icl = [P.sbuf("icl0", [64, 8, W]), P.sbuf("icl1", [64, 8, W])]
    lw = P.sbuf("lw", [64, 8, W])
    kkn = P.sbuf("kkn", [64, 8, W])
    t8a = P.sbuf("t8a", [64, 8, W])
    t8b = P.sbuf("t8b", [64, 8, W])
    bd = P.sbuf("bd", [64, 8, W])
    kd = [P.sbuf("kd0", [64, 8, W]), P.sbuf("kd1", [64, 8, W])]
    cw = P.sbuf("cw", [64, 8, W])
    E1s = [P.sbuf("E1_%d" % i, [64, 8, W]) for i in range(2)]
    vHs = [P.sbuf("vH_%d" % i, [64, 8, W]) for i in range(2)]
    Einv = P.sbuf("Einv", [64, 8, W])
    Ehat = P.sbuf("Ehat", [64, 8, W])
    ARs = [P.sbuf("AR_%d" % i, [64, 8, nch, 128], BF16) for i in range(2)]
    Bts = [P.sbuf("Bt_%d" % i, [64, 8, W], BF16) for i in range(2)]
    Kts = [P.sbuf("Kt_%d" % i, [64, 8, W], BF16) for i in range(2)]
    Bhs = [P.sbuf("Bh_%d" % i, [64, 8, W], BF16) for i in range(2)]
    Khs = [P.sbuf("Kh_%d" % i, [64, 8, W], BF16) for i in range(2)]
    Yw = P.sbuf("Yw", [64, 8, W])
    CH = {n: P.sbuf(n, [64, 8, 64], BF16) for n in
          ("AtT", "BhT", "KhT", "VT", "X0T", "XA", "XB", "XTA", "XTB", "T", "AhT", "Xs", "VhT", "Gs", "Qs")}
    CH["dW"] = P.sbuf("dW", [64, 8, 64])
    MB = P.sbuf("MB", [64, 8, 128], BF16)
    MK = P.sbuf("MK", [64, 8, 128], BF16)
    banks, bbanks, bki = PB

    def bank():
        bki[0] += 1
        return banks[bki[0] % len(banks)]

    def bbank():
        bki[1] += 1
        return bbanks[bki[1] % len(bbanks)]

    ew = EW()
    S3 = [64, 24, W]
    S8 = [64, 8, W]

    def shift3():
        for q in range(3):
            qs = slice(8 * q, 8 * q + 8)
            ctr, prv, nxt = (slice(None), qs, slice(1, W + 1)), (slice(None), qs, slice(0, W)), (slice(None), qs, slice(2, W + 2))
            mup = prm("mup3")[:, qs, None]
            mun = prm("mun3")[:, qs, None]
            e1 = "dve" if q != 1 else "pool"
            tmp = t8a if q != 1 else t8b
            tt(P, e1, tmp[:], Z3[prv], Z3[ctr], ALU.subtract, [Z3], [tmp])
            tt(P, e1, tmp[:], tmp[:], bc(mup, S8), ALU.mult, [tmp, pp], [tmp])
            tt(P, e1, zs3[:, qs, :], tmp[:], Z3[ctr], ALU.add, [tmp, Z3], [zs3])
            tt(P, e1, tmp[:], Z3[nxt], Z3[ctr], ALU.subtract, [Z3], [tmp])
            tt(P, e1, tmp[:], tmp[:], bc(mun, S8), ALU.mult, [tmp, pp], [tmp])
            tt(P, e1, zs3[:, qs, :], zs3[:, qs, :], tmp[:], ALU.add, [tmp, zs3], [zs3])

    zr = dr["zrT"]
    state = {"feat": -1, "chunk": -1}

    def feat():
        for wn, wi in enumerate(order):
            while state["chunk"] < wn - 2:
                yield
            hs = wn % 2
            AR, Bt, Kt, Bh, Kh, E1, vH = ARs[hs], Bts[hs], Kts[hs], Bhs[hs], Khs[hs], E1s[hs], vHs[hs]
            t0 = wi * W
            is_ctx = t0 < CTX
            lo_d, hi_d = (0, CTX) if is_ctx else (CTX, TT)
            lo = max(t0 - 1, lo_d)
            hi = min(t0 + W + 1, hi_d)
            a = lo - (t0 - 1)
            b = a + (hi - lo)
            for Z in (Z3, ZW, ZA, ZG):
                nd = len(Z.t.shape)
                if a > 0:
                    ix = (slice(None),) * (nd - 1) + (slice(0, 1),)
                    P.op("pool", lambda e, Z=Z, ix=ix: e.memset(Z[ix], 0.0), writes=[Z])
                if b < W + 2:
                    ix = (slice(None),) * (nd - 1) + (slice(W + 1, W + 2),)
                    P.op("pool", lambda e, Z=Z, ix=ix: e.memset(Z[ix], 0.0), writes=[Z])
            P.dma("sp", Z3[:, :, a:b], zr[0:1536, lo:hi].rearrange("(q j) t -> j q t", j=64), reads=[zr], writes=[Z3])
            P.dma("sp", ZW[:, :, a:b], zr[1536:1664, lo:hi].rearrange("(q j) t -> j q t", j=64), reads=[zr], writes=[ZW])
            P.dma("sp", ZA[:, :, a:b], zr[1664:1792, lo:hi].rearrange("(q j) t -> j q t", j=64), reads=[zr], writes=[ZA])
            P.dma("sp", ZG[:, a:b], zr[1792:1920, lo:hi], reads=[zr], writes=[ZG])
            yield
            shift3()
            yield
            for (zraw, zs, mupn, munn) in ((ZW, wls, "mup_wl", "mun_wl"), (ZA, als, "mup_al", "mun_al")):
                tv = t8a[:, 0:2, :]
                shp = [64, 2, W]
                c_, p_, n_ = (slice(None), slice(None), slice(1, W + 1)), (slice(None), slice(None), slice(0, W)), (slice(None), slice(None), slice(2, W + 2))
                tt(P, "dve", tv, zraw[p_], zraw[c_], ALU.subtract, [zraw], [t8a])
                tt(P, "dve", tv, tv, bc(prm(mupn)[:, :, None], shp), ALU.mult, [t8a, pp], [t8a])
                tt(P, "dve", zs[:], tv, zraw[c_], ALU.add, [t8a, zraw], [zs])
                tt(P, "dve", tv, zraw[n_], zraw[c_], ALU.subtract, [zraw], [t8a])
                tt(P, "dve", tv, tv, bc(prm(munn)[:, :, None], shp), ALU.mult, [t8a, pp], [t8a])
                tt(P, "dve", zs[:], zs[:], tv, ALU.add, [t8a, zs], [zs])
            mugl = R["mugl"]
            c2, p2, n2 = (slice(None), slice(1, W + 1)), (slice(None), slice(0, W)), (slice(None), slice(2, W + 2))
            tt(P, "dve", tmps[:], ZG[p2], ZG[c2], ALU.subtract, [ZG], [tmps])
            P.op("dve", lambda e: e.scalar_tensor_tensor(out=gls[:], in0=tmps[:], scalar=mugl[:, 0:1], in1=ZG[c2],
                                                         op0=ALU.mult, op1=ALU.add), reads=[tmps, mugl, ZG], writes=[gls])
            tt(P, "dve", tmps[:], ZG[n2], ZG[c2], ALU.subtract, [ZG], [tmps])
            P.op("dve", lambda e: e.scalar_tensor_tensor(out=gls[:], in0=tmps[:], scalar=mugl[:, 1:2], in1=gls[:],
                                                         op0=ALU.mult, op1=ALU.add), reads=[tmps, mugl, gls], writes=[gls])
            yield
            r_ = zs3[:, 0:8, :]
            k_ = zs3[:, 8:16, :]
            v_ = zs3[:, 16:24, :]
            for d2 in ((0, 1) if (dirn == 0 and not is_ctx) else (dirn,)):
                a2 = R["a2_f" if d2 == 0 else "a2_b"]
                pb = [bank(), bank()]
                for h in range(8):
                    q = pb[h // 4]
                    mm(P, q[0:64, (h % 4) * W:(h % 4 + 1) * W], a2[:, h * 64:(h + 1) * 64], als[:, d2, :], True, True,
                       [a2, als], [q])
                a0 = prm("a0_f" if d2 == 0 else "a0_b")
                for g in range(2):
                    tt(P, "dve", icl[d2][:, 4 * g:4 * g + 4, :], pb[g][0:64, 0:4 * W].rearrange("p (h t) -> p h t", h=4),
                       bc(a0[:, 4 * g:4 * g + 4, None], [64, 4, W]), ALU.add, [pb[g], pp], [icl[d2]])
                act(P, icl[d2][:], icl[d2][:], AF.Sigmoid, [icl[d2]], [icl[d2]])
            yield
            th = tmps[0:64, :]
            act(P, th, wls[:, dirn, :], AF.Tanh, [wls], [tmps])
            w2 = R["w2_f" if dirn == 0 else "w2_b"]
            pb = [bank(), bank()]
            for h in range(8):
                q = pb[h // 4]
                mm(P, q[0:64, (h % 4) * W:(h % 4 + 1) * W], w2[:, h * 64:(h + 1) * 64], th, True, True, [w2, tmps], [q])
            w0 = prm("w0_f" if dirn == 0 else "w0_b")
            for g in range(2):
                tt(P, "dve", lw[:, 4 * g:4 * g + 4, :], pb[g][0:64, 0:4 * W].rearrange("p (h t) -> p h t", h=4),
                   bc(w0[:, 4 * g:4 * g + 4, None], [64, 4, W]), ALU.add, [pb[g], pp], [lw])
            act(P, lw[:], lw[:], AF.Sigmoid, [lw], [lw])
            P.op("dve", lambda e: e.tensor_scalar(out=lw[:], in0=lw[:], scalar1=-float(np.exp(-0.5)), scalar2=None,
                                                  op0=ALU.mult), reads=[lw], writes=[lw])
            yield
            tt(P, "pool", t8a[:], k_, bc(prm("k_k")[:, :, None], S8), ALU.mult, [zs3, pp], [t8a])
            tt(P, "pool", t8b[:], t8a[:], t8a[:], ALU.mult, [t8a], [t8b])
            pb = [bank(), bank()]
            for g in range(2):
                mm(P, pb[g][0:64, 0:4 * W], ones[0:64, 0:64], t8b[:, 4 * g:4 * g + 4, :], True, True, [ones, t8b], [pb[g]])
            for g in range(2):
                act(P, kkn[:, 4 * g:4 * g + 4, :], pb[g][0:64, 0:4 * W].rearrange("p (h t) -> p h t", h=4), AF.Ln, [pb[g]], [kkn], bias=R["eps18"][:, 0:1])
            act(P, kkn[:], kkn[:], AF.Exp, [kkn], [kkn], scale=-0.5)
            tt(P, "dve", kkn[:], kkn[:], t8a[:], ALU.mult, [kkn, t8a], [kkn])
            yield
            dirs_needed = (0, 1) if (dirn == 0 and not is_ctx) else (dirn,)
            for d2 in dirs_needed:
                e1 = ew()
                tt(P, e1, kd[d2][:], icl[d2][:], bc(prm("k_a")[:, :, None], S8), ALU.mult, [icl[d2], pp], [kd[d2]])
                tt(P, e1, kd[d2][:], kd[d2][:], bc(R["omka"][:, :, None], S8), ALU.add, [kd[d2], R["omka"]], [kd[d2]])
                tt(P, e1, kd[d2][:], kd[d2][:], k_, ALU.mult, [kd[d2], zs3], [kd[d2]])
            tt(P, "pool", bd[:], kkn[:], icl[dirn][:], ALU.mult, [kkn, icl[dirn]], [bd])
            if dirn == 0 and not is_ctx:
                tt(P, "pool", t8a[:], kd[0][:], kd[1][:], ALU.add, [kd[0], kd[1]], [t8a])
                tt(P, "pool", t8a[:], t8a[:], r_, ALU.mult, [t8a, zs3], [t8a])
                tt(P, "pool", t8a[:], t8a[:], bc(prm("r_k")[:, :, None], S8), ALU.mult, [t8a, pp], [t8a])
                pb = [bank(), bank()]
                for g in range(2):
                    mm(P, pb[g][0:64, 0:4 * W], ones[0:64, 0:64], t8a[:, 4 * g:4 * g + 4, :], True, True, [ones, t8a], [pb[g]])
                for g in range(2):
                    tt(P, "dve", t8b[:, 4 * g:4 * g + 4, :], pb[g][0:64, 0:4 * W].rearrange("p (h t) -> p h t", h=4),
                       v_[:, 4 * g:4 * g + 4, :], ALU.mult, [pb[g], zs3], [t8b])
                P.dma("sp", dr["bonusT"][:, t0 - CTX:t0 - CTX + W].rearrange("(h i) t -> i h t", i=64), t8b[:],
                      reads=[t8b], writes=[dr["bonusT"]])
                sg = tmps
                act(P, sg[:], gls[:], AF.Sigmoid, [gls], [tmps])
                pb = [bank(), bank()]
                g2 = R["g2"]
                for h in range(8):
                    q = pb[h // 4]
                    mm(P, q[0:64, (h % 4) * W:(h % 4 + 1) * W], g2[:, h * 64:(h + 1) * 64], sg[:], True, True, [g2, tmps], [q])
                for g in range(2):
                    act(P, t8a[:, 4 * g:4 * g + 4, :], pb[g][0:64, 0:4 * W].rearrange("p (h t) -> p h t", h=4), AF.Copy, [pb[g]], [t8a])
                P.dma("sp", dr["gateT"][:, t0 - CTX:t0 - CTX + W].rearrange("(h i) t -> i h t", i=64), t8a[:],
                      reads=[t8a], writes=[dr["gateT"]])
            yield
            for h in range(8):
                for c in range(nch):
                    if dirn == 0:
                        sl = slice(c * C, (c + 1) * C)
                    else:
                        sl = slice(c * C + C - 1, (c * C - 1) if c > 0 else None, -1)
                    P.op("dve", lambda e, h=h, sl=sl: e.tensor_tensor_scan(
                        out=cw[:, h, sl], data0=ones[0:64, 0:64], data1=lw[:, h, sl], initial=0.0,
                        op0=ALU.mult, op1=ALU.add), reads=[lw, ones], writes=[cw])
            yield
            act(P, E1[:], cw[:], AF.Exp, [cw], [E1])
            act(P, Einv[:], cw[:], AF.Exp, [cw], [Einv], scale=-1.0)
            tt(P, "pool", t8a[:], cw[:], lw[:], ALU.subtract, [cw, lw], [t8a])
            act(P, t8a[:], t8a[:], AF.Exp, [t8a], [t8a])
            cend = C - 1 if dirn == 0 else 0
            E1v = E1[:].rearrange("p h (c t) -> p h c t", t=C)
            Wc = E1v[:, :, :, cend:cend + 1]
            tt(P, "pool", Ehat[:].rearrange("p h (c t) -> p h c t", t=C), Einv[:].rearrange("p h (c t) -> p h c t", t=C),
               bc(Wc, [64, 8, nch, C]), ALU.mult, [Einv, E1], [Ehat])
            P.op("dve", lambda e, AR=AR: e.scalar_tensor_tensor(
                out=AR[:, :, :, 0:64], in0=kkn[:].rearrange("p h (c t) -> p h c t", t=C), scalar=-1.0,
                in1=t8a[:].rearrange("p h (c t) -> p h c t", t=C), op0=ALU.mult, op1=ALU.mult),
                reads=[kkn, t8a], writes=[AR])
            tt(P, "pool", AR[:, :, :, 64:128], r_.rearrange("p h (c t) -> p h c t", t=C), E1v, ALU.mult, [zs3, E1], [AR])
            tt(P, "dve", Bt[:], bd[:], Einv[:], ALU.mult, [bd, Einv], [Bt])
            tt(P, "pool", Kt[:], kd[dirn][:], Einv[:], ALU.mult, [kd[dirn], Einv], [Kt])
            tt(P, "dve", Bh[:], bd[:], Ehat[:], ALU.mult, [bd, Ehat], [Bh])
            tt(P, "pool", Kh[:], kd[dirn][:], Ehat[:], ALU.mult, [kd[dirn], Ehat], [Kh])

            act(P, vH[:], zs3[:, 16:24, :], AF.Copy, [zs3], [vH])
            state["feat"] = wn
            yield

    def chunk():
        for wn, wi in enumerate(order):
            while state["feat"] < wn:
                yield
            hs = wn % 2
            AR, Bt, Kt, Bh, Kh, E1, vH = ARs[hs], Bts[hs], Kts[hs], Bhs[hs], Khs[hs], E1s[hs], vHs[hs]
            t0 = wi * W
            is_ctx = t0 < CTX
            cend = C - 1 if dirn == 0 else 0
            corder = range(nch) if dirn == 0 else range(nch - 1, -1, -1)
            for c in corder:
                sl = slice(c * C, (c + 1) * C)
                for (srcb, srcf, dst) in ((AR, lambda h: AR[:, h, c, 0:64], "AtT"), (Bh, lambda h: Bh[:, h, sl], "BhT"),
                                          (Kh, lambda h: Kh[:, h, sl], "KhT")):
                    q = bbank()
                    for h in range(8):
                        tr(P, q[0:64, h * 64:(h + 1) * 64], srcf(h), identb[:], [srcb, identb], [q])
                    act(P, CH[dst][:], q[0:64, 0:512].rearrange("p (h t) -> p h t", h=8), AF.Copy, [q], [CH[dst]])
                q = bank()
                for h in range(8):
                    tr(P, q[0:64, h * 64:(h + 1) * 64], vH[:, h, sl], idn, [vH, ident], [q])
                act(P, CH["VT"][:], q[0:64, :].rearrange("p (h t) -> p h t", h=8), AF.Copy, [q], [CH["VT"]])
                yield
                for (L, dstM) in ((Bt, MB), (Kt, MK)):
                    pb = [bank(), bank()]
                    for h in range(8):
                        q = pb[h // 4]
                        mm(P, q[0:64, (h % 4) * 128:(h % 4 + 1) * 128], L[:, h, sl], AR[:, h, c, :], True, True, [L, AR], [q])
                    for g in range(2):
                        tt(P, "dve", dstM[:, 4 * g:4 * g + 4, :], pb[g][0:64, :].rearrange("p (h t) -> p h t", h=4),
                           bc(mAR[:, None, :], [64, 4, 128]), ALU.mult, [pb[g], mAR], [dstM])
                q = bank()
                for h in range(8):
                    mm(P, q[0:64, h * 64:(h + 1) * 64], AR[:, h, c, 0:64], Bt[:, h, sl], True, True, [AR, Bt], [q])
                X0T = CH["X0T"]
                tt(P, "dve", X0T[:], q[0:64, :].rearrange("p (h t) -> p h t", h=8), bc(mNT[:, None, :], [64, 8, 64]),
                   ALU.mult, [q, mNT], [X0T])
                yield
                q = bank()
                for h in range(8):
                    mm(P, q[0:64, h * 64:(h + 1) * 64], MK[:, h, 0:64], CH["VT"][:, h, :], True, True, [MK, CH["VT"]], [q])
                act(P, CH["Xs"][:], q[0:64, :].rearrange("p (h t) -> p h t", h=8), AF.Copy, [q], [CH["Xs"]])
                yield
                T = CH["T"]
                tt(P, "dve", T[:], MB[:, :, 0:64], bc(idn[:, None, :], [64, 8, 64]), ALU.add, [MB, ident], [T])
                Xc_b, XTc_b = MB, X0T
                Xc = lambda h: MB[:, h, 0:64]
                XTc = lambda h: X0T[:, h, :]
                for k in range(1, 6):
                    Xn_b = CH["XA"] if k % 2 else CH["XB"]
                    XTn_b = CH["XTA"] if k % 2 else CH["XTB"]
                    q1 = bank()
                    for h in range(8):
                        mm(P, q1[0:64, h * 64:(h + 1) * 64], Xc(h), XTc(h), True, True, [Xc_b, XTc_b], [q1])
                    act(P, XTn_b[:], q1[0:64, :].rearrange("p (h t) -> p h t", h=8), AF.Copy, [q1], [XTn_b])
                    if k < 5:
                        q2 = bank()
                        for h in range(8):
                            mm(P, q2[0:64, h * 64:(h + 1) * 64], XTc(h), Xc(h), True, True, [Xc_b, XTc_b], [q2])
                        act(P, Xn_b[:], q2[0:64, :].rearrange("p (h t) -> p h t", h=8), AF.Copy, [q2], [Xn_b])
                    yield
                    q3 = bank()
                    for h in range(8):
                        mm(P, q3[0:64, h * 64:(h + 1) * 64], XTn_b[:, h, :], T[:, h, :], True, True, [XTn_b, T], [q3])
                    tt(P, "dve", T[:], T[:], q3[0:64, :].rearrange("p (h t) -> p h t", h=8), ALU.add, [T, q3], [T])
                    yield
                    Xc_b, XTc_b = Xn_b, XTn_b
                    Xc = lambda h, b_=Xn_b: b_[:, h, :]
                    XTc = lambda h, b_=XTn_b: b_[:, h, :]
                yield
                q = bank()
                for h in range(8):
                    mm(P, q[0:64, h * 64:(h + 1) * 64], T[:, h, :], CH["AtT"][:, h, :], True, True, [T, CH["AtT"]], [q])
                act(P, CH["AhT"][:], q[0:64, :].rearrange("p (h t) -> p h t", h=8), AF.Copy, [q], [CH["AhT"]])
                q = bank()
                for h in range(8):
                    mm(P, q[0:64, h * 64:(h + 1) * 64], T[:, h, :], CH["Xs"][:, h, :], True, True, [T, CH["Xs"]], [q])
                act(P, CH["VhT"][:], q[0:64, :].rearrange("p (h t) -> p h t", h=8), AF.Copy, [q], [CH["VhT"]])
                yield
                tt(P, "pool", CH["dW"][:], bc(idn[:, None, :], [64, 8, 64]),
                   bc(E1[:, :, c * C + cend:c * C + cend + 1], [64, 8, 64]), ALU.mult, [ident, E1], [CH["dW"]])
                q = bank()
                for h in range(8):
                    mm(P, q[0:64, h * 64:(h + 1) * 64], CH["AhT"][:, h, :], CH["BhT"][:, h, :], True, True,
                       [CH["AhT"], CH["BhT"]], [q])
                tt(P, "dve", CH["Gs"][:], q[0:64, :].rearrange("p (h t) -> p h t", h=8), CH["dW"][:], ALU.add,
                   [q, CH["dW"]], [CH["Gs"]])
                if not is_ctx:
                    q = bank()
                    for h in range(8):
                        mm(P, q[0:64, h * 64:(h + 1) * 64], CH["AhT"][:, h, :], MB[:, h, 64:128], True, True,
                           [CH["AhT"], MB], [q])
                    tt(P, "dve", CH["Qs"][:], q[0:64, :].rearrange("p (h t) -> p h t", h=8), AR[:, :, c, 64:128], ALU.add,
                       [q, AR], [CH["Qs"]])
                    q = bank()
                    for h in range(8):
                        o_ = q[0:64, h * 64:(h + 1) * 64]
                        mm(P, o_, ST[:, h, :], CH["Qs"][:, h, :], True, False, [ST, CH["Qs"]], [q])
                        mm(P, o_, CH["VhT"][:, h, :], MB[:, h, 64:128], False, False, [CH["VhT"], MB], [q])
                        mm(P, o_, CH["VT"][:, h, :], MK[:, h, 64:128], False, True, [CH["VT"], MK], [q])
                    act(P, Yw[:, :, sl], q[0:64, :].rearrange("p (h t) -> p h t", h=8), AF.Copy, [q], [Yw])
                yield
                q = bank()
                for h in range(8):
                    o_ = q[0:64, h * 64:(h + 1) * 64]
                    mm(P, o_, CH["Gs"][:, h, :], ST[:, h, :], True, False, [CH["Gs"], ST], [q])
                    mm(P, o_, CH["BhT"][:, h, :], CH["VhT"][:, h, :], False, False, [CH["BhT"], CH["VhT"]], [q])
                    mm(P, o_, CH["KhT"][:, h, :], CH["VT"][:, h, :], False, True, [CH["KhT"], CH["VT"]], [q])
                act(P, ST[:], q[0:64, :].rearrange("p (h t) -> p h t", h=8), AF.Copy, [q], [ST])
            if not is_ctx:
                yT = dr["yT" + dsuf]
                P.dma("sp", yT[:, t0 - CTX:t0 - CTX + W].rearrange("(h i) t -> i h t", i=64), Yw[:], reads=[Yw], writes=[yT])
            if dbg is not None and wi == (nctx - 1 if dirn == 0 else 0) and "ST" + dsuf in dbg:
                P.dma("sp", dbg["ST" + dsuf][:].rearrange("(h j) i -> j h i", j=64), ST[:], reads=[ST], writes=[dbg["ST" + dsuf]])


            state["chunk"] = wn
            yield

    return feat(), chunk()


def phase_adaln(P, cs, dr):
    out = {}
    for n in ("Ax", "Bx", "Ac", "Bc", "Af", "Bf"):
        out[n] = P.sbuf(n, [128, 8])
    out["G2"] = P.sbuf("G2", [128, 1024])
    out["G5"] = P.sbuf("G5", [128, 1024])
    out["Arow"] = P.sbuf("Arow", [128, 1024])
    out["Brow"] = P.sbuf("Brow", [128, 1024])
    m0 = P.mark()
    ones = cs["ones"]
    S16 = P.sbuf("S16", [128, 16])
    P.dma("sp", S16[:], dr["ccol"][:], writes=[S16])
    act(P, S16[:], S16[:], AF.Silu, [S16], [S16])
    Sbc = P.sbuf("Sbc", [128, 8, 128])
    tt(P, "dve", Sbc[:], S16[:, 0:8, None].broadcast_to([128, 8, 128]), ones[:, None, :].broadcast_to([128, 8, 128]),
       ALU.mult, [S16, ones], [Sbc])
    bada = P.sbuf("bada", [128, 48])
    P.dma("sp", bada[:], dr["bada_col"][:], writes=[bada])
    gcols = P.sbuf("gcols", [128, 16])
    P.dma("sp", gcols[:], dr["gcols"][:], writes=[gcols])
    modT = P.sbuf("modT", [128, 48, 2])
    wst = [P.sbuf("wada0", [128, 8, 1024]), P.sbuf("wada1", [128, 8, 1024])]
    brow = P.sbuf("brow", [128, 1024])
    grow = P.sbuf("grow", [128, 1024])
    pm = P.psum("pm", [128, 512])
    pg = [P.psum("pg0", [128, 512]), P.psum("pg1", [128, 512])]
    wv = dr["w_ada"].t.rearrange("(k p) n -> p k n", p=128)
    for m in range(6):
        wb = wst[m % 2]
        for k in range(8):
            P.dma("sp" if k % 2 == 0 else "act", wb[:, k, :], wv[:, k, m * 1024:(m + 1) * 1024], reads=[dr["w_ada"]], writes=[wb])
        for jj in range(8):
            j = m * 8 + jj
            for k in range(8):
                mm(P, pm[:, 2 * jj:2 * jj + 2], wb[:, k, jj * 128:(jj + 1) * 128], S16[:, k:16:8], k == 0, k == 7, [wb, S16], [pm])
        tt(P, "dve", modT[:, m * 8:(m + 1) * 8, :], pm[:, 0:16].rearrange("p (j c) -> p j c", c=2),
           bada[:, m * 8:(m + 1) * 8, None].broadcast_to([128, 8, 2]), ALU.add, [pm, bada], [modT])
        if m in (2, 3, 4, 5):
            G = out[{2: "G2", 3: "Brow", 4: "Arow", 5: "G5"}[m]]
            gsrc = {2: dr["g_post_mix"], 5: dr["g_post_ffn"], 4: dr["g_pre_ffn"], 3: None}[m]
            P.dma("sp", brow[:], dr["b_ada"][0:1, m * 1024:(m + 1) * 1024].broadcast_to([128, 1024]), reads=[dr["b_ada"]], writes=[brow])
            if gsrc is not None:
                P.dma("sp", grow[:], gsrc[0:1, :].broadcast_to([128, 1024]), reads=[gsrc], writes=[grow])
            for hf in range(2):
                for k in range(8):
                    mm(P, pg[hf][:], Sbc[:, k, :], wb[:, k, hf * 512:(hf + 1) * 512], k == 0, k == 7, [Sbc, wb], [pg[hf]])
                tt(P, "dve", G[:, hf * 512:(hf + 1) * 512], pg[hf][:], brow[:, hf * 512:(hf + 1) * 512], ALU.add, [pg[hf], brow], [G])
            if m == 4:
                P.op("dve", lambda e, G=G: e.scalar_tensor_tensor(out=G[:], in0=G[:], scalar=1.0, in1=grow[:], op0=ALU.add, op1=ALU.mult),
                     reads=[G, grow], writes=[G])
            elif gsrc is not None:
                tt(P, "dve", G[:], G[:], grow[:], ALU.mult, [G, grow], [G])
    for (A, B, gsl, msc, msh, ci) in ((out["Ax"], out["Bx"], slice(0, 8), 1, 0, 0), (out["Ac"], out["Bc"], slice(0, 8), 1, 0, 1),
                                      (out["Af"], out["Bf"], slice(8, 16), 4, 3, 0)):
        P.op("dve", lambda e, A=A, msc=msc, ci=ci, gsl=gsl: e.scalar_tensor_tensor(
            out=A[:], in0=modT[:, msc * 8:(msc + 1) * 8, ci], scalar=1.0, in1=gcols[:, gsl], op0=ALU.add, op1=ALU.mult),
            reads=[modT, gcols], writes=[A])
        P.op("dve", lambda e, B=B, msh=msh, ci=ci: e.tensor_copy(out=B[:], in_=modT[:, msh * 8:(msh + 1) * 8, ci]),
             reads=[modT], writes=[B])
    P.barrier()
    P.release(m0)
    return out


def rms_rstd(P, ss, rstd, n, eps):
    P.op("dve", lambda e: e.tensor_scalar(out=rstd[:], in0=ss[:], scalar1=1.0 / n, scalar2=eps, op0=ALU.mult, op1=ALU.add),
         reads=[ss], writes=[rstd])
    act(P, rstd[:], rstd[:], AF.Sqrt, [rstd], [rstd])
    P.op("dve", lambda e: e.reciprocal(out=rstd[:], in_=rstd[:]), reads=[rstd], writes=[rstd])


NCH_W = 42


def phase_proj(P, cs, mod, dr):
    m0 = P.mark()
    ident = cs["ident"]
    ones = cs["ones"]
    hT = P.sbuf("hT", [128, 8, TT], BF16)
    xt = [P.sbuf("xt0", [128, 1024]), P.sbuf("xt1", [128, 1024])]
    junk = P.sbuf("junk", [128, 1024])
    ss = P.sbuf("ss", [128, 1])
    rstd = P.sbuf("rstd", [128, 1])
    pt = [P.psum("pt%d" % i, [128, 512]) for i in range(4)]
    for ti in range(TT // 128):
        X = xt[ti % 2]
        if ti < 2:
            src, srcb = dr["ctx"][ti * 128:(ti + 1) * 128, :], dr["ctx"]
            A, B = mod["Ac"], mod["Bc"]
        else:
            src, srcb = dr["x"][(ti - 2) * 128:(ti - 1) * 128, :], dr["x"]
            A, B = mod["Ax"], mod["Bx"]
        P.dma("sp", X[:], src, reads=[srcb], writes=[X])
        P.op("act", lambda e, X=X: e.activation(out=junk[:], in_=X[:], func=AF.Square, accum_out=ss[:]), reads=[X], writes=[junk, ss])
        rms_rstd(P, ss, rstd, 1024.0, 1e-6)
        P.op("dve", lambda e, X=X: e.tensor_scalar(out=X[:], in0=X[:], scalar1=rstd[:, 0:1], scalar2=None, op0=ALU.mult),
             reads=[X, rstd], writes=[X])
        for k in range(8):
            q = pt[(ti % 2) * 2 + k // 4]
            tr(P, q[:, (k % 4) * 128:(k % 4 + 1) * 128], X[:, k * 128:(k + 1) * 128], ident[:], [X, ident], [q])
        for k in range(8):
            q = pt[(ti % 2) * 2 + k // 4]
            P.op("dve" if k % 2 else "act", (lambda e, q=q, k=k, A=A, B=B, ti=ti: e.tensor_scalar(
                out=hT[:, k, ti * 128:(ti + 1) * 128], in0=q[:, (k % 4) * 128:(k % 4 + 1) * 128], scalar1=A[:, k:k + 1],
                scalar2=B[:, k:k + 1], op0=ALU.mult, op1=ALU.add)) if k % 2 else (lambda e, q=q, k=k, A=A, B=B, ti=ti: e.activation(
                    out=hT[:, k, ti * 128:(ti + 1) * 128], in_=q[:, (k % 4) * 128:(k % 4 + 1) * 128], func=AF.Identity,
                    scale=A[:, k:k + 1], bias=B[:, k:k + 1])), reads=[q, A, B], writes=[hT])
    bcol = P.sbuf("bcol", [128, NCH_W])
    P.dma("sp", bcol[:], dr["bin_col"][:], writes=[bcol])
    bv = P.sbuf("bv", [1, 128])
    P.dma("sp", bv[:], dr["bv_row"][:], writes=[bv])
    NWS = 5
    wst = [P.sbuf("wst%d" % i, [128, 8, 128]) for i in range(NWS)]
    wbf = [P.sbuf("wbf%d" % i, [128, 8, 128], BF16) for i in range(NWS)]
    cosb = P.sbuf("cosb", [128, 512])
    sinb = P.sbuf("sinb", [128, 512])
    ot = [P.sbuf("ot%d" % i, [128, 512]) for i in range(2)]
    otb = [P.sbuf("otb%d" % i, [128, 512], BF16) for i in range(2)]
    t1 = P.sbuf("rp1", [128, 512])
    pp = [P.psum("pp%d" % i, [128, 512]) for i in range(4)]
    wv = dr["w_in"].t.rearrange("(k p) n -> p k n", p=128)
    groups = [(0, 256)] + [(256 + 512 * g, 512) for g in range(8)]
    wi = [0]
    oi = [0]

    def load_w(j):
        s = wi[0] % NWS
        wi[0] += 1
        P.dma("act", wst[s][:], wv[:, :, j * 128:(j + 1) * 128], reads=[dr["w_in"]], writes=[wst[s]])
        P.op("pool", lambda e: e.tensor_copy(out=wbf[s][:], in_=wst[s][:]), reads=[wst[s]], writes=[wbf[s]])
        return wbf[s]

    def proj(q, wb, t0, n):
        for k in range(8):
            mm(P, q[:, 0:n], wb[:, k, :], hT[:, k, t0:t0 + n], k == 0, k == 7, [wb, hT], [q])

    jobs = list(range(0, 5)) + list(range(6, 37))

    def load_job(j):
        return load_w(j), (load_w(37 + j) if j < 5 else None)

    nxt = load_job(jobs[0])
    for ji, j in enumerate(jobs):
        wb, wr = nxt
        if ji + 1 < len(jobs):
            nxt = load_job(jobs[ji + 1])
        for gi, (t0, n) in enumerate(groups):
            if gi == 0 and not (j == 4 or 6 <= j <= 20):
                continue
            q = pp[oi[0] % 2]
            proj(q, wb, t0, n)
            o = oi[0] % 2
            oi[0] += 1
            if j < 5 and gi > 0:
                q2 = pp[2 + o]
                proj(q2, wr, t0, n)
                xs = t0 - CTX
                P.dma("sp", cosb[:], dr["cos_t"][:, xs:xs + 512], reads=[dr["cos_t"]], writes=[cosb])
                P.dma("sp", sinb[:], dr["sin_t"][:, xs:xs + 512], reads=[dr["sin_t"]], writes=[sinb])
                P.op("dve", lambda e, q=q, j=j: e.scalar_tensor_tensor(out=t1[:], in0=q[:], scalar=bcol[:, j:j + 1], in1=cosb[:],
                                                                      op0=ALU.add, op1=ALU.mult), reads=[q, bcol, cosb], writes=[t1])
                P.op("dve", lambda e, q2=q2, j=j, o=o: e.scalar_tensor_tensor(out=ot[o][:], in0=q2[:], scalar=bcol[:, 37 + j:38 + j], in1=sinb[:],
                                                                             op0=ALU.add, op1=ALU.mult), reads=[q2, bcol, sinb], writes=[ot[o]])
                tt(P, "pool", otb[o][:], ot[o][:], t1[:], ALU.add, [ot[o], t1], [otb[o]])
                if j < 4:
                    P.dma("sp", dr["qT"][j * 128:(j + 1) * 128, xs:xs + 512], otb[o][:], reads=[otb[o]], writes=[dr["qT"]])
                else:
                    P.dma("sp", dr["kT"][:, t0:t0 + 512], otb[o][:], reads=[otb[o]], writes=[dr["kT"]])
            elif j == 4:
                act(P, otb[o][:, 0:n], q[:, 0:n], AF.Identity, [q, bcol], [otb[o]], bias=bcol[:, j:j + 1])
                P.dma("sp", dr["kT"][:, t0:t0 + n], otb[o][:, 0:n], reads=[otb[o]], writes=[dr["kT"]])
            elif j <= 20:
                act(P, ot[o][:, 0:n], q[:, 0:n], AF.Identity, [q, bcol], [ot[o]], bias=bcol[:, j:j + 1])
                P.dma("sp", dr["zrT"][(j - 6) * 128:(j - 5) * 128, t0:t0 + n], ot[o][:, 0:n], reads=[ot[o]], writes=[dr["zrT"]])
            else:
                act(P, otb[o][:], q[:], AF.Sigmoid, [q, bcol], [otb[o]], bias=bcol[:, j:j + 1])
                P.dma("sp", dr["sgT"][(j - 21) * 128:(j - 20) * 128, t0 - CTX:t0 - CTX + 512], otb[o][:], reads=[otb[o]], writes=[dr["sgT"]])
    wb = load_w(5)
    onesb = P.sbuf("onesb", [1, 128], BF16)
    bvb = P.sbuf("bvb", [1, 128], BF16)
    P.op("dve", lambda e: e.tensor_copy(out=onesb[:], in_=ones[0:1, :]), reads=[ones], writes=[onesb])
    P.op("dve", lambda e: e.tensor_copy(out=bvb[:], in_=bv[:]), reads=[bv], writes=[bvb])
    vt = [P.sbuf("vt%d" % i, [128, 128], BF16) for i in range(2)]
    for ti in range(TT // 128):
        q = pp[ti % 4]
        for k in range(8):
            mm(P, q[:, 0:128], hT[:, k, ti * 128:(ti + 1) * 128], wb[:, k, :], k == 0, False, [wb, hT], [q])
        mm(P, q[:, 0:128], onesb[:], bvb[:], False, True, [onesb, bvb], [q])
        V = vt[ti % 2]
        act(P, V[:], q[:, 0:128], AF.Copy, [q], [V])
        P.dma("sp", dr["vtok"][ti * 128:(ti + 1) * 128, :], V[:], reads=[V], writes=[dr["vtok"]])
    P.barrier()
    P.release(m0)


def phase_attn(P, cs, dr):
    m0 = P.mark()
    ones = cs["ones"]
    qT = P.sbuf("qTs", [64, 8, SEQ], BF16)
    kT = P.sbuf("kTs", [64, 2, TT], BF16)
    vt = P.sbuf("vts", [128, TT // 128, 128], BF16)
    for h in range(8):
        P.dma("sp" if h % 2 else "act", qT[:, h, :], dr["qT"][h * 64:(h + 1) * 64, :], reads=[dr["qT"]], writes=[qT])
    for g in range(2):
        P.dma("sp", kT[:, g, :], dr["kT"][g * 64:(g + 1) * 64, :], reads=[dr["kT"]], writes=[kT])
    P.dma("sp", vt[:], dr["vtok"].t.rearrange("(n p) c -> p n c", p=128), reads=[dr["vtok"]], writes=[vt])
    esr = P.sbuf("esr", [1, 1024])
    esb = P.sbuf("esb", [1, 1024], BF16)
    P.dma("sp", esr[:], dr["sink_row"][:], writes=[esr])
    act(P, esb[:], esr[:], AF.Exp, [esr], [esb])
    onesb = P.sbuf("onesb2", [128, 64], BF16)
    P.op("dve", lambda e: e.tensor_copy(out=onesb[:], in_=ones[:, 0:64]), reads=[ones], writes=[onesb])
    mL = P.sbuf("mL", [128, 128], BF16)
    mR = P.sbuf("mR", [128, 128], BF16)
    P.op("dve", lambda e: e.tensor_copy(out=mL[:], in_=cs["Uincl"][:]), reads=[cs["Uincl"]], writes=[mL])
    P.op("dve", lambda e: e.tensor_copy(out=mR[:], in_=cs["Lincl"][:]), reads=[cs["Lincl"]], writes=[mR])
    ps = [P.psum("ps%d" % i, [128, 512]) for i in range(4)]
    po = [P.psum("po%d" % i, [128, 512]) for i in range(2)]
    pd = [P.psum("pd%d" % i, [128, 512]) for i in range(2)]
    pT = [P.sbuf("pT%d" % i, [128, 512], BF16) for i in range(6)]
    rden = P.sbuf("rden", [64, 512])
    ao = [P.sbuf("ao%d" % i, [64, 512], BF16) for i in range(2)]
    si = 0
    for n in range(SEQ // 128):
        blocks = []
        if n > 0:
            blocks.append((2 + n - 1, mL))
        blocks.append((2 + n, None))
        if n < SEQ // 128 - 1:
            blocks.append((2 + n + 1, mR))
        blocks += [(0, None), (1, None)]
        for g in range(2):
            o = po[g]
            d = pd[g]
            rhs_q = qT[:, 4 * g:4 * g + 4, n * 128:(n + 1) * 128]
            for bi, (kb, msk) in enumerate(blocks):
                s = ps[si % 4]
                p = pT[si % 6]
                si += 1
                mm(P, s[:].rearrange("p (h t) -> p h t", h=4), kT[:, g, kb * 128:(kb + 1) * 128], rhs_q, True, True, [kT, qT], [s])
                act(P, p[:], s[:], AF.Exp, [s], [p], scale=0.125)
                if msk is not None:
                    tt(P, "pool", p[:].rearrange("p (h t) -> p h t", h=4), p[:].rearrange("p (h t) -> p h t", h=4),
                       msk[:, None, :].broadcast_to([128, 4, 128]), ALU.mult, [p, msk], [p])
                mm(P, o[0:64, :], vt[:, kb, g * 64:(g + 1) * 64], p[:], bi == 0, bi == len(blocks) - 1, [vt, p], [o])
                mm(P, d[0:64, :], onesb[:], p[:], bi == 0, False, [onesb, p], [d])
            mm(P, d[0:64, :], onesb[0:1, :], esb[0:1, g * 512:(g + 1) * 512], False, True, [onesb, esb], [d])
            P.op("dve", lambda e, d=d: e.reciprocal(out=rden[:], in_=d[0:64, :]), reads=[d], writes=[rden])
            A = ao[g]
            tt(P, "dve", A[:], o[0:64, :], rden[:], ALU.mult, [o, rden], [A])
            P.dma("sp", dr["attT"][g * 256:(g + 1) * 256, n * 128:(n + 1) * 128].rearrange("(h d) t -> d h t", d=64),
                  A[:].rearrange("p (h t) -> p h t", h=4), reads=[A], writes=[dr["attT"]])
    P.barrier()
    P.release(m0)


def phase_rwkv_out(P, cs, R, dr):
    m0 = P.mark()
    ones = cs["ones"]
    pp = R["pp"]
    N = 512

    def prm(n):
        o, w = P64[n]
        return pp[:, o:o + w]
    yf = P.sbuf("yf", [64, 8, N])
    yb = P.sbuf("yb", [64, 8, N])
    bo = P.sbuf("bo", [64, 8, N])
    ga = P.sbuf("ga", [64, 8, N])
    sq = P.sbuf("sq", [64, 8, N])
    ob = P.sbuf("ob", [64, 8, N], BF16)
    pb = [P.psum("pr%d" % i, [128, 512]) for i in range(8)]
    for g in range(SEQ // N):
        ts = slice(g * N, (g + 1) * N)
        for (buf, nm, q) in ((yf, "yT_f", "sp"), (yb, "yT_b", "act"), (bo, "bonusT", "sp"), (ga, "gateT", "act")):
            P.dma(q, buf[:], dr[nm][:, ts].rearrange("(h i) t -> i h t", i=64), reads=[dr[nm]], writes=[buf])
        tt(P, "pool", yf[:], yf[:], yb[:], ALU.add, [yf, yb], [yf])
        for h in range(8):
            mm(P, pb[h][0:64, :], ones[0:64, 0:64], yf[:, h, :], True, True, [ones, yf], [pb[h]])
        for h in range(8):
            P.op("dve", lambda e, h=h: e.scalar_tensor_tensor(out=yf[:, h, :], in0=pb[h][0:64, :], scalar=-1.0 / 64, in1=yf[:, h, :],
                                                             op0=ALU.mult, op1=ALU.add), reads=[pb[h], yf], writes=[yf])
        tt(P, "pool", sq[:], yf[:], yf[:], ALU.mult, [yf], [sq])
        for h in range(8):
            mm(P, pb[h][0:64, :], ones[0:64, 0:64], sq[:, h, :], True, True, [ones, sq], [pb[h]])
        for h in range(8):
            P.op("dve", lambda e, h=h: e.tensor_scalar(out=sq[:, h, :], in0=pb[h][0:64, :], scalar1=1.0 / 64, scalar2=64e-5,
                                                      op0=ALU.mult, op1=ALU.add), reads=[pb[h]], writes=[sq])
        act(P, sq[:], sq[:], AF.Ln, [sq], [sq])
        act(P, sq[:], sq[:], AF.Exp, [sq], [sq], scale=-0.5)
        tt(P, "dve", yf[:], yf[:], sq[:], ALU.mult, [yf, sq], [yf])
        tt(P, "pool", yf[:], yf[:], prm("ln_w")[:, :, None].broadcast_to([64, 8, N]), ALU.mult, [yf, pp], [yf])
        tt(P, "pool", yf[:], yf[:], prm("ln_b")[:, :, None].broadcast_to([64, 8, N]), ALU.add, [yf, pp], [yf])
        tt(P, "dve", yf[:], yf[:], bo[:], ALU.add, [yf, bo], [yf])
        tt(P, "dve", ob[:], yf[:], ga[:], ALU.mult, [yf, ga], [ob])
        P.dma("sp", dr["rwkT"][:, ts].rearrange("(h i) t -> i h t", i=64), ob[:], reads=[ob], writes=[dr["rwkT"]])
    P.barrier()
    P.release(m0)


def phase_merge(P, cs, mod, dr, rt):
    m0 = P.mark()
    ident = cs["ident"]
    ones = cs["ones"]
    N = 512
    wua = P.sbuf("wua", [64, 8, 1024], BF16)
    wur = P.sbuf("wur", [64, 8, 1024], BF16)
    wo = P.sbuf("wo", [128, 8, 1024], BF16)
    stg = P.sbuf("stg", [128, 8, 1024])
    P.dma("sp", stg[0:64, :, :], dr["w_up_att"].t.rearrange("(h d) n -> d h n", d=64), reads=[dr["w_up_att"]], writes=[stg])
    P.op("pool", lambda e: e.tensor_copy(out=wua[:], in_=stg[0:64, :, :]), reads=[stg], writes=[wua])
    P.dma("sp", stg[0:64, :, :], dr["w_up_rwkv"].t.rearrange("(h d) n -> d h n", d=64), reads=[dr["w_up_rwkv"]], writes=[stg])
    P.op("pool", lambda e: e.tensor_copy(out=wur[:], in_=stg[0:64, :, :]), reads=[stg], writes=[wur])
    P.dma("sp", stg[:], dr["w_out"].t.rearrange("(k p) n -> p k n", p=128), reads=[dr["w_out"]], writes=[stg])
    P.op("pool", lambda e: e.tensor_copy(out=wo[:], in_=stg[:]), reads=[stg], writes=[wo])
    wr = P.sbuf("wr", [128, 8, 32])
    P.dma("sp", wr[:], dr["w_router"].t.rearrange("(k p) n -> p k n", p=128), reads=[dr["w_router"]], writes=[wr])
    br = P.sbuf("br", [1, 32])
    P.dma("sp", br[:], dr["b_router"][:], writes=[br])
    aT = P.sbuf("aT", [64, 8, N], BF16)
    rT = P.sbuf("rT", [64, 8, N], BF16)
    sg = P.sbuf("sg", [128, 16, N], BF16)
    mT = P.sbuf("mT", [128, 8, N], BF16)
    m1 = P.sbuf("m1", [128, N])
    m2 = P.sbuf("m2", [128, N])
    xt = P.sbuf("xtd", [128, 1024])
    x1 = P.sbuf("x1d", [128, 1024])
    junk = P.sbuf("junkd", [128, 1024])
    ssa = P.sbuf("ssa", [128, 2])
    ss = P.sbuf("ssd", [128, 1])
    rstd = P.sbuf("rstdd", [128, 1])
    hf32 = P.sbuf("hf32", [128, 8, 128])
    hfrow = P.sbuf("hfrow", [128, 1024], BF16)
    lg = P.sbuf("lg", [128, 32])
    m8 = P.sbuf("m8", [128, 8])
    nmx = P.sbuf("nmx", [128, 1])
    msk = P.sbuf("msk", [128, 32])
    ex = P.sbuf("ex", [128, 32])
    sm = P.sbuf("sm", [128, 1])
    gts = P.sbuf("gts", [32, 128])
    pa = P.psum("pa", [128, 512]); pr = P.psum("prr", [128, 512])
    pm = [P.psum("pmx0", [128, 512]), P.psum("pmx1", [128, 512])]
    ptr = [P.psum("ptr0", [128, 512]), P.psum("ptr1", [128, 512])]
    pl = P.psum("pl", [128, 512]); pg = P.psum("pgt", [128, 512])
    for g in range(SEQ // N):
        ts = slice(g * N, (g + 1) * N)
        P.dma("sp", aT[:], dr["attT"][:, ts].rearrange("(h d) t -> d h t", d=64), reads=[dr["attT"]], writes=[aT])
        P.dma("act", rT[:], dr["rwkT"][:, ts].rearrange("(h d) t -> d h t", d=64), reads=[dr["rwkT"]], writes=[rT])
        P.dma("sp", sg[:], dr["sgT"][:, ts].rearrange("(c p) t -> p c t", p=128), reads=[dr["sgT"]], writes=[sg])
        for dc in range(8):
            for h in range(8):
                mm(P, pa[:], wua[:, h, dc * 128:(dc + 1) * 128], aT[:, h, :], h == 0, h == 7, [wua, aT], [pa])
            for h in range(8):
                mm(P, pr[:], wur[:, h, dc * 128:(dc + 1) * 128], rT[:, h, :], h == 0, h == 7, [wur, rT], [pr])
            tt(P, "dve", m1[:], pa[:], sg[:, dc, :], ALU.mult, [pa, sg], [m1])
            tt(P, "dve", m2[:], pr[:], sg[:, 8 + dc, :], ALU.mult, [pr, sg], [m2])
            tt(P, "pool", mT[:, dc, :], m1[:], m2[:], ALU.add, [m1, m2], [mT])
        for t4 in range(4):
            tok = g * N + t4 * 128
            P.dma("act", xt[:], dr["x"][tok:tok + 128, :], reads=[dr["x"]], writes=[xt])
            for hf in range(2):
                for k in range(8):
                    mm(P, pm[hf][:], mT[:, k, t4 * 128:(t4 + 1) * 128], wo[:, k, hf * 512:(hf + 1) * 512], k == 0, k == 7, [mT, wo], [pm[hf]])
                P.op("act", lambda e, hf=hf: e.activation(out=junk[:, 0:512], in_=pm[hf][:], func=AF.Square, accum_out=ssa[:, hf:hf + 1]),
                     reads=[pm[hf]], writes=[junk, ssa])
            tt(P, "dve", ss[:], ssa[:, 0:1], ssa[:, 1:2], ALU.add, [ssa], [ss])
            rms_rstd(P, ss, rstd, 1024.0, 1e-6)
            for hf in range(2):
                hs = slice(hf * 512, (hf + 1) * 512)
                P.op("dve", lambda e, hf=hf, hs=hs: e.scalar_tensor_tensor(out=x1[:, hs], in0=pm[hf][:], scalar=rstd[:, 0:1], in1=mod["G2"][:, hs],
                                                                          op0=ALU.mult, op1=ALU.mult), reads=[pm[hf], rstd, mod["G2"]], writes=[x1])
            tt(P, "pool", x1[:], x1[:], xt[:], ALU.add, [x1, xt], [x1])
            P.dma("sp", dr["x1"][tok:tok + 128, :], x1[:], reads=[x1], writes=[dr["x1"]])
            P.op("act", lambda e: e.activation(out=junk[:], in_=x1[:], func=AF.Square, accum_out=ss[:]), reads=[x1], writes=[junk, ss])
            rms_rstd(P, ss, rstd, 1024.0, 1e-6)
            P.op("dve", lambda e: e.tensor_scalar(out=xt[:], in0=x1[:], scalar1=rstd[:, 0:1], scalar2=None, op0=ALU.mult),
                 reads=[x1, rstd], writes=[xt])
            for k in range(8):
                tr(P, ptr[k // 4][:, (k % 4) * 128:(k % 4 + 1) * 128], xt[:, k * 128:(k + 1) * 128], ident[:], [xt, ident], [ptr[k // 4]])
            for k in range(8):
                q = ptr[k // 4]
                P.op("dve", lambda e, q=q, k=k: e.tensor_scalar(out=hf32[:, k, :], in0=q[:, (k % 4) * 128:(k % 4 + 1) * 128],
                                                               scalar1=mod["Af"][:, k:k + 1], scalar2=mod["Bf"][:, k:k + 1],
                                                               op0=ALU.mult, op1=ALU.add), reads=[q, mod["Af"], mod["Bf"]], writes=[hf32])
            tt(P, "pool", junk[:], xt[:], mod["Arow"][:], ALU.mult, [xt, mod["Arow"]], [junk])
            tt(P, "pool", hfrow[:], junk[:], mod["Brow"][:], ALU.add, [junk, mod["Brow"]], [hfrow])
            P.dma("sp", dr["hftok"][tok:tok + 128, :], hfrow[:], reads=[hfrow], writes=[dr["hftok"]])
            for k in range(8):
                mm(P, pl[:, 0:32], hf32[:, k, :], wr[:, k, :], k == 0, False, [hf32, wr], [pl])
            mm(P, pl[:, 0:32], ones[0:1, :], br[:], False, True, [ones, br], [pl])
            P.op("dve", lambda e: e.tensor_copy(out=lg[:], in_=pl[:, 0:32]), reads=[pl], writes=[lg])
            P.op("dve", lambda e: e.max(out=m8[:], in_=lg[:]), reads=[lg], writes=[m8])
            P.op("dve", lambda e: e.tensor_scalar(out=nmx[:], in0=m8[:, 0:1], scalar1=-1.0, scalar2=None, op0=ALU.mult), reads=[m8], writes=[nmx])
            P.op("dve", lambda e: e.tensor_scalar(out=msk[:], in0=lg[:], scalar1=m8[:, 3:4], scalar2=None, op0=ALU.is_ge), reads=[lg, m8], writes=[msk])
            act(P, ex[:], lg[:], AF.Exp, [lg, nmx], [ex], bias=nmx[:, 0:1])
            tt(P, "dve", ex[:], ex[:], msk[:], ALU.mult, [ex, msk], [ex])
            P.op("dve", lambda e: e.reduce_sum(out=sm[:], in_=ex[:], axis=mybir.AxisListType.X), reads=[ex], writes=[sm])
            P.op("dve", lambda e: e.reciprocal(out=sm[:], in_=sm[:]), reads=[sm], writes=[sm])
            P.op("dve", lambda e: e.tensor_scalar(out=ex[:], in0=ex[:], scalar1=sm[:, 0:1], scalar2=None, op0=ALU.mult), reads=[ex, sm], writes=[ex])
            ti_ = g * 4 + t4
            P.op("dve", lambda e, ti_=ti_: e.tensor_copy(out=rt["Gall"][:, ti_, :], in_=ex[:]), reads=[ex], writes=[rt["Gall"]])
            P.op("dve", lambda e, ti_=ti_: e.tensor_copy(out=rt["Mall"][:, ti_, :], in_=msk[:]), reads=[msk], writes=[rt["Mall"]])
    P.barrier()
    P.release(m0)


NB = SEQ * 4 // 128 + NEXP
I32 = mybir.dt.int32
BIGI = 1.0e6


def phase_moe(P, cs, mod, rt, dr):
    m0 = P.mark()
    ones, ident = cs["ones"], cs["ident"]
    Mall, Gall = rt["Mall"], rt["Gall"]
    NT = SEQ // 128
    xg, yb = dr["xg"], dr["yb"]
    IDX = P.sbuf("IDX", [128, NB, 8], I32)
    DEST = P.sbuf("DEST", [128, NT, 4], I32)
    GK = P.sbuf("GK", [128, NT, 4])
    m1 = P.mark()
    zt = P.sbuf("zt", [128, 2 * 1024], BF16)
    P.op("pool", lambda e: e.memset(zt[:], 0.0), writes=[zt])
    xgv = xg.t.rearrange("(n p j) d -> n p (j d)", p=128, j=2)
    for n in range(NB * 128 // 256):
        P.dma("sp" if n % 2 else "act", xgv[n], zt[:], reads=[zt], shared=[xg])
    P.fence(xg)
    it = P.sbuf("it", [128, 192], I32)
    itf = P.sbuf("itf", [128, 192])
    P.op("pool", lambda e: e.iota(it[:], pattern=[[128, 192]], base=0, channel_multiplier=0), writes=[it])
    P.op("dve", lambda e: e.tensor_copy(out=itf[:], in_=it[:]), reads=[it], writes=[itf])
    pi = P.sbuf("pi", [128, 1], I32)
    pif = P.sbuf("pif", [128, 1])
    P.op("pool", lambda e: e.iota(pi[:], pattern=[[0, 1]], base=0, channel_multiplier=1), writes=[pi])
    P.op("dve", lambda e: e.tensor_copy(out=pif[:], in_=pi[:]), reads=[pi], writes=[pif])
    pc = P.psum("pc", [128, 512])
    pq = P.psum("pq", [128, 512])
    for i in range(NT):
        mm(P, pc[:, 0:32], ones[:], Mall[:, i, :], i == 0, i == NT - 1, [ones, Mall], [pc])
    cnt = P.sbuf("cnt", [128, 32])
    P.op("dve", lambda e: e.tensor_copy(out=cnt[:], in_=pc[:, 0:32]), reads=[pc], writes=[cnt])
    cmp = P.sbuf("cmp", [128, 32, 32])
    tt(P, "dve", cmp[:], cnt[:, :, None].broadcast_to([128, 32, 32]), itf[:, None, 0:32].broadcast_to([128, 32, 32]),
       ALU.is_gt, [cnt, itf], [cmp])
    padded = P.sbuf("padded", [128, 32])
    P.op("dve", lambda e: e.reduce_sum(out=padded[:], in_=cmp[:], axis=mybir.AxisListType.X), reads=[cmp], writes=[padded])
    P.op("dve", lambda e: e.tensor_scalar(out=padded[:], in0=padded[:], scalar1=128.0, scalar2=None, op0=ALU.mult), reads=[padded], writes=[padded])
    p_end = P.sbuf("p_end", [128, 32])
    P.op("dve", lambda e: e.tensor_tensor_scan(out=p_end[:], data0=ones[:, 0:32], data1=padded[:], initial=0.0, op0=ALU.mult, op1=ALU.add),
         reads=[ones, padded], writes=[p_end])
    base0 = P.sbuf("base0", [128, 32])
    tt(P, "dve", base0[:], p_end[:], padded[:], ALU.subtract, [p_end, padded], [base0])
    ebc = P.sbuf("ebc", [128, NB, 32])
    tt(P, "dve", ebc[:], p_end[:, None, :].broadcast_to([128, NB, 32]), itf[:, 0:NB, None].broadcast_to([128, NB, 32]),
       ALU.is_le, [p_end, itf], [ebc])
    eb = P.sbuf("eb", [128, NB])
    P.op("dve", lambda e: e.reduce_sum(out=eb[:], in_=ebc[:], axis=mybir.AxisListType.X), reads=[ebc], writes=[eb])
    P.op("dve", lambda e: e.tensor_scalar(out=eb[:], in0=eb[:], scalar1=31.0, scalar2=None, op0=ALU.min), reads=[eb], writes=[eb])
    sk = P.sbuf("sk", [128, NB])
    P.op("pool", lambda e: e.memset(sk[:], 0.0), writes=[sk])
    tt(P, "dve", sk[:, 1:NB], eb[:, 1:NB], eb[:, 0:NB - 1], ALU.is_equal, [eb], [sk])
    P.op("dve", lambda e: e.tensor_scalar(out=sk[:], in0=sk[:], scalar1=BIGI, scalar2=None, op0=ALU.mult), reads=[sk], writes=[sk])
    basef = P.sbuf("basef", [128, NB])
    P.op("dve", lambda e: e.tensor_scalar(out=basef[:], in0=eb[:], scalar1=128.0, scalar2=pif[:, 0:1], op0=ALU.mult, op1=ALU.add),
         reads=[eb, pif], writes=[basef])
    idxf = P.sbuf("idxf", [128, NB, 8])
    for pc_ in range(6):
        mul, add = (4.0, float(pc_)) if pc_ < 4 else (2.0, float(pc_ - 4))
        P.op("dve", lambda e, pc_=pc_, mul=mul, add=add: e.tensor_scalar(out=idxf[:, :, pc_], in0=basef[:], scalar1=mul, scalar2=add,
                                                                         op0=ALU.mult, op1=ALU.add), reads=[basef], writes=[idxf])
        tt(P, "dve", idxf[:, :, pc_], idxf[:, :, pc_], sk[:], ALU.add, [idxf, sk], [idxf])
    tt(P, "dve", idxf[:, :, 6], eb[:], sk[:], ALU.add, [eb, sk], [idxf])
    P.op("dve", lambda e: e.tensor_copy(out=IDX[:, :, 0:7], in_=idxf[:, :, 0:7]), reads=[idxf], writes=[IDX])
    DESTf = P.sbuf("DESTf", [128, NT, 4])
    Dt = P.sbuf("Dt", [128, 32])
    Vt = P.sbuf("Vt", [128, 32])
    oh = P.sbuf("oh", [128, 32])
    m8 = P.sbuf("m8s", [128, 8])
    for i in range(NT):
        mm(P, pq[:, 0:32], cs["Lstrict"][:], Mall[:, i, :], True, True, [cs["Lstrict"], Mall], [pq])
        tt(P, "dve", Dt[:], pq[:, 0:32], base0[:], ALU.add, [pq, base0], [Dt])
        mm(P, pc[:, 0:32], ones[:], Mall[:, i, :], True, True, [ones, Mall], [pc])
        tt(P, "dve", base0[:], base0[:], pc[:, 0:32], ALU.add, [base0, pc], [base0])
        P.op("dve", lambda e: e.tensor_scalar(out=Vt[:], in0=Dt[:], scalar1=-1.0, scalar2=32768.0, op0=ALU.mult, op1=ALU.add), reads=[Dt], writes=[Vt])
        tt(P, "dve", Vt[:], Vt[:], Mall[:, i, :], ALU.mult, [Vt, Mall], [Vt])
        P.op("dve", lambda e: e.max(out=m8[:], in_=Vt[:]), reads=[Vt], writes=[m8])
        P.op("dve", lambda e, i=i: e.tensor_scalar(out=DESTf[:, i, :], in0=m8[:, 0:4], scalar1=-1.0, scalar2=32768.0, op0=ALU.mult, op1=ALU.add),
             reads=[m8], writes=[DESTf])
        for k in range(4):
            P.op("dve", lambda e, k=k: e.tensor_scalar(out=oh[:], in0=Vt[:], scalar1=m8[:, k:k + 1], scalar2=None, op0=ALU.is_equal),
                 reads=[Vt, m8], writes=[oh])
            tt(P, "dve", oh[:], oh[:], Gall[:, i, :], ALU.mult, [oh, Gall], [oh])
            P.op("dve", lambda e, i=i, k=k: e.reduce_sum(out=GK[:, i, k:k + 1], in_=oh[:], axis=mybir.AxisListType.X), reads=[oh], writes=[GK])
    P.op("dve", lambda e: e.tensor_copy(out=DEST[:], in_=DESTf[:]), reads=[DESTf], writes=[DEST])
    hr = [P.sbuf("hr%d" % i, [128, 1024], BF16) for i in range(2)]
    for i in range(NT):
        H = hr[i % 2]
        P.dma("sp", H[:], dr["hftok"][i * 128:(i + 1) * 128, :], reads=[dr["hftok"]], writes=[H])
        for k in range(4):
            P.dma("pool", xg.t[:, :], H[:], reads=[H, DEST], shared=[xg], ind=(DEST[:, i, k:k + 1], True, NB * 128 - 1))
    P.barrier()
    P.release(m1)
    m2 = P.mark()
    identb = P.sbuf("identb", [128, 128], BF16)
    P.op("dve", lambda e: e.tensor_copy(out=identb[:], in_=ident[:]), reads=[ident], writes=[identb])
    wgu_p = [P.sbuf("wgu%d" % i, [128, 8, 512], BF16) for i in range(4)]
    wdn_p = [P.sbuf("wdn%d" % i, [128, 8, 512], BF16) for i in range(2)]
    bgu_b = P.sbuf("bgu_b", [128, 2048])
    bdn_b = P.sbuf("bdn_b", [128, 1024])
    NS = 2
    xs = [P.sbuf("xs%d" % i, [128, 1024], BF16) for i in range(3)]
    xgT = [P.sbuf("xgT%d" % i, [128, 8, 128], BF16) for i in range(NS)]
    hgu = [P.sbuf("hgu%d" % i, [128, 2048]) for i in range(NS)]
    gcb = [P.sbuf("gcb%d" % i, [128, 1024]) for i in range(NS)]
    sgb = [P.sbuf("sgb%d" % i, [128, 1024]) for i in range(NS)]
    u1b = [P.sbuf("u1b%d" % i, [128, 1024]) for i in range(NS)]
    actb = [P.sbuf("actb%d" % i, [128, 1024], BF16) for i in range(NS)]
    actT = [P.sbuf("actT%d" % i, [128, 8, 128], BF16) for i in range(NS)]
    ysb = [P.sbuf("ysb%d" % i, [128, 1024]) for i in range(NS)]
    ptx = P.psum("ptx", [128, 1024], BF16)
    pta = P.psum("pta", [128, 1024], BF16)
    pgu = [P.psum("pgu%d" % i, [128, 512]) for i in range(2)]
    pdn = [P.psum("pdn%d" % i, [128, 512]) for i in range(2)]
    wgu2d, wdn2d = dr["wgu2d"], dr["wdn2d"]
    def stage1(b):
        s_ = b % NS
        for ng in range(4):
            P.dma("pool", wgu_p[ng][:].rearrange("p k n -> p (k n)"), wgu2d.t[:, :], reads=[wgu2d, IDX], writes=[wgu_p[ng]],
                  ind=(IDX[:, b, ng:ng + 1], False, NEXP * 128 * 4 - 1))
        P.dma("pool", bgu_b[:], dr["b_gate_up"].t[:, :], reads=[dr["b_gate_up"], IDX], writes=[bgu_b], ind=(IDX[:, b, 6:7], False, NEXP - 1))
        X = xs[b % 3]
        for k in range(8):
            tr(P, ptx[:, k * 128:(k + 1) * 128], X[:, k * 128:(k + 1) * 128], identb[:], [X, identb], [ptx])
        act(P, xgT[s_][:].rearrange("p k t -> p (k t)"), ptx[:], AF.Copy, [ptx], [xgT[s_]])
        H = hgu[s_]
        for ng in range(4):
            q = pgu[ng % 2]
            for k in range(8):
                mm(P, q[:], xgT[s_][:, k, :], wgu_p[ng][:, k, :], k == 0, k == 7, [xgT[s_], wgu_p[ng]], [q])
            tt(P, "dve", H[:, ng * 512:(ng + 1) * 512], q[:], bgu_b[:, ng * 512:(ng + 1) * 512], ALU.add, [q, bgu_b], [H])

    def stage1b(b):
        s_ = b % NS
        H = hgu[s_]
        P.op("dve", lambda e, H=H, s_=s_: e.tensor_scalar(out=gcb[s_][:], in0=H[:, 0:2048:2], scalar1=7.0, scalar2=None, op0=ALU.min),
             reads=[H], writes=[gcb[s_]])
        act(P, sgb[s_][:], gcb[s_][:], AF.Sigmoid, [gcb[s_]], [sgb[s_]], scale=1.702)
        P.op("dve", lambda e, H=H, s_=s_: e.tensor_scalar(out=u1b[s_][:], in0=H[:, 1:2048:2], scalar1=7.0, scalar2=-7.0, op0=ALU.min, op1=ALU.max),
             reads=[H], writes=[u1b[s_]])
        tt(P, "dve", gcb[s_][:], gcb[s_][:], sgb[s_][:], ALU.mult, [gcb[s_], sgb[s_]], [gcb[s_]])
        P.op("dve", lambda e, s_=s_: e.scalar_tensor_tensor(out=actb[s_][:], in0=u1b[s_][:], scalar=1.0, in1=gcb[s_][:], op0=ALU.add, op1=ALU.mult),
             reads=[u1b[s_], gcb[s_]], writes=[actb[s_]])

    def stage2(b):
        s_ = b % NS
        for hf in range(2):
            P.dma("pool", wdn_p[hf][:].rearrange("p k n -> p (k n)"), wdn2d.t[:, :], reads=[wdn2d, IDX], writes=[wdn_p[hf]],
                  ind=(IDX[:, b, 4 + hf:5 + hf], False, NEXP * 128 * 2 - 1))
        P.dma("pool", bdn_b[:], dr["b_down"].t[:, :], reads=[dr["b_down"], IDX], writes=[bdn_b], ind=(IDX[:, b, 6:7], False, NEXP - 1))
        for k in range(8):
            tr(P, pta[:, k * 128:(k + 1) * 128], actb[s_][:, k * 128:(k + 1) * 128], identb[:], [actb[s_], identb], [pta])
        act(P, actT[s_][:].rearrange("p k t -> p (k t)"), pta[:], AF.Copy, [pta], [actT[s_]])
        Y = ysb[s_]
        for hf in range(2):
            q = pdn[hf]
            for k in range(8):
                mm(P, q[:], actT[s_][:, k, :], wdn_p[hf][:, k, :], k == 0, k == 7, [actT[s_], wdn_p[hf]], [q])
            tt(P, "dve", Y[:, hf * 512:(hf + 1) * 512], q[:], bdn_b[:, hf * 512:(hf + 1) * 512], ALU.add, [q, bdn_b], [Y])
        P.dma("sp", yb.t[b * 128:(b + 1) * 128, :], Y[:], reads=[Y], shared=[yb])

    def loadx(b):
        P.dma("sp", xs[b % 3][:], xg.t[b * 128:(b + 1) * 128, :], reads=[xg], writes=[xs[b % 3]])

    loadx(0)
    loadx(1)
    stage1(0)
    stage1b(0)
    for b in range(NB):
        if b + 2 < NB:
            loadx(b + 2)
        if b + 1 < NB:
            stage1(b + 1)
        stage2(b)
        if b + 1 < NB:
            stage1b(b + 1)
    P.barrier()
    P.release(m2)
    ygs = [[P.sbuf("yg%d_%d" % (i, t), [128, 1024]) for i in range(4)] for t in range(2)]
    xts = [P.sbuf("xte%d" % t, [128, 1024]) for t in range(2)]
    ots = [P.sbuf("ote%d" % t, [128, 1024]) for t in range(2)]
    yas = [P.sbuf("ya%d" % t, [128, 1024]) for t in range(2)]
    ss = P.sbuf("sse", [128, 1])
    rstd = P.sbuf("rstde", [128, 1])

    def gather(i):
        yg = ygs[i % 2]
        for k in range(4):
            P.dma("pool", yg[k][:], yb.t[:, :], reads=[yb, DEST], writes=[yg[k]], ind=(DEST[:, i, k:k + 1], False, NB * 128 - 1))
        P.dma("sp", xts[i % 2][:], dr["x1"][i * 128:(i + 1) * 128, :], reads=[dr["x1"]], writes=[xts[i % 2]])

    gather(0)
    for i in range(NT):
        tok = i * 128
        if i + 1 < NT:
            gather(i + 1)
        yg, xt, ot, ya = ygs[i % 2], xts[i % 2], ots[i % 2], yas[i % 2]
        P.op("dve", lambda e, i=i, yg=yg, ya=ya: e.tensor_scalar(out=ya[:], in0=yg[0][:], scalar1=GK[:, i, 0:1], scalar2=None, op0=ALU.mult),
             reads=[yg[0], GK], writes=[ya])
        for k in range(1, 4):
            P.op("dve", lambda e, i=i, k=k, yg=yg, ya=ya: e.scalar_tensor_tensor(out=ya[:], in0=yg[k][:], scalar=GK[:, i, k:k + 1], in1=ya[:],
                                                                               op0=ALU.mult, op1=ALU.add),
                 reads=[yg[k], GK, ya], writes=[ya])
        P.op("act", lambda e, ot=ot, ya=ya: e.activation(out=ot[:], in_=ya[:], func=AF.Square, accum_out=ss[:]), reads=[ya], writes=[ot, ss])
        rms_rstd(P, ss, rstd, 1024.0, 1e-6)
        P.op("dve", lambda e, ot=ot, ya=ya: e.scalar_tensor_tensor(out=ot[:], in0=ya[:], scalar=rstd[:, 0:1], in1=mod["G5"][:], op0=ALU.mult, op1=ALU.mult),
             reads=[ya, rstd, mod["G5"]], writes=[ot])
        tt(P, "pool", ot[:], ot[:], xt[:], ALU.add, [ot, xt], [ot])
        P.dma("sp", dr["out"][tok:tok + 128, :], ot[:], reads=[ot], writes=[dr["out"]])
    P.barrier()
    P.release(m0)


IN_SPECS = [
    ("x", [SEQ, D], F32), ("ctx", [CTX, D], F32), ("ccol", [128, 16], F32), ("w_ada", [D, 6 * D], F32),
    ("b_ada", [1, 6 * D], F32), ("bada_col", [128, 48], F32), ("gcols", [128, 16], F32),
    ("g_post_mix", [1, D], F32), ("g_post_ffn", [1, D], F32), ("g_pre_ffn", [1, D], F32), ("w_in", [D, NCH_W * 128], F32),
    ("bin_col", [128, NCH_W], F32), ("bv_row", [1, 128], F32), ("cos_t", [128, SEQ], F32), ("sin_t", [128, SEQ], F32),
    ("sink_row", [1, 1024], F32), ("pp64", [64, NP64], F32), ("w2_f", [64, 512], F32), ("w2_b", [64, 512], F32),
    ("a2_f", [64, 512], F32), ("a2_b", [64, 512], F32), ("g2", [128, 512], F32), ("mugl", [128, 2], F32),
    ("w_up_att", [512, D], F32), ("w_up_rwkv", [512, D], F32), ("w_out", [D, D], F32), ("w_router", [D, 32], F32),
    ("b_router", [1, 32], F32), ("wgu2d", [NEXP * 128 * 4, 4096], F32), ("wdn2d", [NEXP * 128 * 2, 4096], F32),
    ("b_down", [NEXP, D], F32), ("b_gate_up", [NEXP, 2 * D], F32),
]
SCRATCH = [
    ("zrT", [1920, TT], F32), ("qT", [512, SEQ], BF16), ("kT", [128, TT], BF16), ("vtok", [TT, 128], BF16),
    ("sgT", [2048, SEQ], BF16), ("attT", [512, SEQ], BF16), ("yT_f", [512, SEQ], F32), ("yT_b", [512, SEQ], F32),
    ("bonusT", [512, SEQ], F32), ("gateT", [512, SEQ], F32), ("rwkT", [512, SEQ], BF16), ("x1", [SEQ, D], F32),
    ("hftok", [SEQ, D], BF16), ("xg", [NB * 128, D], BF16), ("yb", [NB * 128, D], F32),
]


def build(debug=False, phases=None, nexp=NEXP):
    nc = bass.Bass("TRN2", target_bir_lowering=False)
    P = Prog(nc)
    dr = {}
    for n, shp, dt in IN_SPECS:
        dr[n] = Buf(n, nc.dram_tensor(n, list(shp), dt, kind="ExternalInput").ap())
    for n, shp, dt in SCRATCH:
        kind = "ExternalOutput" if debug else "Internal"
        dr[n] = Buf(n, nc.dram_tensor(n, list(shp), dt, kind=kind).ap())
    dr["out"] = Buf("out", nc.dram_tensor("out", [SEQ, D], F32, kind="ExternalOutput").ap())
    ph = phases or ("adaln", "proj", "attn", "rwkv", "rwkv_out", "merge", "moe")
    cs = make_consts(P)
    mod = phase_adaln(P, cs, dr)
    if "proj" in ph:
        phase_proj(P, cs, mod, dr)
    if "attn" in ph:
        phase_attn(P, cs, dr)
    mR = P.mark()
    R = rwkv_setup(P, dr)
    if "rwkv" in ph:
        mW = P.mark()
        PB = ([P.psum("bk%d" % i, [128, 512]) for i in range(6)], [P.psum("bb%d" % i, [128, 1024], BF16) for i in range(2)], [0, 0])
        gens = list(rwkv_dir(P, cs, R, 0, dr, PB)) + list(rwkv_dir(P, cs, R, 1, dr, PB))
        while gens:
            for g_ in list(gens):
                try:
                    next(g_)
                except StopIteration:
                    gens.remove(g_)
        P.barrier()
        P.release(mW)
    if "rwkv_out" in ph:
        phase_rwkv_out(P, cs, R, dr)
    P.barrier()
    P.release(mR)
    rt = {"Mall": P.sbuf("Mall", [128, SEQ // 128, 32]), "Gall": P.sbuf("Gall", [128, SEQ // 128, 32])}
    if "merge" in ph:
        phase_merge(P, cs, mod, dr, rt)
    if "moe" in ph:
        phase_moe(P, cs, mod, rt, dr)
    P.barrier()
    P.emit()
    P.close()
    return nc, P


def host_layout(inp):
    f = lambda a: np.ascontiguousarray(np.asarray(a, np.float32))
    col = lambda v: f(np.asarray(v).reshape(-1, 128).T)
    sh = {}
    sh["w_ada"] = f(inp["w_ada"][0]); sh["b_ada"] = f(inp["b_ada"][0][None])
    sh["bada_col"] = col(inp["b_ada"][0])
    sh["gcols"] = f(np.concatenate([col(inp["g_pre_mix"][0]), col(inp["g_pre_ffn"][0])], 1))
    sh["g_post_mix"] = f(inp["g_post_mix"][0][None]); sh["g_post_ffn"] = f(inp["g_post_ffn"][0][None]); sh["g_pre_ffn"] = f(inp["g_pre_ffn"][0][None])
    w_in = np.asarray(inp["w_in"][0], np.float32); b_in = np.asarray(inp["b_in"][0], np.float32)
    d = np.arange(64)
    partner = np.where((d % 32) < 16, d + 16, d - 16)
    qperm = (np.arange(8)[:, None] * 64 + partner[None, :]).reshape(-1)
    kperm = 512 + (np.arange(2)[:, None] * 64 + partner[None, :]).reshape(-1)
    cols = np.concatenate([np.arange(4736), qperm, kperm])
    sh["w_in"] = f(w_in[:, cols])
    sh["bin_col"] = col(b_in[cols])
    sh["bv_row"] = f(b_in[640:768][None])
    half = 32
    inv_freq = (np.float32(10000.0) ** (-np.arange(0, half, 2, dtype=np.float32) / np.float32(half))).astype(np.float32)
    t = np.arange(SEQ)
    row = (t // 64).astype(np.float32); colp = (t % 64).astype(np.float32)
    dd = np.arange(128) % 64
    pos = np.where((dd < 32)[:, None], row[None, :], colp[None, :]).astype(np.float32)
    ang = (pos * inv_freq[dd % 16][:, None]).astype(np.float32)
    sign = np.where((dd % 32) < 16, -1.0, 1.0).astype(np.float32)[:, None]
    sh["cos_t"] = f(np.cos(ang)); sh["sin_t"] = f(np.sin(ang) * sign)
    sh["sink_row"] = f(np.repeat(np.asarray(inp["att_sinks"][0], np.float32), 128)[None])
    sh["pp64"] = pack64(inp)
    for n in ("w2_f", "w2_b", "a2_f", "a2_b", "g2", "w_up_att", "w_up_rwkv", "w_out", "w_router", "b_down", "b_gate_up"):
        sh[n] = f(inp[n][0])
    wgu = np.asarray(inp["w_gate_up"][0], np.float32).reshape(NEXP, 8, 128, 4, 512)
    sh["wgu2d"] = np.ascontiguousarray(wgu.transpose(0, 2, 3, 1, 4)).reshape(NEXP * 128 * 4, 4096)
    wdn = np.asarray(inp["w_down"][0], np.float32).reshape(NEXP, 8, 128, 2, 512)
    sh["wdn2d"] = np.ascontiguousarray(wdn.transpose(0, 2, 3, 1, 4)).reshape(NEXP * 128 * 2, 4096)
    sh["mugl"] = f(np.stack([inp["mu_prev"][0][1792:1920], inp["mu_next"][0][1792:1920]], 1))
    sh["b_router"] = f(inp["b_router"][0][None])
    cc = np.asarray(inp["c_ctx"], np.float32)
    percore = []
    for b in range(inp["x"].shape[0]):
        m = dict(sh)
        m["x"] = f(inp["x"][b]); m["ctx"] = f(inp["ctx"][b])
        m["ccol"] = f(np.concatenate([col(inp["c"][b]), col(cc)], 1))
        percore.append(m)
    return percore


_NC = {}


def kernel(**inputs):
    if "nc" not in _NC:
        _NC["nc"] = build()[0]
    in_maps = host_layout(inputs)
    res = run_bass_kernel_spmd(_NC["nc"], in_maps, core_ids=list(range(len(in_maps))))
    return np.stack([np.asarray(r["out"], np.float32) for r in res.results], 0)
```

```python
import numpy as np
import concourse.bass as bass
import concourse.mybir as mybir
from concourse.bass_utils import run_bass_kernel_spmd

F32 = mybir.dt.float32
BF16 = mybir.dt.bfloat16
AF = mybir.ActivationFunctionType
ALU = mybir.AluOpType

SEQ = 4096
CTX = 256
TT = SEQ + CTX
D = 1024
HD = 64
NH = 8
C = 64
W = 64
NEXP = 32


class Buf:
    __slots__ = ("name", "t", "lw", "rd", "fence")

    def __init__(self, name, t):
        self.name = name
        self.t = t
        self.lw = []
        self.rd = []
        self.fence = []

    def __getitem__(self, idx):
        return self.t[idx]


class Prog:
    ENG = ("pe", "act", "dve", "pool", "sp")

    def __init__(self, nc, n_dma_sems=32):
        self.nc = nc
        self.q = {e: [] for e in self.ENG}
        self.cnt = {}
        self.known = {e: {} for e in self.ENG}
        self.sems = {}
        self.n_dma_sems = n_dma_sems
        self.dma_i = 0
        self.stack = []
        self.ninst = 0
        self.uid = 0
        self.tot = {}
        self.regs = {}

    def enter(self, cm):
        v = cm.__enter__()
        self.stack.append(cm)
        return v

    def mark(self):
        return len(self.stack)

    def release(self, mark):
        while len(self.stack) > mark:
            self.stack.pop().__exit__(None, None, None)

    def sbuf(self, name, shape, dtype=F32):
        self.uid += 1
        t = self.enter(self.nc.sbuf_tensor("%s_%d" % (name, self.uid), list(shape), dtype))
        return Buf(name, t)

    def psum(self, name, shape, dtype=F32):
        self.uid += 1
        t = self.enter(self.nc.psum_tensor("%s_%d" % (name, self.uid), list(shape), dtype))
        return Buf(name, t)

    def dram(self, name, shape, dtype=F32):
        t = self.nc.dram_tensor(name, list(shape), dtype, kind="Internal")
        return Buf(name, t.ap())

    def sem(self, key):
        if key not in self.sems:
            nm = "s_" + "_".join(str(k) for k in (key if isinstance(key, tuple) else (key,)))
            self.sems[key] = self.enter(self.nc.semaphore(nm))
            self.cnt[key] = 0
        return self.sems[key]

    def _deps(self, eng, reads, writes, shared=()):
        deps = {}

        def add(d):
            k, v = d
            if deps.get(k, 0) < v:
                deps[k] = v
        for b in reads:
            for d in b.lw:
                add(d)
            for d in b.fence:
                add(d)
        for b in writes:
            for d in b.lw:
                add(d)
            for d in b.fence:
                add(d)
            for d in b.rd:
                add(d)
        for b in shared:
            for d in b.fence:
                add(d)
            for d in b.rd:
                add(d)
        out = []
        for k, v in deps.items():
            if eng == "pe" and isinstance(k, tuple) and k[0] == "pe":
                continue
            if self.known[eng].get(k, 0) >= v:
                continue
            self.known[eng][k] = v
            out.append((k, v))
        return out

    @staticmethod
    def _compact(lst):
        mx = {}
        for k, v in lst:
            if mx.get(k, 0) < v:
                mx[k] = v
        return list(mx.items())

    def _mark(self, key, val, reads, writes, shared=()):
        for b in reads:
            b.rd.append((key, val))
            if len(b.rd) > 64:
                b.rd = self._compact(b.rd)
        for b in writes:
            b.lw = [(key, val)]
            b.rd = []
            b.fence = []
        for b in shared:
            b.lw.append((key, val))
            if len(b.lw) > 64:
                b.lw = self._compact(b.lw)

    def fence(self, b):
        b.fence = self._compact(b.fence + b.lw)
        b.lw = []

    EPOCH = 4000

    def op(self, eng, fn, reads=(), writes=()):
        n = self.tot.get(eng, 0)
        self.tot[eng] = n + 1
        key = (eng, n // self.EPOCH)
        self.sem(key)
        waits = self._deps(eng, reads, writes)
        self.cnt[key] += 1
        val = self.cnt[key]
        self.q[eng].append(("op", fn, waits, key, 1))
        self._mark(key, val, reads, writes)
        self.ninst += 1

    def dma(self, eng, out, in_, reads=(), writes=(), shared=(), ind=None):
        i = self.dma_i % self.n_dma_sems
        self.dma_i += 1
        key = ("dma", i)
        self.sem(key)
        waits = self._deps(eng, reads, writes, shared)
        prev = self.cnt[key]
        if prev > 0 and self.known[eng].get(key, 0) < prev:
            self.known[eng][key] = prev
            waits.append((key, prev))
        self.cnt[key] += 16
        val = self.cnt[key]
        self.q[eng].append(("dma", (out, in_, ind), waits, key, 16))
        self._mark(key, val, reads, writes, shared)
        self.ninst += 1

    def reg(self, e, val):
        k = (id(e), val)
        if k not in self.regs:
            self.regs[k] = e.to_reg(val)
        return self.regs[k]

    def barrier(self):
        snap = dict(self.cnt)
        for e in self.ENG:
            waits = []
            for k, v in snap.items():
                if v > 0 and self.known[e].get(k, 0) < v:
                    self.known[e][k] = v
                    waits.append((k, v))
            if waits:
                self.q[e].append(("wait", None, waits, None, 0))

    def emit(self):
        nc = self.nc
        block = self.enter(nc.Block())
        engmap = {"pe": "tensor", "act": "scalar", "dve": "vector", "pool": "gpsimd", "sp": "sync"}
        prog = self

        def make(ename):
            items = prog.q[ename]

            def body(e):
                for kind, payload, waits, key, inc in items:
                    for (k, v) in waits:
                        e.wait_ge(prog.sems[k], v)
                    if kind == "op":
                        payload(e).then_inc(prog.sems[key], inc)
                    elif kind == "dma":
                        o, i, ind = payload
                        if ind is None:
                            e.dma_start(out=o, in_=i).then_inc(prog.sems[key], inc)
                        else:
                            idx_ap, on_out, bound = ind
                            off = bass.IndirectOffsetOnAxis(ap=idx_ap, axis=0)
                            try:
                                ins = e.indirect_dma_start(out=o, out_offset=off if on_out else None, in_=i,
                                                           in_offset=None if on_out else off, bounds_check=prog.reg(e, bound),
                                                           oob_is_err=False)
                            except Exception:
                                print("INDIRECT FAIL", o.shape, o.ap, i.shape, i.ap, idx_ap.shape, idx_ap.ap, on_out, bound)
                                raise
                            ins.then_inc(prog.sems[key], inc)
            return body

        for ename in self.ENG:
            if self.q[ename]:
                getattr(block, engmap[ename])(make(ename))

    def close(self):
        self.release(0)


def tt(P, eng, out, i0, i1, op, reads, writes):
    P.op(eng, lambda e: e.tensor_tensor(out=out, in0=i0, in1=i1, op=op), reads=reads, writes=writes)


def act(P, out, in_, func, reads, writes, bias=None, scale=None):
    kw = {}
    if bias is not None:
        kw["bias"] = bias
    if scale is not None:
        kw["scale"] = scale
    P.op("act", lambda e: e.activation(out=out, in_=in_, func=func, **kw), reads=reads, writes=writes)


def mm(P, out, lhsT, rhs, start, stop, reads, writes):
    P.op("pe", lambda e: e.matmul(out, lhsT=lhsT, rhs=rhs, start=start, stop=stop), reads=reads, writes=writes)


def tr(P, out, in_, ident, reads, writes):
    P.op("pe", lambda e: e.transpose(out, in_, ident), reads=reads, writes=writes)


def make_consts(P):
    cs = {}
    ones = P.sbuf("ones128", [128, 128])
    P.op("pool", lambda e: e.memset(ones[:], 1.0), writes=[ones])
    cs["ones"] = ones

    def sel(name, cmp, base, cm, pat):
        b = P.sbuf(name, [128, 128])
        P.op("pool", lambda e: e.affine_select(out=b[:], in_=ones[:], pattern=[[pat, 128]], compare_op=cmp,
                                               fill=P.reg(e, 0.0), base=base, channel_multiplier=cm),
             reads=[ones], writes=[b])
        cs[name] = b
    sel("ident", ALU.is_equal, 0, 1, -1)
    sel("Lstrict", ALU.is_gt, 0, -1, 1)
    sel("Lincl", ALU.is_ge, 0, -1, 1)
    sel("Ustrict", ALU.is_gt, 0, 1, -1)
    sel("Uincl", ALU.is_ge, 0, 1, -1)
    return cs


P64 = {}
_o = 0
for _n, _w in [("mup3", 24), ("mun3", 24), ("mup_wl", 2), ("mun_wl", 2), ("mup_al", 2), ("mun_al", 2),
               ("k_k", 8), ("k_a", 8), ("r_k", 8), ("w0_f", 8), ("w0_b", 8), ("a0_f", 8), ("a0_b", 8),
               ("ln_w", 8), ("ln_b", 8)]:
    P64[_n] = (_o, _w)
    _o += _w
NP64 = _o


def pack64(inp):
    def hj(v):
        return np.ascontiguousarray(np.asarray(v, np.float32).reshape(-1, 64).T)
    mp, mn = inp["mu_prev"][0], inp["mu_next"][0]
    parts = {
        "mup3": hj(mp[0:1536]), "mun3": hj(mn[0:1536]),
        "mup_wl": hj(mp[1536:1664]), "mun_wl": hj(mn[1536:1664]),
        "mup_al": hj(mp[1664:1792]), "mun_al": hj(mn[1664:1792]),
        "k_k": hj(inp["k_k"][0]), "k_a": hj(inp["k_a"][0]), "r_k": hj(inp["r_k"][0].reshape(-1)),
        "w0_f": hj(inp["w0_f"][0]), "w0_b": hj(inp["w0_b"][0]),
        "a0_f": hj(inp["a0_f"][0]), "a0_b": hj(inp["a0_b"][0]),
        "ln_w": hj(inp["ln_x_w"][0]), "ln_b": hj(inp["ln_x_b"][0]),
    }
    out = np.zeros((64, NP64), np.float32)
    for n, (o, w) in P64.items():
        out[:, o:o + w] = parts[n]
    return out


class EW:
    def __init__(self, engines=("dve", "pool")):
        self.e = engines
        self.i = 0

    def __call__(self):
        self.i += 1
        return self.e[self.i % len(self.e)]


def rwkv_setup(P, dr):
    R = {}
    pp = P.sbuf("pp64", [64, NP64])
    P.dma("sp", pp[:], dr["pp64"][:], writes=[pp])
    R["pp"] = pp
    for n in ("w2_f", "w2_b", "a2_f", "a2_b"):
        b = P.sbuf(n, [64, 512])
        P.dma("sp", b[:], dr[n][:], writes=[b])
        R[n] = b
    g2 = P.sbuf("g2", [128, 512])
    P.dma("sp", g2[:], dr["g2"][:], writes=[g2])
    R["g2"] = g2
    mugl = P.sbuf("mugl", [128, 2])
    P.dma("sp", mugl[:], dr["mugl"][:], writes=[mugl])
    R["mugl"] = mugl
    eps18 = P.sbuf("eps18", [64, 1])
    P.op("pool", lambda e: e.memset(eps18[:], 1e-18), writes=[eps18])
    R["eps18"] = eps18
    omka = P.sbuf("omka", [64, 8])
    o, w = P64["k_a"]
    P.op("dve", lambda e: e.tensor_scalar(out=omka[:], in0=pp[:, o:o + w], scalar1=-1.0, scalar2=1.0,
                                          op0=ALU.mult, op1=ALU.add), reads=[pp], writes=[omka])
    R["omka"] = omka
    return R


def rwkv_dir(P, cs, R, dirn, dr, PB, dbg=None, max_win=None):
    nwin = TT // W
    nch = W // C
    nctx = CTX // W
    if dirn == 0:
        order = list(range(nwin))
    else:
        order = list(range(nctx - 1, -1, -1)) + list(range(nwin - 1, nctx - 1, -1))
    if max_win is not None:
        order = order[:max_win]
    pp = R["pp"]
    ident = cs["ident"]
    ones = cs["ones"]
    idn = ident[0:64, 0:64]

    def prm(n):
        o, w = P64[n]
        return pp[:, o:o + w]

    def bc(ap, shape):
        return ap.broadcast_to(shape)

    dsuf = "_f" if dirn == 0 else "_b"
    mAR = P.sbuf("mAR", [64, 128])
    mNT = P.sbuf("mNT", [64, 64])
    st, inc, ntm = ("Lstrict", "Lincl", "Ustrict") if dirn == 0 else ("Ustrict", "Uincl", "Lstrict")
    P.op("dve", lambda e: e.tensor_copy(out=mAR[:, 0:64], in_=cs[st][0:64, 0:64]), reads=[cs[st]], writes=[mAR])
    P.op("dve", lambda e: e.tensor_copy(out=mAR[:, 64:128], in_=cs[inc][0:64, 0:64]), reads=[cs[inc]], writes=[mAR])
    P.op("dve", lambda e: e.tensor_copy(out=mNT[:], in_=cs[ntm][0:64, 0:64]), reads=[cs[ntm]], writes=[mNT])

    ST = P.sbuf("ST", [64, 8, 64], BF16)
    P.op("pool", lambda e: e.memset(ST[:], 0.0), writes=[ST])
    identb = P.sbuf("identb_r", [64, 64], BF16)
    P.op("dve", lambda e: e.tensor_copy(out=identb[:], in_=ident[0:64, 0:64]), reads=[ident], writes=[identb])

    Z3 = P.sbuf("Z3", [64, 24, W + 2])
    ZW = P.sbuf("ZW", [64, 2, W + 2])
    ZA = P.sbuf("ZA", [64, 2, W + 2])
    ZG = P.sbuf("ZG", [128, W + 2])
    zs3 = P.sbuf("zs3", [64, 24, W])
    wls = P.sbuf("wls", [64, 2, W])
    als = P.sbuf("als", [64, 2, W])
    gls = P.sbuf("gls", [128, W])
    tmps = P.sbuf("tmps", [128, W])
    icl = [P.sbuf("icl0", [64, 8, W]), P.sbuf("icl1", [64, 8, W])]
    lw = P.sbuf("lw", [64, 8, W])
    kkn = P.sbuf("kkn", [64, 8, W])
    t8a = P.sbuf("t8a", [64, 8, W])
    t8b = P.sbuf("t8b", [64, 8, W])
    bd = P.sbuf("bd", [64, 8, W])
    kd = [P.sbuf("kd0", [64, 8, W]), P.sbuf("kd1", [64, 8, W])]
    cw = P.sbuf("cw", [64, 8, W])
    E1s = [P.sbuf("E1_%d" % i, [64, 8, W]) for i in range(2)]
    vHs = [P.sbuf("vH_%d" % i, [64, 8, W]) for i in range(2)]
    Einv = P.sbuf("Einv", [64, 8, W])
    Ehat = P.sbuf("Ehat", [64, 8, W])
    ARs = [P.sbuf("AR_%d" % i, [64, 8, nch, 128], BF16) for i in range(2)]
    Bts = [P.sbuf("Bt_%d" % i, [64, 8, W], BF16) for i in range(2)]
    Kts = [P.sbuf("Kt_%d" % i, [64, 8, W], BF16) for i in range(2)]
    Bhs = [P.sbuf("Bh_%d" % i, [64, 8, W], BF16) for i in range(2)]
    Khs = [P.sbuf("Kh_%d" % i, [64, 8, W], BF16) for i in range(2)]
    Yw = P.sbuf("Yw", [64, 8, W])
    CH = {n: P.sbuf(n, [64, 8, 64], BF16) for n in
          ("AtT", "BhT", "KhT", "VT", "X0T", "XA", "XB", "XTA", "XTB", "T", "AhT", "Xs", "VhT", "Gs", "Qs")}
    CH["dW"] = P.sbuf("dW", [64, 8, 64])
    MB = P.sbuf("MB", [64, 8, 128], BF16)
    MK = P.sbuf("MK", [64, 8, 128], BF16)
    banks, bbanks, bki = PB

    def bank():
        bki[0] += 1
        return banks[bki[0] % len(banks)]

    def bbank():
        bki[1] += 1
        return bbanks[bki[1] % len(bbanks)]

    ew = EW()
    S3 = [64, 24, W]
    S8 = [64, 8, W]

    def shift3():
        for q in range(3):
            qs = slice(8 * q, 8 * q + 8)
            ctr, prv, nxt = (slice(None), qs, slice(1, W + 1)), (slice(None), qs, slice(0, W)), (slice(None), qs, slice(2, W + 2))
            mup = prm("mup3")[:, qs, None]
            mun = prm("mun3")[:, qs, None]
            e1 = "dve" if q != 1 else "pool"
            tmp = t8a if q != 1 else t8b
            tt(P, e1, tmp[:], Z3[prv], Z3[ctr], ALU.subtract, [Z3], [tmp])
            tt(P, e1, tmp[:], tmp[:], bc(mup, S8), ALU.mult, [tmp, pp], [tmp])
            tt(P, e1, zs3[:, qs, :], tmp[:], Z3[ctr], ALU.add, [tmp, Z3], [zs3])
            tt(P, e1, tmp[:], Z3[nxt], Z3[ctr], ALU.subtract, [Z3], [tmp])
            tt(P, e1, tmp[:], tmp[:], bc(mun, S8), ALU.mult, [tmp, pp], [tmp])
            tt(P, e1, zs3[:, qs, :], zs3[:, qs, :], tmp[:], ALU.add, [tmp, zs3], [zs3])

    zr = dr["zrT"]
    state = {"feat": -1, "chunk": -1}

    def feat():
        for wn, wi in enumerate(order):
            while state["chunk"] < wn - 2:
                yield
            hs = wn % 2
            AR, Bt, Kt, Bh, Kh, E1, vH = ARs[hs], Bts[hs], Kts[hs], Bhs[hs], Khs[hs], E1s[hs], vHs[hs]
            t0 = wi * W
            is_ctx = t0 < CTX
            lo_d, hi_d = (0, CTX) if is_ctx else (CTX, TT)
            lo = max(t0 - 1, lo_d)
            hi = min(t0 + W + 1, hi_d)
            a = lo - (t0 - 1)
            b = a + (hi - lo)
            for Z in (Z3, ZW, ZA, ZG):
                nd = len(Z.t.shape)
                if a > 0:
                    ix = (slice(None),) * (nd - 1) + (slice(0, 1),)
                    P.op("pool", lambda e, Z=Z, ix=ix: e.memset(Z[ix], 0.0), writes=[Z])
                if b < W + 2:
                    ix = (slice(None),) * (nd - 1) + (slice(W + 1, W + 2),)
                    P.op("pool", lambda e, Z=Z, ix=ix: e.memset(Z[ix], 0.0), writes=[Z])
            P.dma("sp", Z3[:, :, a:b], zr[0:1536, lo:hi].rearrange("(q j) t -> j q t", j=64), reads=[zr], writes=[Z3])
            P.dma("sp", ZW[:, :, a:b], zr[1536:1664, lo:hi].rearrange("(q j) t -> j q t", j=64), reads=[zr], writes=[ZW])
            P.dma("sp", ZA[:, :, a:b], zr[1664:1792, lo:hi].rearrange("(q j) t -> j q t", j=64), reads=[zr], writes=[ZA])
            P.dma("sp", ZG[:, a:b], zr[1792:1920, lo:hi], reads=[zr], writes=[ZG])
            yield
            shift3()
            yield
            for (zraw, zs, mupn, munn) in ((ZW, wls, "mup_wl", "mun_wl"), (ZA, als, "mup_al", "mun_al")):
                tv = t8a[:, 0:2, :]
                shp = [64, 2, W]
                c_, p_, n_ = (slice(None), slice(None), slice(1, W + 1)), (slice(None), slice(None), slice(0, W)), (slice(None), slice(None), slice(2, W + 2))
                tt(P, "dve", tv, zraw[p_], zraw[c_], ALU.subtract, [zraw], [t8a])
                tt(P, "dve", tv, tv, bc(prm(mupn)[:, :, None], shp), ALU.mult, [t8a, pp], [t8a])
                tt(P, "dve", zs[:], tv, zraw[c_], ALU.add, [t8a, zraw], [zs])
                tt(P, "dve", tv, zraw[n_], zraw[c_], ALU.subtract, [zraw], [t8a])
                tt(P, "dve", tv, tv, bc(prm(munn)[:, :, None], shp), ALU.mult, [t8a, pp], [t8a])
                tt(P, "dve", zs[:], zs[:], tv, ALU.add, [t8a, zs], [zs])
            mugl = R["mugl"]
            c2, p2, n2 = (slice(None), slice(1, W + 1)), (slice(None), slice(0, W)), (slice(None), slice(2, W + 2))
            tt(P, "dve", tmps[:], ZG[p2], ZG[c2], ALU.subtract, [ZG], [tmps])
            P.op("dve", lambda e: e.scalar_tensor_tensor(out=gls[:], in0=tmps[:], scalar=mugl[:, 0:1], in1=ZG[c2],
                                                         op0=ALU.mult, op1=ALU.add), reads=[tmps, mugl, ZG], writes=[gls])
            tt(P, "dve", tmps[:], ZG[n2], ZG[c2], ALU.subtract, [ZG], [tmps])
            P.op("dve", lambda e: e.scalar_tensor_tensor(out=gls[:], in0=tmps[:], scalar=mugl[:, 1:2], in1=gls[:],
                                                         op0=ALU.mult, op1=ALU.add), reads=[tmps, mugl, gls], writes=[gls])
            yield
            r_ = zs3[:, 0:8, :]
            k_ = zs3[:, 8:16, :]
            v_ = zs3[:, 16:24, :]
            for d2 in ((0, 1) if (dirn == 0 and not is_ctx) else (dirn,)):
                a2 = R["a2_f" if d2 == 0 else "a2_b"]
                pb = [bank(), bank()]
                for h in range(8):
                    q = pb[h // 4]
                    mm(P, q[0:64, (h % 4) * W:(h % 4 + 1) * W], a2[:, h * 64:(h + 1) * 64], als[:, d2, :], True, True,
                       [a2, als], [q])
                a0 = prm("a0_f" if d2 == 0 else "a0_b")
                for g in range(2):
                    tt(P, "dve", icl[d2][:, 4 * g:4 * g + 4, :], pb[g][0:64, 0:4 * W].rearrange("p (h t) -> p h t", h=4),
                       bc(a0[:, 4 * g:4 * g + 4, None], [64, 4, W]), ALU.add, [pb[g], pp], [icl[d2]])
                act(P, icl[d2][:], icl[d2][:], AF.Sigmoid, [icl[d2]], [icl[d2]])
            yield
            th = tmps[0:64, :]
            act(P, th, wls[:, dirn, :], AF.Tanh, [wls], [tmps])
            w2 = R["w2_f" if dirn == 0 else "w2_b"]
            pb = [bank(), bank()]
            for h in range(8):
                q = pb[h // 4]
                mm(P, q[0:64, (h % 4) * W:(h % 4 + 1) * W], w2[:, h * 64:(h + 1) * 64], th, True, True, [w2, tmps], [q])
            w0 = prm("w0_f" if dirn == 0 else "w0_b")
            for g in range(2):
                tt(P, "dve", lw[:, 4 * g:4 * g + 4, :], pb[g][0:64, 0:4 * W].rearrange("p (h t) -> p h t", h=4),
                   bc(w0[:, 4 * g:4 * g + 4, None], [64, 4, W]), ALU.add, [pb[g], pp], [lw])
            act(P, lw[:], lw[:], AF.Sigmoid, [lw], [lw])
            P.op("dve", lambda e: e.tensor_scalar(out=lw[:], in0=lw[:], scalar1=-float(np.exp(-0.5)), scalar2=None,
                                                  op0=ALU.mult), reads=[lw], writes=[lw])
            yield
            tt(P, "pool", t8a[:], k_, bc(prm("k_k")[:, :, None], S8), ALU.mult, [zs3, pp], [t8a])
            tt(P, "pool", t8b[:], t8a[:], t8a[:], ALU.mult, [t8a], [t8b])
            pb = [bank(), bank()]
            for g in range(2):
                mm(P, pb[g][0:64, 0:4 * W], ones[0:64, 0:64], t8b[:, 4 * g:4 * g + 4, :], True, True, [ones, t8b], [pb[g]])
            for g in range(2):
                act(P, kkn[:, 4 * g:4 * g + 4, :], pb[g][0:64, 0:4 * W].rearrange("p (h t) -> p h t", h=4), AF.Ln, [pb[g]], [kkn], bias=R["eps18"][:, 0:1])
            act(P, kkn[:], kkn[:], AF.Exp, [kkn], [kkn], scale=-0.5)
            tt(P, "dve", kkn[:], kkn[:], t8a[:], ALU.mult, [kkn, t8a], [kkn])
            yield
            dirs_needed = (0, 1) if (dirn == 0 and not is_ctx) else (dirn,)
            for d2 in dirs_needed:
                e1 = ew()
                tt(P, e1, kd[d2][:], icl[d2][:], bc(prm("k_a")[:, :, None], S8), ALU.mult, [icl[d2], pp], [kd[d2]])
                tt(P, e1, kd[d2][:], kd[d2][:], bc(R["omka"][:, :, None], S8), ALU.add, [kd[d2], R["omka"]], [kd[d2]])
                tt(P, e1, kd[d2][:], kd[d2][:], k_, ALU.mult, [kd[d2], zs3], [kd[d2]])
            tt(P, "pool", bd[:], kkn[:], icl[dirn][:], ALU.mult, [kkn, icl[dirn]], [bd])
            if dirn == 0 and not is_ctx:
                tt(P, "pool", t8a[:], kd[0][:], kd[1][:], ALU.add, [kd[0], kd[1]], [t8a])
                tt(P, "pool", t8a[:], t8a[:], r_, ALU.mult, [t8a, zs3], [t8a])
                tt(P, "pool", t8a[:], t8a[:], bc(prm("r_k")[:, :, None], S8), ALU.mult, [t8a, pp], [t8a])
                pb = [bank(), bank()]
                for g in range(2):
                    mm(P, pb[g][0:64, 0:4 * W], ones[0:64, 0:64], t8a[:, 4 * g:4 * g + 4, :], True, True, [ones, t8a], [pb[g]])
                for g in range(2):
                    tt(P, "dve", t8b[:, 4 * g:4 * g + 4, :], pb[g][0:64, 0:4 * W].rearrange("p (h t) -> p h t", h=4),
                       v_[:, 4 * g:4 * g + 4, :], ALU.mult, [pb[g], zs3], [t8b])
                P.dma("sp", dr["bonusT"][:, t0 - CTX:t0 - CTX + W].rearrange("(h i) t -> i h t", i=64), t8b[:],
                      reads=[t8b], writes=[dr["bonusT"]])
                sg = tmps
                act(P, sg[:], gls[:], AF.Sigmoid, [gls], [tmps])
                pb = [bank(), bank()]
                g2 = R["g2"]
                for h in range(8):
                    q = pb[h // 4]
                    mm(P, q[0:64, (h % 4) * W:(h % 4 + 1) * W], g2[:, h * 64:(h + 1) * 64], sg[:], True, True, [g2, tmps], [q])
                for g in range(2):
                    act(P, t8a[:, 4 * g:4 * g + 4, :], pb[g][0:64, 0:4 * W].rearrange("p (h t) -> p h t", h=4), AF.Copy, [pb[g]], [t8a])
                P.dma("sp", dr["gateT"][:, t0 - CTX:t0 - CTX + W].rearrange("(h i) t -> i h t", i=64), t8a[:],
                      reads=[t8a], writes=[dr["gateT"]])
            yield
            for h in range(8):
                for c in range(nch):
                    if dirn == 0:
                        sl = slice(c * C, (c + 1) * C)
                    else:
                        sl = slice(c * C + C - 1, (c * C - 1) if c > 0 else None, -1)
                    P.op("dve", lambda e, h=h, sl=sl: e.tensor_tensor_scan(
                        out=cw[:, h, sl], data0=ones[0:64, 0:64], data1=lw[:, h, sl], initial=0.0,
                        op0=ALU.mult, op1=ALU.add), reads=[lw, ones], writes=[cw])
            yield
            act(P, E1[:], cw[:], AF.Exp, [cw], [E1])
            act(P, Einv[:], cw[:], AF.Exp, [cw], [Einv], scale=-1.0)
            tt(P, "pool", t8a[:], cw[:], lw[:], ALU.subtract, [cw, lw], [t8a])
            act(P, t8a[:], t8a[:], AF.Exp, [t8a], [t8a])
            cend = C - 1 if dirn == 0 else 0
            E1v = E1[:].rearrange("p h (c t) -> p h c t", t=C)
            Wc = E1v[:, :, :, cend:cend + 1]
            tt(P, "pool", Ehat[:].rearrange("p h (c t) -> p h c t", t=C), Einv[:].rearrange("p h (c t) -> p h c t", t=C),
               bc(Wc, [64, 8, nch, C]), ALU.mult, [Einv, E1], [Ehat])
            P.op("dve", lambda e, AR=AR: e.scalar_tensor_tensor(
                out=AR[:, :, :, 0:64], in0=kkn[:].rearrange("p h (c t) -> p h c t", t=C), scalar=-1.0,
                in1=t8a[:].rearrange("p h (c t) -> p h c t", t=C), op0=ALU.mult, op1=ALU.mult),
                reads=[kkn, t8a], writes=[AR])
            tt(P, "pool", AR[:, :, :, 64:128], r_.rearrange("p h (c t) -> p h c t", t=C), E1v, ALU.mult, [zs3, E1], [AR])
            tt(P, "dve", Bt[:], bd[:], Einv[:], ALU.mult, [bd, Einv], [Bt])
            tt(P, "pool", Kt[:], kd[dirn][:], Einv[:], ALU.mult, [kd[dirn], Einv], [Kt])
            tt(P, "dve", Bh[:], bd[:], Ehat[:], ALU.mult, [bd, Ehat], [Bh])
            tt(P, "pool", Kh[:], kd[dirn][:], Ehat[:], ALU.mult, [kd[dirn], Ehat], [Kh])

            act(P, vH[:], zs3[:, 16:24, :], AF.Copy, [zs3], [vH])
            state["feat"] = wn
            yield

    def chunk():
        for wn, wi in enumerate(order):
            while state["feat"] < wn:
                yield
            hs = wn % 2
            AR, Bt, Kt, Bh, Kh, E1, vH = ARs[hs], Bts[hs], Kts[hs], Bhs[hs], Khs[hs], E1s[hs], vHs[hs]
            t0 = wi * W
            is_ctx = t0 < CTX
            cend = C - 1 if dirn == 0 else 0
            corder = range(nch) if dirn == 0 else range(nch - 1, -1, -1)
            for c in corder:
                sl = slice(c * C, (c + 1) * C)
                for (srcb, srcf, dst) in ((AR, lambda h: AR[:, h, c, 0:64], "AtT"), (Bh, lambda h: Bh[:, h, sl], "BhT"),
                                          (Kh, lambda h: Kh[:, h, sl], "KhT")):
                    q = bbank()
                    for h in range(8):
                        tr(P, q[0:64, h * 64:(h + 1) * 64], srcf(h), identb[:], [srcb, identb], [q])
                    act(P, CH[dst][:], q[0:64, 0:512].rearrange("p (h t) -> p h t", h=8), AF.Copy, [q], [CH[dst]])
                q = bank()
                for h in range(8):
                    tr(P, q[0:64, h * 64:(h + 1) * 64], vH[:, h, sl], idn, [vH, ident], [q])
                act(P, CH["VT"][:], q[0:64, :].rearrange("p (h t) -> p h t", h=8), AF.Copy, [q], [CH["VT"]])
                yield
                for (L, dstM) in ((Bt, MB), (Kt, MK)):
                    pb = [bank(), bank()]
                    for h in range(8):
                        q = pb[h // 4]
                        mm(P, q[0:64, (h % 4) * 128:(h % 4 + 1) * 128], L[:, h, sl], AR[:, h, c, :], True, True, [L, AR], [q])
                    for g in range(2):
                        tt(P, "dve", dstM[:, 4 * g:4 * g + 4, :], pb[g][0:64, :].rearrange("p (h t) -> p h t", h=4),
                           bc(mAR[:, None, :], [64, 4, 128]), ALU.mult, [pb[g], mAR], [dstM])
                q = bank()
                for h in range(8):
                    mm(P, q[0:64, h * 64:(h + 1) * 64], AR[:, h, c, 0:64], Bt[:, h, sl], True, True, [AR, Bt], [q])
                X0T = CH["X0T"]
                tt(P, "dve", X0T[:], q[0:64, :].rearrange("p (h t) -> p h t", h=8), bc(mNT[:, None, :], [64, 8, 64]),
                   ALU.mult, [q, mNT], [X0T])
                yield
                q = bank()
                for h in range(8):
                    mm(P, q[0:64, h * 64:(h + 1) * 64], MK[:, h, 0:64], CH["VT"][:, h, :], True, True, [MK, CH["VT"]], [q])
                act(P, CH["Xs"][:], q[0:64, :].rearrange("p (h t) -> p h t", h=8), AF.Copy, [q], [CH["Xs"]])
                yield
                T = CH["T"]
                tt(P, "dve", T[:], MB[:, :, 0:64], bc(idn[:, None, :], [64, 8, 64]), ALU.add, [MB, ident], [T])
                Xc_b, XTc_b = MB, X0T
                Xc = lambda h: MB[:, h, 0:64]
                XTc = lambda h: X0T[:, h, :]
                for k in range(1, 6):
                    Xn_b = CH["XA"] if k % 2 else CH["XB"]
                    XTn_b = CH["XTA"] if k % 2 else CH["XTB"]
                    q1 = bank()
                    for h in range(8):
                        mm(P, q1[0:64, h * 64:(h + 1) * 64], Xc(h), XTc(h), True, True, [Xc_b, XTc_b], [q1])
                    act(P, XTn_b[:], q1[0:64, :].rearrange("p (h t) -> p h t", h=8), AF.Copy, [q1], [XTn_b])
                    if k < 5:
                        q2 = bank()
                        for h in range(8):
                            mm(P, q2[0:64, h * 64:(h + 1) * 64], XTc(h), Xc(h), True, True, [Xc_b, XTc_b], [q2])
                        act(P, Xn_b[:], q2[0:64, :].rearrange("p (h t) -> p h t", h=8), AF.Copy, [q2], [Xn_b])
                    yield
                    q3 = bank()
                    for h in range(8):
                        mm(P, q3[0:64, h * 64:(h + 1) * 64], XTn_b[:, h, :], T[:, h, :], True, True, [XTn_b, T], [q3])
                    tt(P, "dve", T[:], T[:], q3[0:64, :].rearrange("p (h t) -> p h t", h=8), ALU.add, [T, q3], [T])
                    yield
                    Xc_b, XTc_b = Xn_b, XTn_b
                    Xc = lambda h, b_=Xn_b: b_[:, h, :]
                    XTc = lambda h, b_=XTn_b: b_[:, h, :]
                yield
                q = bank()
                for h in range(8):
                    mm(P, q[0:64, h * 64:(h + 1) * 64], T[:, h, :], CH["AtT"][:, h, :], True, True, [T, CH["AtT"]], [q])
                act(P, CH["AhT"][:], q[0:64, :].rearrange("p (h t) -> p h t", h=8), AF.Copy, [q], [CH["AhT"]])
                q = bank()
                for h in range(8):
                    mm(P, q[0:64, h * 64:(h + 1) * 64], T[:, h, :], CH["Xs"][:, h, :], True, True, [T, CH["Xs"]], [q])
                act(P, CH["VhT"][:], q[0:64, :].rearrange("p (h t) -> p h t", h=8), AF.Copy, [q], [CH["VhT"]])
                yield
                tt(P, "pool", CH["dW"][:], bc(idn[:, None, :], [64, 8, 64]),
                   bc(E1[:, :, c * C + cend:c * C + cend + 1], [64, 8, 64]), ALU.mult, [ident, E1], [CH["dW"]])
                q = bank()
                for h in range(8):
                    mm(P, q[0:64, h * 64:(h + 1) * 64], CH["AhT"][:, h, :], CH["BhT"][:, h, :], True, True,
                       [CH["AhT"], CH["BhT"]], [q])
                tt(P, "dve", CH["Gs"][:], q[0:64, :].rearrange("p (h t) -> p h t", h=8), CH["dW"][:], ALU.add,
                   [q, CH["dW"]], [CH["Gs"]])
                if not is_ctx:
                    q = bank()
                    for h in range(8):
                        mm(P, q[0:64, h * 64:(h + 1) * 64], CH["AhT"][:, h, :], MB[:, h, 64:128], True, True,
                           [CH["AhT"], MB], [q])
                    tt(P, "dve", CH["Qs"][:], q[0:64, :].rearrange("p (h t) -> p h t", h=8), AR[:, :, c, 64:128], ALU.add,
                       [q, AR], [CH["Qs"]])
                    q = bank()
                    for h in range(8):
                        o_ = q[0:64, h * 64:(h + 1) * 64]
                        mm(P, o_, ST[:, h, :], CH["Qs"][:, h, :], True, False, [ST, CH["Qs"]], [q])
                        mm(P, o_, CH["VhT"][:, h, :], MB[:, h, 64:128], False, False, [CH["VhT"], MB], [q])
                        mm(P, o_, CH["VT"][:, h, :], MK[:, h, 64:128], False, True, [CH["VT"], MK], [q])
                    act(P, Yw[:, :, sl], q[0:64, :].rearrange("p (h t) -> p h t", h=8), AF.Copy, [q], [Yw])
                yield
                q = bank()
                for h in range(8):
                    o_ = q[0:64, h * 64:(h + 1) * 64]
                    mm(P, o_, CH["Gs"][:, h, :], ST[:, h, :], True, False, [CH["Gs"], ST], [q])
                    mm(P, o_, CH["BhT"][:, h, :], CH["VhT"][:, h, :], False, False, [CH["BhT"], CH["VhT"]], [q])
                    mm(P, o_, CH["KhT"][:, h, :], CH["VT"][:, h, :], False, True, [CH["KhT"], CH["VT"]], [q])
                act(P, ST[:], q[0:64, :].rearrange("p (h t) -> p h t", h=8), AF.Copy, [q], [ST])
            if not is_ctx:
                yT = dr["yT" + dsuf]
                P.dma("sp", yT[:, t0 - CTX:t0 - CTX + W].rearrange("(h i) t -> i h t", i=64), Yw[:], reads=[Yw], writes=[yT])
            if dbg is not None and wi == (nctx - 1 if dirn == 0 else 0) and "ST" + dsuf in dbg:
                P.dma("sp", dbg["ST" + dsuf][:].rearrange("(h j) i -> j h i", j=64), ST[:], reads=[ST], writes=[dbg["ST" + dsuf]])


            state["chunk"] = wn
            yield

    return feat(), chunk()


def phase_adaln(P, cs, dr):
    out = {}
    for n in ("Ax", "Bx", "Ac", "Bc", "Af", "Bf"):
        out[n] = P.sbuf(n, [128, 8])
    out["G2"] = P.sbuf("G2", [128, 1024])
    out["G5"] = P.sbuf("G5", [128, 1024])
    out["Arow"] = P.sbuf("Arow", [128, 1024])
    out["Brow"] = P.sbuf("Brow", [128, 1024])
    m0 = P.mark()
    ones = cs["ones"]
    S16 = P.sbuf("S16", [128, 16])
    P.dma("sp", S16[:], dr["ccol"][:], writes=[S16])
    act(P, S16[:], S16[:], AF.Silu, [S16], [S16])
    Sbc = P.sbuf("Sbc", [128, 8, 128])
    tt(P, "dve", Sbc[:], S16[:, 0:8, None].broadcast_to([128, 8, 128]), ones[:, None, :].broadcast_to([128, 8, 128]),
       ALU.mult, [S16, ones], [Sbc])
    bada = P.sbuf("bada", [128, 48])
    P.dma("sp", bada[:], dr["bada_col"][:], writes=[bada])
    gcols = P.sbuf("gcols", [128, 16])
    P.dma("sp", gcols[:], dr["gcols"][:], writes=[gcols])
    modT = P.sbuf("modT", [128, 48, 2])
    wst = [P.sbuf("wada0", [128, 8, 1024]), P.sbuf("wada1", [128, 8, 1024])]
    brow = P.sbuf("brow", [128, 1024])
    grow = P.sbuf("grow", [128, 1024])
    pm = P.psum("pm", [128, 512])
    pg = [P.psum("pg0", [128, 512]), P.psum("pg1", [128, 512])]
    wv = dr["w_ada"].t.rearrange("(k p) n -> p k n", p=128)
    for m in range(6):
        wb = wst[m % 2]
        for k in range(8):
            P.dma("sp" if k % 2 == 0 else "act", wb[:, k, :], wv[:, k, m * 1024:(m + 1) * 1024], reads=[dr["w_ada"]], writes=[wb])
        for jj in range(8):
            j = m * 8 + jj
            for k in range(8):
                mm(P, pm[:, 2 * jj:2 * jj + 2], wb[:, k, jj * 128:(jj + 1) * 128], S16[:, k:16:8], k == 0, k == 7, [wb, S16], [pm])
        tt(P, "dve", modT[:, m * 8:(m + 1) * 8, :], pm[:, 0:16].rearrange("p (j c) -> p j c", c=2),
           bada[:, m * 8:(m + 1) * 8, None].broadcast_to([128, 8, 2]), ALU.add, [pm, bada], [modT])
        if m in (2, 3, 4, 5):
            G = out[{2: "G2", 3: "Brow", 4: "Arow", 5: "G5"}[m]]
            gsrc = {2: dr["g_post_mix"], 5: dr["g_post_ffn"], 4: dr["g_pre_ffn"], 3: None}[m]
            P.dma("sp", brow[:], dr["b_ada"][0:1, m * 1024:(m + 1) * 1024].broadcast_to([128, 1024]), reads=[dr["b_ada"]], writes=[brow])
            if gsrc is not None:
                P.dma("sp", grow[:], gsrc[0:1, :].broadcast_to([128, 1024]), reads=[gsrc], writes=[grow])
            for hf in range(2):
                for k in range(8):
                    mm(P, pg[hf][:], Sbc[:, k, :], wb[:, k, hf * 512:(hf + 1) * 512], k == 0, k == 7, [Sbc, wb], [pg[hf]])
                tt(P, "dve", G[:, hf * 512:(hf + 1) * 512], pg[hf][:], brow[:, hf * 512:(hf + 1) * 512], ALU.add, [pg[hf], brow], [G])
            if m == 4:
                P.op("dve", lambda e, G=G: e.scalar_tensor_tensor(out=G[:], in0=G[:], scalar=1.0, in1=grow[:], op0=ALU.add, op1=ALU.mult),
                     reads=[G, grow], writes=[G])
            elif gsrc is not None:
                tt(P, "dve", G[:], G[:], grow[:], ALU.mult, [G, grow], [G])
    for (A, B, gsl, msc, msh, ci) in ((out["Ax"], out["Bx"], slice(0, 8), 1, 0, 0), (out["Ac"], out["Bc"], slice(0, 8), 1, 0, 1),
                                      (out["Af"], out["Bf"], slice(8, 16), 4, 3, 0)):
        P.op("dve", lambda e, A=A, msc=msc, ci=ci, gsl=gsl: e.scalar_tensor_tensor(
            out=A[:], in0=modT[:, msc * 8:(msc + 1) * 8, ci], scalar=1.0, in1=gcols[:, gsl], op0=ALU.add, op1=ALU.mult),
            reads=[modT, gcols], writes=[A])
        P.op("dve", lambda e, B=B, msh=msh, ci=ci: e.tensor_copy(out=B[:], in_=modT[:, msh * 8:(msh + 1) * 8, ci]),
             reads=[modT], writes=[B])
    P.barrier()
    P.release(m0)
    return out


def rms_rstd(P, ss, rstd, n, eps):
    P.op("dve", lambda e: e.tensor_scalar(out=rstd[:], in0=ss[:], scalar1=1.0 / n, scalar2=eps, op0=ALU.mult, op1=ALU.add),
         reads=[ss], writes=[rstd])
    act(P, rstd[:], rstd[:], AF.Sqrt, [rstd], [rstd])
    P.op("dve", lambda e: e.reciprocal(out=rstd[:], in_=rstd[:]), reads=[rstd], writes=[rstd])


NCH_W = 42


def phase_proj(P, cs, mod, dr):
    m0 = P.mark()
    ident = cs["ident"]
    ones = cs["ones"]
    hT = P.sbuf("hT", [128, 8, TT], BF16)
    xt = [P.sbuf("xt0", [128, 1024]), P.sbuf("xt1", [128, 1024])]
    junk = P.sbuf("junk", [128, 1024])
    ss = P.sbuf("ss", [128, 1])
    rstd = P.sbuf("rstd", [128, 1])
    pt = [P.psum("pt%d" % i, [128, 512]) for i in range(4)]
    for ti in range(TT // 128):
        X = xt[ti % 2]
        if ti < 2:
            src, srcb = dr["ctx"][ti * 128:(ti + 1) * 128, :], dr["ctx"]
            A, B = mod["Ac"], mod["Bc"]
        else:
            src, srcb = dr["x"][(ti - 2) * 128:(ti - 1) * 128, :], dr["x"]
            A, B = mod["Ax"], mod["Bx"]
        P.dma("sp", X[:], src, reads=[srcb], writes=[X])
        P.op("act", lambda e, X=X: e.activation(out=junk[:], in_=X[:], func=AF.Square, accum_out=ss[:]), reads=[X], writes=[junk, ss])
        rms_rstd(P, ss, rstd, 1024.0, 1e-6)
        P.op("dve", lambda e, X=X: e.tensor_scalar(out=X[:], in0=X[:], scalar1=rstd[:, 0:1], scalar2=None, op0=ALU.mult),
             reads=[X, rstd], writes=[X])
        for k in range(8):
            q = pt[(ti % 2) * 2 + k // 4]
            tr(P, q[:, (k % 4) * 128:(k % 4 + 1) * 128], X[:, k * 128:(k + 1) * 128], ident[:], [X, ident], [q])
        for k in range(8):
            q = pt[(ti % 2) * 2 + k // 4]
            P.op("dve" if k % 2 else "act", (lambda e, q=q, k=k, A=A, B=B, ti=ti: e.tensor_scalar(
                out=hT[:, k, ti * 128:(ti + 1) * 128], in0=q[:, (k % 4) * 128:(k % 4 + 1) * 128], scalar1=A[:, k:k + 1],
                scalar2=B[:, k:k + 1], op0=ALU.mult, op1=ALU.add)) if k % 2 else (lambda e, q=q, k=k, A=A, B=B, ti=ti: e.activation(
                    out=hT[:, k, ti * 128:(ti + 1) * 128], in_=q[:, (k % 4) * 128:(k % 4 + 1) * 128], func=AF.Identity,
                    scale=A[:, k:k + 1], bias=B[:, k:k + 1])), reads=[q, A, B], writes=[hT])
    bcol = P.sbuf("bcol", [128, NCH_W])
    P.dma("sp", bcol[:], dr["bin_col"][:], writes=[bcol])
    bv = P.sbuf("bv", [1, 128])
    P.dma("sp", bv[:], dr["bv_row"][:], writes=[bv])
    NWS = 5
    wst = [P.sbuf("wst%d" % i, [128, 8, 128]) for i in range(NWS)]
    wbf = [P.sbuf("wbf%d" % i, [128, 8, 128], BF16) for i in range(NWS)]
    cosb = P.sbuf("cosb", [128, 512])
    sinb = P.sbuf("sinb", [128, 512])
    ot = [P.sbuf("ot%d" % i, [128, 512]) for i in range(2)]
    otb = [P.sbuf("otb%d" % i, [128, 512], BF16) for i in range(2)]
    t1 = P.sbuf("rp1", [128, 512])
    pp = [P.psum("pp%d" % i, [128, 512]) for i in range(4)]
    wv = dr["w_in"].t.rearrange("(k p) n -> p k n", p=128)
    groups = [(0, 256)] + [(256 + 512 * g, 512) for g in range(8)]
    wi = [0]
    oi = [0]

    def load_w(j):
        s = wi[0] % NWS
        wi[0] += 1
        P.dma("act", wst[s][:], wv[:, :, j * 128:(j + 1) * 128], reads=[dr["w_in"]], writes=[wst[s]])
        P.op("pool", lambda e: e.tensor_copy(out=wbf[s][:], in_=wst[s][:]), reads=[wst[s]], writes=[wbf[s]])
        return wbf[s]

    def proj(q, wb, t0, n):
        for k in range(8):
            mm(P, q[:, 0:n], wb[:, k, :], hT[:, k, t0:t0 + n], k == 0, k == 7, [wb, hT], [q])

    jobs = list(range(0, 5)) + list(range(6, 37))

    def load_job(j):
        return load_w(j), (load_w(37 + j) if j < 5 else None)

    nxt = load_job(jobs[0])
    for ji, j in enumerate(jobs):
        wb, wr = nxt
        if ji + 1 < len(jobs):
            nxt = load_job(jobs[ji + 1])
        for gi, (t0, n) in enumerate(groups):
            if gi == 0 and not (j == 4 or 6 <= j <= 20):
                continue
            q = pp[oi[0] % 2]
            proj(q, wb, t0, n)
            o = oi[0] % 2
            oi[0] += 1
            if j < 5 and gi > 0:
                q2 = pp[2 + o]
                proj(q2, wr, t0, n)
                xs = t0 - CTX
                P.dma("sp", cosb[:], dr["cos_t"][:, xs:xs + 512], reads=[dr["cos_t"]], writes=[cosb])
                P.dma("sp", sinb[:], dr["sin_t"][:, xs:xs + 512], reads=[dr["sin_t"]], writes=[sinb])
                P.op("dve", lambda e, q=q, j=j: e.scalar_tensor_tensor(out=t1[:], in0=q[:], scalar=bcol[:, j:j + 1], in1=cosb[:],
                                                                      op0=ALU.add, op1=ALU.mult), reads=[q, bcol, cosb], writes=[t1])
                P.op("dve", lambda e, q2=q2, j=j, o=o: e.scalar_tensor_tensor(out=ot[o][:], in0=q2[:], scalar=bcol[:, 37 + j:38 + j], in1=sinb[:],
                                                                             op0=ALU.add, op1=ALU.mult), reads=[q2, bcol, sinb], writes=[ot[o]])
                tt(P, "pool", otb[o][:], ot[o][:], t1[:], ALU.add, [ot[o], t1], [otb[o]])
                if j < 4:
                    P.dma("sp", dr["qT"][j * 128:(j + 1) * 128, xs:xs + 512], otb[o][:], reads=[otb[o]], writes=[dr["qT"]])
                else:
                    P.dma("sp", dr["kT"][:, t0:t0 + 512], otb[o][:], reads=[otb[o]], writes=[dr["kT"]])
            elif j == 4:
                act(P, otb[o][:, 0:n], q[:, 0:n], AF.Identity, [q, bcol], [otb[o]], bias=bcol[:, j:j + 1])
                P.dma("sp", dr["kT"][:, t0:t0 + n], otb[o][:, 0:n], reads=[otb[o]], writes=[dr["kT"]])
            elif j <= 20:
                act(P, ot[o][:, 0:n], q[:, 0:n], AF.Identity, [q, bcol], [ot[o]], bias=bcol[:, j:j + 1])
                P.dma("sp", dr["zrT"][(j - 6) * 128:(j - 5) * 128, t0:t0 + n], ot[o][:, 0:n], reads=[ot[o]], writes=[dr["zrT"]])
            else:
                act(P, otb[o][:], q[:], AF.Sigmoid, [q, bcol], [otb[o]], bias=bcol[:, j:j + 1])
                P.dma("sp", dr["sgT"][(j - 21) * 128:(j - 20) * 128, t0 - CTX:t0 - CTX + 512], otb[o][:], reads=[otb[o]], writes=[dr["sgT"]])
    wb = load_w(5)
    onesb = P.sbuf("onesb", [1, 128], BF16)
    bvb = P.sbuf("bvb", [1, 128], BF16)
    P.op("dve", lambda e: e.tensor_copy(out=onesb[:], in_=ones[0:1, :]), reads=[ones], writes=[onesb])
    P.op("dve", lambda e: e.tensor_copy(out=bvb[:], in_=bv[:]), reads=[bv], writes=[bvb])
    vt = [P.sbuf("vt%d" % i, [128, 128], BF16) for i in range(2)]
    for ti in range(TT // 128):
        q = pp[ti % 4]
        for k in range(8):
            mm(P, q[:, 0:128], hT[:, k, ti * 128:(ti + 1) * 128], wb[:, k, :], k == 0, False, [wb, hT], [q])
        mm(P, q[:, 0:128], onesb[:], bvb[:], False, True, [onesb, bvb], [q])
        V = vt[ti % 2]
        act(P, V[:], q[:, 0:128], AF.Copy, [q], [V])
        P.dma("sp", dr["vtok"][ti * 128:(ti + 1) * 128, :], V[:], reads=[V], writes=[dr["vtok"]])
    P.barrier()
    P.release(m0)


def phase_attn(P, cs, dr):
    m0 = P.mark()
    ones = cs["ones"]
    qT = P.sbuf("qTs", [64, 8, SEQ], BF16)
    kT = P.sbuf("kTs", [64, 2, TT], BF16)
    vt = P.sbuf("vts", [128, TT // 128, 128], BF16)
    for h in range(8):
        P.dma("sp" if h % 2 else "act", qT[:, h, :], dr["qT"][h * 64:(h + 1) * 64, :], reads=[dr["qT"]], writes=[qT])
    for g in range(2):
        P.dma("sp", kT[:, g, :], dr["kT"][g * 64:(g + 1) * 64, :], reads=[dr["kT"]], writes=[kT])
    P.dma("sp", vt[:], dr["vtok"].t.rearrange("(n p) c -> p n c", p=128), reads=[dr["vtok"]], writes=[vt])
    esr = P.sbuf("esr", [1, 1024])
    esb = P.sbuf("esb", [1, 1024], BF16)
    P.dma("sp", esr[:], dr["sink_row"][:], writes=[esr])
    act(P, esb[:], esr[:], AF.Exp, [esr], [esb])
    onesb = P.sbuf("onesb2", [128, 64], BF16)
    P.op("dve", lambda e: e.tensor_copy(out=onesb[:], in_=ones[:, 0:64]), reads=[ones], writes=[onesb])
    mL = P.sbuf("mL", [128, 128], BF16)
    mR = P.sbuf("mR", [128, 128], BF16)
    P.op("dve", lambda e: e.tensor_copy(out=mL[:], in_=cs["Uincl"][:]), reads=[cs["Uincl"]], writes=[mL])
    P.op("dve", lambda e: e.tensor_copy(out=mR[:], in_=cs["Lincl"][:]), reads=[cs["Lincl"]], writes=[mR])
    ps = [P.psum("ps%d" % i, [128, 512]) for i in range(4)]
    po = [P.psum("po%d" % i, [128, 512]) for i in range(2)]
    pd = [P.psum("pd%d" % i, [128, 512]) for i in range(2)]
    pT = [P.sbuf("pT%d" % i, [128, 512], BF16) for i in range(6)]
    rden = P.sbuf("rden", [64, 512])
    ao = [P.sbuf("ao%d" % i, [64, 512], BF16) for i in range(2)]
    si = 0
    for n in range(SEQ // 128):
        blocks = []
        if n > 0:
            blocks.append((2 + n - 1, mL))
        blocks.append((2 + n, None))
        if n < SEQ // 128 - 1:
            blocks.append((2 + n + 1, mR))
        blocks += [(0, None), (1, None)]
        for g in range(2):
            o = po[g]
            d = pd[g]
            rhs_q = qT[:, 4 * g:4 * g + 4, n * 128:(n + 1) * 128]
            for bi, (kb, msk) in enumerate(blocks):
                s = ps[si % 4]
                p = pT[si % 6]
                si += 1
                mm(P, s[:].rearrange("p (h t) -> p h t", h=4), kT[:, g, kb * 128:(kb + 1) * 128], rhs_q, True, True, [kT, qT], [s])
                act(P, p[:], s[:], AF.Exp, [s], [p], scale=0.125)
                if msk is not None:
                    tt(P, "pool", p[:].rearrange("p (h t) -> p h t", h=4), p[:].rearrange("p (h t) -> p h t", h=4),
                       msk[:, None, :].broadcast_to([128, 4, 128]), ALU.mult, [p, msk], [p])
                mm(P, o[0:64, :], vt[:, kb, g * 64:(g + 1) * 64], p[:], bi == 0, bi == len(blocks) - 1, [vt, p], [o])
                mm(P, d[0:64, :], onesb[:], p[:], bi == 0, False, [onesb, p], [d])
            mm(P, d[0:64, :], onesb[0:1, :], esb[0:1, g * 512:(g + 1) * 512], False, True, [onesb, esb], [d])
            P.op("dve", lambda e, d=d: e.reciprocal(out=rden[:], in_=d[0:64, :]), reads=[d], writes=[rden])
            A = ao[g]
            tt(P, "dve", A[:], o[0:64, :], rden[:], ALU.mult, [o, rden], [A])
            P.dma("sp", dr["attT"][g * 256:(g + 1) * 256, n * 128:(n + 1) * 128].rearrange("(h d) t -> d h t", d=64),
                  A[:].rearrange("p (h t) -> p h t", h=4), reads=[A], writes=[dr["attT"]])
    P.barrier()
    P.release(m0)


def phase_rwkv_out(P, cs, R, dr):
    m0 = P.mark()
    ones = cs["ones"]
    pp = R["pp"]
    N = 512

    def prm(n):
        o, w = P64[n]
        return pp[:, o:o + w]
    yf = P.sbuf("yf", [64, 8, N])
    yb = P.sbuf("yb", [64, 8, N])
    bo = P.sbuf("bo", [64, 8, N])
    ga = P.sbuf("ga", [64, 8, N])
    sq = P.sbuf("sq", [64, 8, N])
    ob = P.sbuf("ob", [64, 8, N], BF16)
    pb = [P.psum("pr%d" % i, [128, 512]) for i in range(8)]
    for g in range(SEQ // N):
        ts = slice(g * N, (g + 1) * N)
        for (buf, nm, q) in ((yf, "yT_f", "sp"), (yb, "yT_b", "act"), (bo, "bonusT", "sp"), (ga, "gateT", "act")):
            P.dma(q, buf[:], dr[nm][:, ts].rearrange("(h i) t -> i h t", i=64), reads=[dr[nm]], writes=[buf])
        tt(P, "pool", yf[:], yf[:], yb[:], ALU.add, [yf, yb], [yf])
        for h in range(8):
            mm(P, pb[h][0:64, :], ones[0:64, 0:64], yf[:, h, :], True, True, [ones, yf], [pb[h]])
        for h in range(8):
            P.op("dve", lambda e, h=h: e.scalar_tensor_tensor(out=yf[:, h, :], in0=pb[h][0:64, :], scalar=-1.0 / 64, in1=yf[:, h, :],
                                                             op0=ALU.mult, op1=ALU.add), reads=[pb[h], yf], writes=[yf])
        tt(P, "pool", sq[:], yf[:], yf[:], ALU.mult, [yf], [sq])
        for h in range(8):
            mm(P, pb[h][0:64, :], ones[0:64, 0:64], sq[:, h, :], True, True, [ones, sq], [pb[h]])
        for h in range(8):
            P.op("dve", lambda e, h=h: e.tensor_scalar(out=sq[:, h, :], in0=pb[h][0:64, :], scalar1=1.0 / 64, scalar2=64e-5,
                                                      op0=ALU.mult, op1=ALU.add), reads=[pb[h]], writes=[sq])
        act(P, sq[:], sq[:], AF.Ln, [sq], [sq])
        act(P, sq[:], sq[:], AF.Exp, [sq], [sq], scale=-0.5)
        tt(P, "dve", yf[:], yf[:], sq[:], ALU.mult, [yf, sq], [yf])
        tt(P, "pool", yf[:], yf[:], prm("ln_w")[:, :, None].broadcast_to([64, 8, N]), ALU.mult, [yf, pp], [yf])
        tt(P, "pool", yf[:], yf[:], prm("ln_b")[:, :, None].broadcast_to([64, 8, N]), ALU.add, [yf, pp], [yf])
        tt(P, "dve", yf[:], yf[:], bo[:], ALU.add, [yf, bo], [yf])
        tt(P, "dve", ob[:], yf[:], ga[:], ALU.mult, [yf, ga], [ob])
        P.dma("sp", dr["rwkT"][:, ts].rearrange("(h i) t -> i h t", i=64), ob[:], reads=[ob], writes=[dr["rwkT"]])
    P.barrier()
    P.release(m0)


def phase_merge(P, cs, mod, dr, rt):
    m0 = P.mark()
    ident = cs["ident"]
    ones = cs["ones"]
    N = 512
    wua = P.sbuf("wua", [128, 4, 1024], BF16)
    wur = P.sbuf("wur", [128, 4, 1024], BF16)
    wo = P.sbuf("wo", [128, 8, 1024], BF16)
    stg = P.sbuf("stg", [128, 8, 1024])
    P.dma("sp", stg[:, 0:4, :], dr["w_up_att"].t.rearrange("(c p) n -> p c n", p=128), reads=[dr["w_up_att"]], writes=[stg])
    P.op("pool", lambda e: e.tensor_copy(out=wua[:], in_=stg[:, 0:4, :]), reads=[stg], writes=[wua])
    P.dma("sp", stg[:, 0:4, :], dr["w_up_rwkv"].t.rearrange("(c p) n -> p c n", p=128), reads=[dr["w_up_rwkv"]], writes=[stg])
    P.op("pool", lambda e: e.tensor_copy(out=wur[:], in_=stg[:, 0:4, :]), reads=[stg], writes=[wur])
    P.dma("sp", stg[:], dr["w_out"].t.rearrange("(k p) n -> p k n", p=128), reads=[dr["w_out"]], writes=[stg])
    P.op("pool", lambda e: e.tensor_copy(out=wo[:], in_=stg[:]), reads=[stg], writes=[wo])
    wr = P.sbuf("wr", [128, 8, 32])
    P.dma("sp", wr[:], dr["w_router"].t.rearrange("(k p) n -> p k n", p=128), reads=[dr["w_router"]], writes=[wr])
    br = P.sbuf("br", [1, 32])
    P.dma("sp", br[:], dr["b_router"][:], writes=[br])
    aT = P.sbuf("aT", [128, 4, N], BF16)
    rT = P.sbuf("rT", [128, 4, N], BF16)
    sg = P.sbuf("sg", [128, 16, N], BF16)
    mT = P.sbuf("mT", [128, 8, N], BF16)
    m1 = P.sbuf("m1", [128, N])
    m2 = P.sbuf("m2", [128, N])
    xt = P.sbuf("xtd", [128, 1024])
    x1 = P.sbuf("x1d", [128, 1024])
    junk = P.sbuf("junkd", [128, 1024])
    ssa = P.sbuf("ssa", [128, 2])
    ss = P.sbuf("ssd", [128, 1])
    rstd = P.sbuf("rstdd", [128, 1])
    hf32 = P.sbuf("hf32", [128, 8, 128])
    hfrow = P.sbuf("hfrow", [128, 1024], BF16)
    lg = P.sbuf("lg", [128, 32])
    m8 = P.sbuf("m8", [128, 8])
    nmx = P.sbuf("nmx", [128, 1])
    msk = P.sbuf("msk", [128, 32])
    ex = P.sbuf("ex", [128, 32])
    sm = P.sbuf("sm", [128, 1])
    gts = P.sbuf("gts", [32, 128])
    pa = P.psum("pa", [128, 512]); pr = P.psum("prr", [128, 512])
    pm = [P.psum("pmx0", [128, 512]), P.psum("pmx1", [128, 512])]
    ptr = [P.psum("ptr0", [128, 512]), P.psum("ptr1", [128, 512])]
    pl = P.psum("pl", [128, 512]); pg = P.psum("pgt", [128, 512])
    for g in range(SEQ // N):
        ts = slice(g * N, (g + 1) * N)
        P.dma("sp", aT[:], dr["attT"][:, ts].rearrange("(c p) t -> p c t", p=128), reads=[dr["attT"]], writes=[aT])
        P.dma("act", rT[:], dr["rwkT"][:, ts].rearrange("(c p) t -> p c t", p=128), reads=[dr["rwkT"]], writes=[rT])
        P.dma("sp", sg[:], dr["sgT"][:, ts].rearrange("(c p) t -> p c t", p=128), reads=[dr["sgT"]], writes=[sg])
        for dc in range(8):
            for h in range(4):
                mm(P, pa[:], wua[:, h, dc * 128:(dc + 1) * 128], aT[:, h, :], h == 0, h == 3, [wua, aT], [pa])
            for h in range(4):
                mm(P, pr[:], wur[:, h, dc * 128:(dc + 1) * 128], rT[:, h, :], h == 0, h == 3, [wur, rT], [pr])
            tt(P, "dve", m1[:], pa[:], sg[:, dc, :], ALU.mult, [pa, sg], [m1])
            tt(P, "dve", m2[:], pr[:], sg[:, 8 + dc, :], ALU.mult, [pr, sg], [m2])
            tt(P, "pool", mT[:, dc, :], m1[:], m2[:], ALU.add, [m1, m2], [mT])
        for t4 in range(4):
            tok = g * N + t4 * 128
            P.dma("act", xt[:], dr["x"][tok:tok + 128, :], reads=[dr["x"]], writes=[xt])
            for hf in range(2):
                for k in range(8):
                    mm(P, pm[hf][:], mT[:, k, t4 * 128:(t4 + 1) * 128], wo[:, k, hf * 512:(hf + 1) * 512], k == 0, k == 7, [mT, wo], [pm[hf]])
                P.op("act", lambda e, hf=hf: e.activation(out=junk[:, 0:512], in_=pm[hf][:], func=AF.Square, accum_out=ssa[:, hf:hf + 1]),
                     reads=[pm[hf]], writes=[junk, ssa])
            tt(P, "dve", ss[:], ssa[:, 0:1], ssa[:, 1:2], ALU.add, [ssa], [ss])
            rms_rstd(P, ss, rstd, 1024.0, 1e-6)
            for hf in range(2):
                hs = slice(hf * 512, (hf + 1) * 512)
                P.op("dve", lambda e, hf=hf, hs=hs: e.scalar_tensor_tensor(out=x1[:, hs], in0=pm[hf][:], scalar=rstd[:, 0:1], in1=mod["G2"][:, hs],
                                                                          op0=ALU.mult, op1=ALU.mult), reads=[pm[hf], rstd, mod["G2"]], writes=[x1])
            tt(P, "pool", x1[:], x1[:], xt[:], ALU.add, [x1, xt], [x1])
            P.dma("sp", dr["x1"][tok:tok + 128, :], x1[:], reads=[x1], writes=[dr["x1"]])
            P.op("act", lambda e: e.activation(out=junk[:], in_=x1[:], func=AF.Square, accum_out=ss[:]), reads=[x1], writes=[junk, ss])
            rms_rstd(P, ss, rstd, 1024.0, 1e-6)
            P.op("dve", lambda e: e.tensor_scalar(out=xt[:], in0=x1[:], scalar1=rstd[:, 0:1], scalar2=None, op0=ALU.mult),
                 reads=[x1, rstd], writes=[xt])
            for k in range(8):
                tr(P, ptr[k // 4][:, (k % 4) * 128:(k % 4 + 1) * 128], xt[:, k * 128:(k + 1) * 128], ident[:], [xt, ident], [ptr[k // 4]])
            for k in range(8):
                q = ptr[k // 4]
                P.op("dve", lambda e, q=q, k=k: e.tensor_scalar(out=hf32[:, k, :], in0=q[:, (k % 4) * 128:(k % 4 + 1) * 128],
                                                               scalar1=mod["Af"][:, k:k + 1], scalar2=mod["Bf"][:, k:k + 1],
                                                               op0=ALU.mult, op1=ALU.add), reads=[q, mod["Af"], mod["Bf"]], writes=[hf32])
            tt(P, "pool", junk[:], xt[:], mod["Arow"][:], ALU.mult, [xt, mod["Arow"]], [junk])
            tt(P, "pool", hfrow[:], junk[:], mod["Brow"][:], ALU.add, [junk, mod["Brow"]], [hfrow])
            P.dma("sp", dr["hftok"][tok:tok + 128, :], hfrow[:], reads=[hfrow], writes=[dr["hftok"]])
            for k in range(8):
                mm(P, pl[:, 0:32], hf32[:, k, :], wr[:, k, :], k == 0, False, [hf32, wr], [pl])
            mm(P, pl[:, 0:32], ones[0:1, :], br[:], False, True, [ones, br], [pl])
            P.op("dve", lambda e: e.tensor_copy(out=lg[:], in_=pl[:, 0:32]), reads=[pl], writes=[lg])
            P.op("dve", lambda e: e.max(out=m8[:], in_=lg[:]), reads=[lg], writes=[m8])
            P.op("dve", lambda e: e.tensor_scalar(out=nmx[:], in0=m8[:, 0:1], scalar1=-1.0, scalar2=None, op0=ALU.mult), reads=[m8], writes=[nmx])
            P.op("dve", lambda e: e.tensor_scalar(out=msk[:], in0=lg[:], scalar1=m8[:, 3:4], scalar2=None, op0=ALU.is_ge), reads=[lg, m8], writes=[msk])
            act(P, ex[:], lg[:], AF.Exp, [lg, nmx], [ex], bias=nmx[:, 0:1])
            tt(P, "dve", ex[:], ex[:], msk[:], ALU.mult, [ex, msk], [ex])
            P.op("dve", lambda e: e.reduce_sum(out=sm[:], in_=ex[:], axis=mybir.AxisListType.X), reads=[ex], writes=[sm])
            P.op("dve", lambda e: e.reciprocal(out=sm[:], in_=sm[:]), reads=[sm], writes=[sm])
            P.op("dve", lambda e: e.tensor_scalar(out=ex[:], in0=ex[:], scalar1=sm[:, 0:1], scalar2=None, op0=ALU.mult), reads=[ex, sm], writes=[ex])
            ti_ = g * 4 + t4
            P.op("dve", lambda e, ti_=ti_: e.tensor_copy(out=rt["Gall"][:, ti_, :], in_=ex[:]), reads=[ex], writes=[rt["Gall"]])
            P.op("dve", lambda e, ti_=ti_: e.tensor_copy(out=rt["Mall"][:, ti_, :], in_=msk[:]), reads=[msk], writes=[rt["Mall"]])
    P.barrier()
    P.release(m0)


NB = SEQ * 4 // 128 + NEXP
I32 = mybir.dt.int32
BIGI = 1.0e6


def phase_moe(P, cs, mod, rt, dr):
    m0 = P.mark()
    ones, ident = cs["ones"], cs["ident"]
    Mall, Gall = rt["Mall"], rt["Gall"]
    NT = SEQ // 128
    xg, yb = dr["xg"], dr["yb"]
    IDX = P.sbuf("IDX", [128, NB, 8], I32)
    DEST = P.sbuf("DEST", [128, NT, 4], I32)
    GK = P.sbuf("GK", [128, NT, 4])
    m1 = P.mark()
    zt = P.sbuf("zt", [128, 2 * 1024], BF16)
    P.op("pool", lambda e: e.memset(zt[:], 0.0), writes=[zt])
    xgv = xg.t.rearrange("(n p j) d -> n p (j d)", p=128, j=2)
    for n in range(NB * 128 // 256):
        P.dma("sp" if n % 2 else "act", xgv[n], zt[:], reads=[zt], shared=[xg])
    P.fence(xg)
    it = P.sbuf("it", [128, 192], I32)
    itf = P.sbuf("itf", [128, 192])
    P.op("pool", lambda e: e.iota(it[:], pattern=[[128, 192]], base=0, channel_multiplier=0), writes=[it])
    P.op("dve", lambda e: e.tensor_copy(out=itf[:], in_=it[:]), reads=[it], writes=[itf])
    pi = P.sbuf("pi", [128, 1], I32)
    pif = P.sbuf("pif", [128, 1])
    P.op("pool", lambda e: e.iota(pi[:], pattern=[[0, 1]], base=0, channel_multiplier=1), writes=[pi])
    P.op("dve", lambda e: e.tensor_copy(out=pif[:], in_=pi[:]), reads=[pi], writes=[pif])
    pc = P.psum("pc", [128, 512])
    pq = P.psum("pq", [128, 512])
    for i in range(NT):
        mm(P, pc[:, 0:32], ones[:], Mall[:, i, :], i == 0, i == NT - 1, [ones, Mall], [pc])
    cnt = P.sbuf("cnt", [128, 32])
    P.op("dve", lambda e: e.tensor_copy(out=cnt[:], in_=pc[:, 0:32]), reads=[pc], writes=[cnt])
    cmp = P.sbuf("cmp", [128, 32, 32])
    tt(P, "dve", cmp[:], cnt[:, :, None].broadcast_to([128, 32, 32]), itf[:, None, 0:32].broadcast_to([128, 32, 32]),
       ALU.is_gt, [cnt, itf], [cmp])
    padded = P.sbuf("padded", [128, 32])
    P.op("dve", lambda e: e.reduce_sum(out=padded[:], in_=cmp[:], axis=mybir.AxisListType.X), reads=[cmp], writes=[padded])
    P.op("dve", lambda e: e.tensor_scalar(out=padded[:], in0=padded[:], scalar1=128.0, scalar2=None, op0=ALU.mult), reads=[padded], writes=[padded])
    p_end = P.sbuf("p_end", [128, 32])
    P.op("dve", lambda e: e.tensor_tensor_scan(out=p_end[:], data0=ones[:, 0:32], data1=padded[:], initial=0.0, op0=ALU.mult, op1=ALU.add),
         reads=[ones, padded], writes=[p_end])
    base0 = P.sbuf("base0", [128, 32])
    tt(P, "dve", base0[:], p_end[:], padded[:], ALU.subtract, [p_end, padded], [base0])
    ebc = P.sbuf("ebc", [128, NB, 32])
    tt(P, "dve", ebc[:], p_end[:, None, :].broadcast_to([128, NB, 32]), itf[:, 0:NB, None].broadcast_to([128, NB, 32]),
       ALU.is_le, [p_end, itf], [ebc])
    eb = P.sbuf("eb", [128, NB])
    P.op("dve", lambda e: e.reduce_sum(out=eb[:], in_=ebc[:], axis=mybir.AxisListType.X), reads=[ebc], writes=[eb])
    P.op("dve", lambda e: e.tensor_scalar(out=eb[:], in0=eb[:], scalar1=31.0, scalar2=None, op0=ALU.min), reads=[eb], writes=[eb])
    sk = P.sbuf("sk", [128, NB])
    P.op("pool", lambda e: e.memset(sk[:], 0.0), writes=[sk])
    tt(P, "dve", sk[:, 1:NB], eb[:, 1:NB], eb[:, 0:NB - 1], ALU.is_equal, [eb], [sk])
    P.op("dve", lambda e: e.tensor_scalar(out=sk[:], in0=sk[:], scalar1=BIGI, scalar2=None, op0=ALU.mult), reads=[sk], writes=[sk])
    basef = P.sbuf("basef", [128, NB])
    P.op("dve", lambda e: e.tensor_scalar(out=basef[:], in0=eb[:], scalar1=128.0, scalar2=pif[:, 0:1], op0=ALU.mult, op1=ALU.add),
         reads=[eb, pif], writes=[basef])
    idxf = P.sbuf("idxf", [128, NB, 8])
    for pc_ in range(6):
        mul, add = (4.0, float(pc_)) if pc_ < 4 else (2.0, float(pc_ - 4))
        P.op("dve", lambda e, pc_=pc_, mul=mul, add=add: e.tensor_scalar(out=idxf[:, :, pc_], in0=basef[:], scalar1=mul, scalar2=add,
                                                                         op0=ALU.mult, op1=ALU.add), reads=[basef], writes=[idxf])
        tt(P, "dve", idxf[:, :, pc_], idxf[:, :, pc_], sk[:], ALU.add, [idxf, sk], [idxf])
    tt(P, "dve", idxf[:, :, 6], eb[:], sk[:], ALU.add, [eb, sk], [idxf])
    P.op("dve", lambda e: e.tensor_copy(out=IDX[:, :, 0:7], in_=idxf[:, :, 0:7]), reads=[idxf], writes=[IDX])
    DESTf = P.sbuf("DESTf", [128, NT, 4])
    Dt = P.sbuf("Dt", [128, 32])
    Vt = P.sbuf("Vt", [128, 32])
    oh = P.sbuf("oh", [128, 32])
    m8 = P.sbuf("m8s", [128, 8])
    for i in range(NT):
        mm(P, pq[:, 0:32], cs["Lstrict"][:], Mall[:, i, :], True, True, [cs["Lstrict"], Mall], [pq])
        tt(P, "dve", Dt[:], pq[:, 0:32], base0[:], ALU.add, [pq, base0], [Dt])
        mm(P, pc[:, 0:32], ones[:], Mall[:, i, :], True, True, [ones, Mall], [pc])
        tt(P, "dve", base0[:], base0[:], pc[:, 0:32], ALU.add, [base0, pc], [base0])
        P.op("dve", lambda e: e.tensor_scalar(out=Vt[:], in0=Dt[:], scalar1=-1.0, scalar2=32768.0, op0=ALU.mult, op1=ALU.add), reads=[Dt], writes=[Vt])
        tt(P, "dve", Vt[:], Vt[:], Mall[:, i, :], ALU.mult, [Vt, Mall], [Vt])
        P.op("dve", lambda e: e.max(out=m8[:], in_=Vt[:]), reads=[Vt], writes=[m8])
        P.op("dve", lambda e, i=i: e.tensor_scalar(out=DESTf[:, i, :], in0=m8[:, 0:4], scalar1=-1.0, scalar2=32768.0, op0=ALU.mult, op1=ALU.add),
             reads=[m8], writes=[DESTf])
        for k in range(4):
            P.op("dve", lambda e, k=k: e.tensor_scalar(out=oh[:], in0=Vt[:], scalar1=m8[:, k:k + 1], scalar2=None, op0=ALU.is_equal),
                 reads=[Vt, m8], writes=[oh])
            tt(P, "dve", oh[:], oh[:], Gall[:, i, :], ALU.mult, [oh, Gall], [oh])
            P.op("dve", lambda e, i=i, k=k: e.reduce_sum(out=GK[:, i, k:k + 1], in_=oh[:], axis=mybir.AxisListType.X), reads=[oh], writes=[GK])
    P.op("dve", lambda e: e.tensor_copy(out=DEST[:], in_=DESTf[:]), reads=[DESTf], writes=[DEST])
    hr = [P.sbuf("hr%d" % i, [128, 1024], BF16) for i in range(2)]
    for i in range(NT):
        H = hr[i % 2]
        P.dma("sp", H[:], dr["hftok"][i * 128:(i + 1) * 128, :], reads=[dr["hftok"]], writes=[H])
        for k in range(4):
            P.dma("pool", xg.t[:, :], H[:], reads=[H, DEST], shared=[xg], ind=(DEST[:, i, k:k + 1], True, NB * 128 - 1))
    P.barrier()
    P.release(m1)
    m2 = P.mark()
    identb = P.sbuf("identb", [128, 128], BF16)
    P.op("dve", lambda e: e.tensor_copy(out=identb[:], in_=ident[:]), reads=[ident], writes=[identb])
    wgu_p = [P.sbuf("wgu%d" % i, [128, 8, 512], BF16) for i in range(4)]
    wdn_p = [P.sbuf("wdn%d" % i, [128, 8, 512], BF16) for i in range(2)]
    bgu_b = P.sbuf("bgu_b", [128, 2048])
    bdn_b = P.sbuf("bdn_b", [128, 1024])
    NS = 2
    xs = [P.sbuf("xs%d" % i, [128, 1024], BF16) for i in range(3)]
    xgT = [P.sbuf("xgT%d" % i, [128, 8, 128], BF16) for i in range(NS)]
    hgu = [P.sbuf("hgu%d" % i, [128, 2048]) for i in range(NS)]
    gcb = [P.sbuf("gcb%d" % i, [128, 1024]) for i in range(NS)]
    sgb = [P.sbuf("sgb%d" % i, [128, 1024]) for i in range(NS)]
    u1b = [P.sbuf("u1b%d" % i, [128, 1024]) for i in range(NS)]
    actb = [P.sbuf("actb%d" % i, [128, 1024], BF16) for i in range(NS)]
    actT = [P.sbuf("actT%d" % i, [128, 8, 128], BF16) for i in range(NS)]
    ysb = [P.sbuf("ysb%d" % i, [128, 1024]) for i in range(NS)]
    ptx = P.psum("ptx", [128, 1024], BF16)
    pta = P.psum("pta", [128, 1024], BF16)
    pgu = [P.psum("pgu%d" % i, [128, 512]) for i in range(2)]
    pdn = [P.psum("pdn%d" % i, [128, 512]) for i in range(2)]
    wgu2d, wdn2d = dr["wgu2d"], dr["wdn2d"]
    def stage1(b):
        s_ = b % NS
        for ng in range(4):
            P.dma("pool", wgu_p[ng][:].rearrange("p k n -> p (k n)"), wgu2d.t[:, :], reads=[wgu2d, IDX], writes=[wgu_p[ng]],
                  ind=(IDX[:, b, ng:ng + 1], False, NEXP * 128 * 4 - 1))
        P.dma("pool", bgu_b[:], dr["b_gate_up"].t[:, :], reads=[dr["b_gate_up"], IDX], writes=[bgu_b], ind=(IDX[:, b, 6:7], False, NEXP - 1))
        X = xs[b % 3]
        for k in range(8):
            tr(P, ptx[:, k * 128:(k + 1) * 128], X[:, k * 128:(k + 1) * 128], identb[:], [X, identb], [ptx])
        act(P, xgT[s_][:].rearrange("p k t -> p (k t)"), ptx[:], AF.Copy, [ptx], [xgT[s_]])
        H = hgu[s_]
        for ng in range(4):
            q = pgu[ng % 2]
            for k in range(8):
                mm(P, q[:], xgT[s_][:, k, :], wgu_p[ng][:, k, :], k == 0, k == 7, [xgT[s_], wgu_p[ng]], [q])
            tt(P, "dve", H[:, ng * 512:(ng + 1) * 512], q[:], bgu_b[:, ng * 512:(ng + 1) * 512], ALU.add, [q, bgu_b], [H])

    def stage1b(b):
        s_ = b % NS
        H = hgu[s_]
        P.op("dve", lambda e, H=H, s_=s_: e.tensor_scalar(out=gcb[s_][:], in0=H[:, 0:2048:2], scalar1=7.0, scalar2=None, op0=ALU.min),
             reads=[H], writes=[gcb[s_]])
        act(P, sgb[s_][:], gcb[s_][:], AF.Sigmoid, [gcb[s_]], [sgb[s_]], scale=1.702)
        P.op("dve", lambda e, H=H, s_=s_: e.tensor_scalar(out=u1b[s_][:], in0=H[:, 1:2048:2], scalar1=7.0, scalar2=-7.0, op0=ALU.min, op1=ALU.max),
             reads=[H], writes=[u1b[s_]])
        tt(P, "dve", gcb[s_][:], gcb[s_][:], sgb[s_][:], ALU.mult, [gcb[s_], sgb[s_]], [gcb[s_]])
        P.op("dve", lambda e, s_=s_: e.scalar_tensor_tensor(out=actb[s_][:], in0=u1b[s_][:], scalar=1.0, in1=gcb[s_][:], op0=ALU.add, op1=ALU.mult),
             reads=[u1b[s_], gcb[s_]], writes=[actb[s_]])

    def stage2(b):
        s_ = b % NS
        for hf in range(2):
            P.dma("pool", wdn_p[hf][:].rearrange("p k n -> p (k n)"), wdn2d.t[:, :], reads=[wdn2d, IDX], writes=[wdn_p[hf]],
                  ind=(IDX[:, b, 4 + hf:5 + hf], False, NEXP * 128 * 2 - 1))
        P.dma("pool", bdn_b[:], dr["b_down"].t[:, :], reads=[dr["b_down"], IDX], writes=[bdn_b], ind=(IDX[:, b, 6:7], False, NEXP - 1))
        for k in range(8):
            tr(P, pta[:, k * 128:(k + 1) * 128], actb[s_][:, k * 128:(k + 1) * 128], identb[:], [actb[s_], identb], [pta])
        act(P, actT[s_][:].rearrange("p k t -> p (k t)"), pta[:], AF.Copy, [pta], [actT[s_]])
        Y = ysb[s_]
        for hf in range(2):
            q = pdn[hf]
            for k in range(8):
                mm(P, q[:], actT[s_][:, k, :], wdn_p[hf][:, k, :], k == 0, k == 7, [actT[s_], wdn_p[hf]], [q])
            tt(P, "dve", Y[:, hf * 512:(hf + 1) * 512], q[:], bdn_b[:, hf * 512:(hf + 1) * 512], ALU.add, [q, bdn_b], [Y])
        P.dma("sp", yb.t[b * 128:(b + 1) * 128, :], Y[:], reads=[Y], shared=[yb])

    def loadx(b):
        P.dma("sp", xs[b % 3][:], xg.t[b * 128:(b + 1) * 128, :], reads=[xg], writes=[xs[b % 3]])

    loadx(0)
    loadx(1)
    stage1(0)
    stage1b(0)
    for b in range(NB):
        if b + 2 < NB:
            loadx(b + 2)
        if b + 1 < NB:
            stage1(b + 1)
        stage2(b)
        if b + 1 < NB:
            stage1b(b + 1)
    P.barrier()
    P.release(m2)
    ygs = [[P.sbuf("yg%d_%d" % (i, t), [128, 1024]) for i in range(4)] for t in range(2)]
    xts = [P.sbuf("xte%d" % t, [128, 1024]) for t in range(2)]
    ots = [P.sbuf("ote%d" % t, [128, 1024]) for t in range(2)]
    yas = [P.sbuf("ya%d" % t, [128, 1024]) for t in range(2)]
    ss = P.sbuf("sse", [128, 1])
    rstd = P.sbuf("rstde", [128, 1])

    def gather(i):
        yg = ygs[i % 2]
        for k in range(4):
            P.dma("pool", yg[k][:], yb.t[:, :], reads=[yb, DEST], writes=[yg[k]], ind=(DEST[:, i, k:k + 1], False, NB * 128 - 1))
        P.dma("sp", xts[i % 2][:], dr["x1"][i * 128:(i + 1) * 128, :], reads=[dr["x1"]], writes=[xts[i % 2]])

    gather(0)
    for i in range(NT):
        tok = i * 128
        if i + 1 < NT:
            gather(i + 1)
        yg, xt, ot, ya = ygs[i % 2], xts[i % 2], ots[i % 2], yas[i % 2]
        P.op("dve", lambda e, i=i, yg=yg, ya=ya: e.tensor_scalar(out=ya[:], in0=yg[0][:], scalar1=GK[:, i, 0:1], scalar2=None, op0=ALU.mult),
             reads=[yg[0], GK], writes=[ya])
        for k in range(1, 4):
            P.op("dve", lambda e, i=i, k=k, yg=yg, ya=ya: e.scalar_tensor_tensor(out=ya[:], in0=yg[k][:], scalar=GK[:, i, k:k + 1], in1=ya[:],
                                                                               op0=ALU.mult, op1=ALU.add),
                 reads=[yg[k], GK, ya], writes=[ya])
        P.op("act", lambda e, ot=ot, ya=ya: e.activation(out=ot[:], in_=ya[:], func=AF.Square, accum_out=ss[:]), reads=[ya], writes=[ot, ss])
        rms_rstd(P, ss, rstd, 1024.0, 1e-6)
        P.op("dve", lambda e, ot=ot, ya=ya: e.scalar_tensor_tensor(out=ot[:], in0=ya[:], scalar=rstd[:, 0:1], in1=mod["G5"][:], op0=ALU.mult, op1=ALU.mult),
             reads=[ya, rstd, mod["G5"]], writes=[ot])
        tt(P, "pool", ot[:], ot[:], xt[:], ALU.add, [ot, xt], [ot])
        P.dma("sp", dr["out"][tok:tok + 128, :], ot[:], reads=[ot], writes=[dr["out"]])
    P.barrier()
    P.release(m0)


IN_SPECS = [
    ("x", [SEQ, D], F32), ("ctx", [CTX, D], F32), ("ccol", [128, 16], F32), ("w_ada", [D, 6 * D], F32),
    ("b_ada", [1, 6 * D], F32), ("bada_col", [128, 48], F32), ("gcols", [128, 16], F32),
    ("g_post_mix", [1, D], F32), ("g_post_ffn", [1, D], F32), ("g_pre_ffn", [1, D], F32), ("w_in", [D, NCH_W * 128], F32),
    ("bin_col", [128, NCH_W], F32), ("bv_row", [1, 128], F32), ("cos_t", [128, SEQ], F32), ("sin_t", [128, SEQ], F32),
    ("sink_row", [1, 1024], F32), ("pp64", [64, NP64], F32), ("w2_f", [64, 512], F32), ("w2_b", [64, 512], F32),
    ("a2_f", [64, 512], F32), ("a2_b", [64, 512], F32), ("g2", [128, 512], F32), ("mugl", [128, 2], F32),
    ("w_up_att", [512, D], F32), ("w_up_rwkv", [512, D], F32), ("w_out", [D, D], F32), ("w_router", [D, 32], F32),
    ("b_router", [1, 32], F32), ("wgu2d", [NEXP * 128 * 4, 4096], F32), ("wdn2d", [NEXP * 128 * 2, 4096], F32),
    ("b_down", [NEXP, D], F32), ("b_gate_up", [NEXP, 2 * D], F32),
]
SCRATCH = [
    ("zrT", [1920, TT], F32), ("qT", [512, SEQ], BF16), ("kT", [128, TT], BF16), ("vtok", [TT, 128], BF16),
    ("sgT", [2048, SEQ], BF16), ("attT", [512, SEQ], BF16), ("yT_f", [512, SEQ], F32), ("yT_b", [512, SEQ], F32),
    ("bonusT", [512, SEQ], F32), ("gateT", [512, SEQ], F32), ("rwkT", [512, SEQ], BF16), ("x1", [SEQ, D], F32),
    ("hftok", [SEQ, D], BF16), ("xg", [NB * 128, D], BF16), ("yb", [NB * 128, D], F32),
]


def build(debug=False, phases=None, nexp=NEXP):
    nc = bass.Bass("TRN2", target_bir_lowering=False)
    P = Prog(nc)
    dr = {}
    for n, shp, dt in IN_SPECS:
        dr[n] = Buf(n, nc.dram_tensor(n, list(shp), dt, kind="ExternalInput").ap())
    for n, shp, dt in SCRATCH:
        kind = "ExternalOutput" if debug else "Internal"
        dr[n] = Buf(n, nc.dram_tensor(n, list(shp), dt, kind=kind).ap())
    dr["out"] = Buf("out", nc.dram_tensor("out", [SEQ, D], F32, kind="ExternalOutput").ap())
    ph = phases or ("adaln", "proj", "attn", "rwkv", "rwkv_out", "merge", "moe")
    cs = make_consts(P)
    mod = phase_adaln(P, cs, dr)
    if "proj" in ph:
        phase_proj(P, cs, mod, dr)
    if "attn" in ph:
        phase_attn(P, cs, dr)
    mR = P.mark()
    R = rwkv_setup(P, dr)
    if "rwkv" in ph:
        mW = P.mark()
        PB = ([P.psum("bk%d" % i, [128, 512]) for i in range(6)], [P.psum("bb%d" % i, [128, 1024], BF16) for i in range(2)], [0, 0])
        gens = list(rwkv_dir(P, cs, R, 0, dr, PB)) + list(rwkv_dir(P, cs, R, 1, dr, PB))
        while gens:
            for g_ in list(gens):
                try:
                    next(g_)
                except StopIteration:
                    gens.remove(g_)
        P.barrier()
        P.release(mW)
    if "rwkv_out" in ph:
        phase_rwkv_out(P, cs, R, dr)
    P.barrier()
    P.release(mR)
    rt = {"Mall": P.sbuf("Mall", [128, SEQ // 128, 32]), "Gall": P.sbuf("Gall", [128, SEQ // 128, 32])}
    if "merge" in ph:
        phase_merge(P, cs, mod, dr, rt)
    if "moe" in ph:
        phase_moe(P, cs, mod, rt, dr)
    P.barrier()
    P.emit()
    P.close()
    return nc, P


def host_layout(inp):
    f = lambda a: np.ascontiguousarray(np.asarray(a, np.float32))
    col = lambda v: f(np.asarray(v).reshape(-1, 128).T)
    sh = {}
    sh["w_ada"] = f(inp["w_ada"][0]); sh["b_ada"] = f(inp["b_ada"][0][None])
    sh["bada_col"] = col(inp["b_ada"][0])
    sh["gcols"] = f(np.concatenate([col(inp["g_pre_mix"][0]), col(inp["g_pre_ffn"][0])], 1))
    sh["g_post_mix"] = f(inp["g_post_mix"][0][None]); sh["g_post_ffn"] = f(inp["g_post_ffn"][0][None]); sh["g_pre_ffn"] = f(inp["g_pre_ffn"][0][None])
    w_in = np.asarray(inp["w_in"][0], np.float32); b_in = np.asarray(inp["b_in"][0], np.float32)
    d = np.arange(64)
    partner = np.where((d % 32) < 16, d + 16, d - 16)
    qperm = (np.arange(8)[:, None] * 64 + partner[None, :]).reshape(-1)
    kperm = 512 + (np.arange(2)[:, None] * 64 + partner[None, :]).reshape(-1)
    cols = np.concatenate([np.arange(4736), qperm, kperm])
    sh["w_in"] = f(w_in[:, cols])
    sh["bin_col"] = col(b_in[cols])
    sh["bv_row"] = f(b_in[640:768][None])
    half = 32
    inv_freq = (np.float32(10000.0) ** (-np.arange(0, half, 2, dtype=np.float32) / np.float32(half))).astype(np.float32)
    t = np.arange(SEQ)
    row = (t // 64).astype(np.float32); colp = (t % 64).astype(np.float32)
    dd = np.arange(128) % 64
    pos = np.where((dd < 32)[:, None], row[None, :], colp[None, :]).astype(np.float32)
    ang = (pos * inv_freq[dd % 16][:, None]).astype(np.float32)
    sign = np.where((dd % 32) < 16, -1.0, 1.0).astype(np.float32)[:, None]
    sh["cos_t"] = f(np.cos(ang)); sh["sin_t"] = f(np.sin(ang) * sign)
    sh["sink_row"] = f(np.repeat(np.asarray(inp["att_sinks"][0], np.float32), 128)[None])
    sh["pp64"] = pack64(inp)
    for n in ("w2_f", "w2_b", "a2_f", "a2_b", "g2", "w_up_att", "w_up_rwkv", "w_out", "w_router", "b_down", "b_gate_up"):
        sh[n] = f(inp[n][0])
    wgu = np.asarray(inp["w_gate_up"][0], np.float32).reshape(NEXP, 8, 128, 4, 512)
    sh["wgu2d"] = np.ascontiguousarray(wgu.transpose(0, 2, 3, 1, 4)).reshape(NEXP * 128 * 4, 4096)
    wdn = np.asarray(inp["w_down"][0], np.float32).reshape(NEXP, 8, 128, 2, 512)
    sh["wdn2d"] = np.ascontiguousarray(wdn.transpose(0, 2, 3, 1, 4)).reshape(NEXP * 128 * 2, 4096)
    sh["mugl"] = f(np.stack([inp["mu_prev"][0][1792:1920], inp["mu_next"][0][1792:1920]], 1))
    sh["b_router"] = f(inp["b_router"][0][None])
    cc = np.asarray(inp["c_ctx"], np.float32)
    percore = []
    for b in range(inp["x"].shape[0]):
        m = dict(sh)
        m["x"] = f(inp["x"][b]); m["ctx"] = f(inp["ctx"][b])
        m["ccol"] = f(np.concatenate([col(inp["c"][b]), col(cc)], 1))
        percore.append(m)
    return percore


_NC = {}


def kernel(**inputs):
    if "nc" not in _NC:
        _NC["nc"] = build()[0]
    in_maps = host_layout(inputs)
    res = run_bass_kernel_spmd(_NC["nc"], in_maps, core_ids=list(range(len(in_maps))))
    return np.stack([np.asarray(r["out"], np.float32) for r in res.results], 0)
```
